# Optimizing a Trainium2 kernel written in Bass

```python
import math
import jax, jax.numpy as jnp
from jax import lax
import numpy as np

D_MODEL = 1024
BATCH = 1
SEQ = 16384
DEPTH = 2

N_HEADS_A = 8
HEAD_DIM_A = 64
D_A = N_HEADS_A * HEAD_DIM_A
ATTN_PATTERNS = ((128, 1), (512, 4), (2048, 16))
ATTN_BLOCK = 128
NUM_BUCKETS = 32
MAX_DISTANCE = 2048
N_HEADS_M = 4
HEAD_DIM_M = 128
D_M = N_HEADS_M * HEAD_DIM_M
CONV_K = 4
MLSTM_CHUNK = 128
P_IN = 3 * D_A + 2 * D_M
D_FF = 2816
N_EXPERTS = 8
TOP_K = 2
D_FF_E = 3584
N_DENSE = (DEPTH + 1) // 2
N_MOE = DEPTH // 2
ALPHA = (2.0 * DEPTH) ** 0.25
BETA = (8.0 * DEPTH) ** -0.25
LN_EPS = 1e-5

kernel_name = "hybrid_dilated_attn_mlstm_moe_deepnorm"


def layer_norm(x, g, b):
    xf = x.astype(jnp.float32)
    mu = xf.mean(-1, keepdims=True)
    var = jnp.square(xf - mu).mean(-1, keepdims=True)
    return ((xf - mu) * lax.rsqrt(var + LN_EPS) * g.astype(jnp.float32) + b.astype(jnp.float32)).astype(x.dtype)


def rel_bucket(dist):
    exact = NUM_BUCKETS // 2
    d = jnp.maximum(dist, exact).astype(jnp.float32)
    log_b = exact + (jnp.log(d / exact) / math.log(MAX_DISTANCE / exact) * (NUM_BUCKETS - exact)).astype(jnp.int32)
    return jnp.where(dist < exact, dist, jnp.minimum(log_b, NUM_BUCKETS - 1))


def strided_window_attention(q, k, v, rel_bias, window, dilation):
    B, S, H, hd = q.shape
    L = S // dilation
    W = window // dilation
    blk = ATTN_BLOCK
    nb = -(-L // blk)
    Lp = nb * blk

    def to_classes(t):
        t = t.reshape(B, L, dilation, H, hd).transpose(0, 2, 1, 3, 4).reshape(B * dilation, L, H, hd)
        return jnp.pad(t, ((0, 0), (0, Lp - L), (0, 0), (0, 0)))

    qc, kc, vc = to_classes(q), to_classes(k), to_classes(v)
    qb = qc.reshape(B * dilation, nb, blk, H, hd)

    def band(t):
        t = jnp.pad(t, ((0, 0), (blk, 0), (0, 0), (0, 0))).reshape(B * dilation, nb + 1, blk, H, hd)
        return jnp.concatenate([t[:, :-1], t[:, 1:]], axis=2)

    kb, vb = band(kc), band(vc)
    qi = jnp.arange(blk)[:, None]
    kj = jnp.arange(2 * blk)[None, :]
    rel = qi + blk - kj
    bias = rel_bias[rel_bucket(jnp.maximum(rel, 0) * dilation)]
    bias = bias.transpose(2, 0, 1).astype(jnp.float32)
    key_pos = jnp.arange(nb)[:, None] * blk - blk + kj
    mask = ((rel >= 0) & (rel <= W))[None] & (key_pos >= 0)[:, None, :]

    logits = jnp.einsum('bnqhd,bnkhd->bnhqk', qb.astype(jnp.float32), kb.astype(jnp.float32)) * (hd ** -0.5) + bias
    logits = jnp.where(mask[None, :, None], logits, -jnp.inf)
    m = logits.max(-1, keepdims=True)
    p = jnp.exp(logits - m)
    denom = p.sum(-1, keepdims=True)
    o = jnp.einsum('bnhqk,bnkhd->bnqhd', p / denom, vb.astype(jnp.float32))
    lse = (m + jnp.log(denom))[..., 0].transpose(0, 1, 3, 2)

    def from_classes(t):
        t = t.reshape((B * dilation, Lp) + t.shape[3:])[:, :L]
        t = t.reshape((B, dilation, L) + t.shape[2:])
        t = jnp.moveaxis(t, 1, 2)
        return t.reshape((B, S) + t.shape[3:])

    return from_classes(o), from_classes(lse)


def dilated_attention(q, k, v, rel_bias):
    outs, lses = [], []
    for window, dilation in ATTN_PATTERNS:
        o, lse = strided_window_attention(q, k, v, rel_bias, window, dilation)
        outs.append(o)
        lses.append(lse)
    wts = jax.nn.softmax(jnp.stack(lses), axis=0)
    return jnp.einsum('pbsh,pbshd->bshd', wts, jnp.stack(outs))


def mlstm_chunkwise(q, k, v, i_pre, log_f):
    B, S, H, hd = q.shape
    L = MLSTM_CHUNK
    nc = S // L
    k = k * (hd ** -0.5)
    tri = jnp.tril(jnp.ones((L, L), dtype=bool))

    def chunks(t):
        return jnp.moveaxis(t.reshape((B, nc, L) + t.shape[2:]), 1, 0)

    def step(carry, xs):
        C, n, m = carry
        qc, kc, vc, ic, fc = xs
        b = jnp.cumsum(fc, axis=1).transpose(0, 2, 1)
        ic = ic.transpose(0, 2, 1)
        Dm = jnp.where(tri, b[..., :, None] - b[..., None, :] + ic[..., None, :], -jnp.inf)
        inter = b + m[..., None]
        m_loc = jnp.maximum(inter, Dm.max(-1))
        Dexp = jnp.exp(Dm - m_loc[..., None])
        inter_w = jnp.exp(inter - m_loc)
        s = jnp.einsum('blhd,bshd->bhls', qc, kc) * Dexp
        num = inter_w[..., None] * jnp.einsum('blhd,bhde->bhle', qc, C) + jnp.einsum('bhls,bshe->bhle', s, vc)
        nq = inter_w * jnp.einsum('blhd,bhd->bhl', qc, n) + s.sum(-1)
        h = num / jnp.maximum(jnp.abs(nq), jnp.exp(-m_loc))[..., None]
        bL = b[..., -1]
        g = bL[..., None] - b + ic
        m_new = jnp.maximum(bL + m, g.max(-1))
        decay = jnp.exp(bL + m - m_new)
        w = jnp.exp(g - m_new[..., None])
        C = decay[..., None, None] * C + jnp.einsum('bhs,bshd,bshe->bhde', w, kc, vc)
        n = decay[..., None] * n + jnp.einsum('bhs,bshd->bhd', w, kc)
        return (C, n, m_new), h.transpose(0, 2, 1, 3)

    init = (jnp.zeros((B, H, hd, hd), jnp.float32), jnp.zeros((B, H, hd), jnp.float32),
            jnp.zeros((B, H), jnp.float32))
    _, hs = lax.scan(step, init, (chunks(q), chunks(k), chunks(v), chunks(i_pre), chunks(log_f)))
    return jnp.moveaxis(hs, 0, 1).reshape(B, S, H, hd)


def causal_conv(x, w, b):
    out = lax.conv_general_dilated(x, w[:, None, :].astype(x.dtype), window_strides=(1,),
                                   padding=[(CONV_K - 1, 0)], dimension_numbers=('NWC', 'WIO', 'NWC'),
                                   feature_group_count=x.shape[-1])
    return out + b


def token_mixer(x, rel_bias, w_in, w_gate, b_gate, conv_w, conv_b, w_qk_m, w_v_m, w_if, b_if,
                m_norm_g, w_br_a, w_br_m, w_o):
    B, S, _ = x.shape
    proj = x @ w_in
    q_a, k_a, v_a, x_m, z_m = jnp.split(proj, [D_A, 2 * D_A, 3 * D_A, 3 * D_A + D_M], axis=-1)

    ha = lambda t: t.reshape(B, S, N_HEADS_A, HEAD_DIM_A)
    y_a = dilated_attention(ha(q_a), ha(k_a), ha(v_a), rel_bias).reshape(B, S, D_A).astype(x.dtype)

    hm = lambda t: t.reshape(B, S, N_HEADS_M, HEAD_DIM_M)
    x_c = jax.nn.silu(causal_conv(x_m, conv_w, conv_b))
    q_m = jnp.einsum('bshd,hde->bshe', hm(x_c), w_qk_m[0])
    k_m = jnp.einsum('bshd,hde->bshe', hm(x_c), w_qk_m[1])
    v_m = jnp.einsum('bshd,hde->bshe', hm(x_m), w_v_m)
    qkv = jnp.concatenate([q_m.reshape(B, S, D_M), k_m.reshape(B, S, D_M), v_m.reshape(B, S, D_M)], axis=-1)
    gates = (qkv @ w_if + b_if).astype(jnp.float32)
    i_pre, f_pre = jnp.split(gates, 2, axis=-1)
    f32 = lambda t: t.astype(jnp.float32)
    h = mlstm_chunkwise(f32(q_m), f32(k_m), f32(v_m), i_pre, jax.nn.log_sigmoid(f_pre))
    mu = h.mean(-1, keepdims=True)
    var = jnp.square(h - mu).mean(-1, keepdims=True)
    h = ((h - mu) * lax.rsqrt(var + LN_EPS)).reshape(B, S, D_M) * m_norm_g.astype(jnp.float32)
    y_m = (jax.nn.sigmoid(f32(z_m)) * h).astype(x.dtype)

    g_a, g_m = jnp.split(jax.nn.sigmoid(x @ w_gate + b_gate), 2, axis=-1)
    merged = g_a * (y_a @ w_br_a) + g_m * (y_m @ w_br_m)
    return merged @ w_o


def swiglu(x, w13, w2):
    a, g = jnp.split(x @ w13, 2, axis=-1)
    return (jax.nn.silu(a) * g) @ w2


def moe_ffn(x, router_w, router_b, w13, w2):
    logits = (x @ router_w).astype(jnp.float32) + router_b.astype(jnp.float32)
    top_v, top_i = lax.top_k(logits, TOP_K)
    wts = jax.nn.softmax(top_v, axis=-1)
    gate = jnp.einsum('bsk,bske->bse', wts, jax.nn.one_hot(top_i, N_EXPERTS, dtype=jnp.float32)).astype(x.dtype)
    y = jnp.zeros_like(x)
    for e in range(N_EXPERTS):
        y = y + gate[..., e:e + 1] * swiglu(x, w13[e], w2[e])
    return y


def setup_inputs(seed: int = 0) -> dict:
    key = jax.random.key(seed)
    ks = jax.random.split(key, 24)
    nrm = lambda k, shape, scale: jax.random.normal(k, shape, jnp.float32) * scale
    b_if = jnp.concatenate([nrm(ks[10], (DEPTH, N_HEADS_M), 0.1),
                            3.0 + 3.0 * jax.random.uniform(ks[11], (DEPTH, N_HEADS_M), jnp.float32)], axis=-1)
    return {
        "x": nrm(ks[0], (BATCH, SEQ, D_MODEL), 1.0),
        "rel_bias": nrm(ks[1], (NUM_BUCKETS, N_HEADS_A), 0.2),
        "w_in": nrm(ks[2], (DEPTH, D_MODEL, P_IN), D_MODEL ** -0.5),
        "w_gate": nrm(ks[3], (DEPTH, D_MODEL, 2 * D_MODEL), D_MODEL ** -0.5),
        "b_gate": nrm(ks[4], (DEPTH, 2 * D_MODEL), 0.02),
        "conv_w": nrm(ks[5], (DEPTH, CONV_K, D_M), CONV_K ** -0.5),
        "conv_b": nrm(ks[6], (DEPTH, D_M), 0.02),
        "w_qk_m": nrm(ks[7], (DEPTH, 2, N_HEADS_M, HEAD_DIM_M, HEAD_DIM_M), HEAD_DIM_M ** -0.5),
        "w_v_m": nrm(ks[8], (DEPTH, N_HEADS_M, HEAD_DIM_M, HEAD_DIM_M), HEAD_DIM_M ** -0.5),
        "w_if": nrm(ks[9], (DEPTH, 3 * D_M, 2 * N_HEADS_M), (3 * D_M) ** -0.5),
        "b_if": b_if,
        "m_norm_g": 1.0 + nrm(ks[12], (DEPTH, D_M), 0.02),
        "w_br_a": nrm(ks[13], (DEPTH, D_A, D_MODEL), D_A ** -0.5),
        "w_br_m": nrm(ks[14], (DEPTH, D_M, D_MODEL), D_M ** -0.5),
        "w_o": nrm(ks[15], (DEPTH, D_MODEL, D_MODEL), BETA * D_MODEL ** -0.5),
        "ln_g": 1.0 + nrm(ks[16], (DEPTH, 2, D_MODEL), 0.02),
        "ln_b": nrm(ks[17], (DEPTH, 2, D_MODEL), 0.02),
        "ffn_w13": nrm(ks[18], (N_DENSE, D_MODEL, 2 * D_FF), D_MODEL ** -0.5),
        "ffn_w2": nrm(ks[19], (N_DENSE, D_FF, D_MODEL), BETA * D_FF ** -0.5),
        "router_w": nrm(ks[20], (N_MOE, D_MODEL, N_EXPERTS), D_MODEL ** -0.5),
        "router_b": nrm(ks[21], (N_MOE, N_EXPERTS), 0.01),
        "exp_w13": nrm(ks[22], (N_MOE, N_EXPERTS, D_MODEL, 2 * D_FF_E), D_MODEL ** -0.5),
        "exp_w2": nrm(ks[23], (N_MOE, N_EXPERTS, D_FF_E, D_MODEL), BETA * D_FF_E ** -0.5),
    }


def reference(x, rel_bias, w_in, w_gate, b_gate, conv_w, conv_b, w_qk_m, w_v_m, w_if, b_if, m_norm_g,
              w_br_a, w_br_m, w_o, ln_g, ln_b, ffn_w13, ffn_w2, router_w, router_b, exp_w13, exp_w2):
    for l in range(DEPTH):
        y = token_mixer(x, rel_bias, w_in[l], w_gate[l], b_gate[l], conv_w[l], conv_b[l], w_qk_m[l],
                        w_v_m[l], w_if[l], b_if[l], m_norm_g[l], w_br_a[l], w_br_m[l], w_o[l])
        x = layer_norm(ALPHA * x + y, ln_g[l, 0], ln_b[l, 0])
        j = l // 2
        if l % 2 == 0:
            y = swiglu(x, ffn_w13[j], ffn_w2[j])
        else:
            y = moe_ffn(x, router_w[j], router_b[j], exp_w13[j], exp_w2[j])
        x = layer_norm(ALPHA * x + y, ln_g[l, 1], ln_b[l, 1])
    return x
```

```python
import contextlib
import numpy as np
import ml_dtypes
import concourse.bass as bass
import concourse.mybir as mybir
from concourse.bass_utils import run_bass_kernel_spmd

F32 = mybir.dt.float32
BF = mybir.dt.bfloat16
AF = mybir.ActivationFunctionType
ALU = mybir.AluOpType
BF_NP = ml_dtypes.bfloat16

NCORES = 8
S = 16384
D = 1024
TPC = S // NCORES
DEPTH = 2
ALPHA = (2.0 * DEPTH) ** 0.25
LN_EPS = 1e-5
D_FF = 2816
D_FF_E = 3584
N_EXP = 8
TRUNC = None


class Tile:
    __slots__ = ("name", "w", "r")

    def __init__(self, name=""):
        self.name = name
        self.w = None
        self.r = []


class Op:
    __slots__ = ("eng", "fn", "deps", "signal", "sem", "val", "dma", "idx")


class Prog:
    ENGS = ("pe", "act", "dve", "pool", "sp")

    def __init__(self, nc):
        self.nc = nc
        self.ops = []

    def op(self, eng, fn, reads=(), writes=(), dma=None, nowaw=False):
        o = Op()
        o.eng, o.fn, o.dma, o.signal = eng, fn, dma, dma is not None
        o.idx = len(self.ops)
        deps = set()
        for t in reads:
            if t.w is not None:
                deps.add(t.w)
        for t in writes:
            if t.w is not None and not nowaw:
                deps.add(t.w)
            deps.update(t.r)
        deps.discard(o.idx)
        o.deps = deps
        self.ops.append(o)
        for t in reads:
            t.r.append(o.idx)
        for t in writes:
            t.w = o.idx
            t.r = []
        return o

    def emit(self, final_wait_eng="sp"):
        nc = self.nc
        if TRUNC is not None:
            self.ops = self.ops[:TRUNC]
        ops = self.ops
        for o in ops:
            for d in o.deps:
                ops[d].signal = True
        with contextlib.ExitStack() as st:
            esem = {e: st.enter_context(nc.semaphore("s_" + e)) for e in self.ENGS}
            dkeys = sorted({o.dma for o in ops if o.dma is not None})
            dsem = {k: st.enter_context(nc.semaphore("d_" + str(k))) for k in dkeys}
            cnt = {}
            for o in ops:
                if o.dma is not None:
                    cnt[("d", o.dma)] = cnt.get(("d", o.dma), 0) + 16
                    o.sem, o.val = dsem[o.dma], cnt[("d", o.dma)]
                elif o.signal:
                    cnt[o.eng] = cnt.get(o.eng, 0) + 1
                    o.sem, o.val = esem[o.eng], cnt[o.eng]
                else:
                    o.sem, o.val = None, None
            finals = [(dsem[k], cnt[("d", k)]) for k in dkeys]
            block = st.enter_context(nc.Block())
            per_eng = {e: [o for o in ops if o.eng == e] for e in self.ENGS}

            def run(e, engobj):
                seen = {}
                for o in per_eng[e]:
                    need = {}
                    for d in o.deps:
                        do = ops[d]
                        key = id(do.sem)
                        if seen.get(key, 0) >= do.val:
                            continue
                        if key not in need or need[key][1] < do.val:
                            need[key] = (do.sem, do.val)
                    for key, (s, v) in need.items():
                        engobj.wait_ge(s, v)
                        seen[key] = v
                    ins = o.fn(engobj)
                    if o.signal:
                        ins.then_inc(o.sem, 16 if o.dma is not None else 1)
                if e == final_wait_eng:
                    for s, v in finals:
                        engobj.wait_ge(s, v)

            @block.tensor
            def _(eng):
                run("pe", eng)

            @block.scalar
            def _(eng):
                run("act", eng)

            @block.vector
            def _(eng):
                run("dve", eng)

            @block.gpsimd
            def _(eng):
                run("pool", eng)

            @block.sync
            def _(eng):
                run("sp", eng)


class Builder:
    def __init__(self):
        self.nc = bass.Bass("TRN2", target_bir_lowering=False)
        self.P = Prog(self.nc)
        self.st = contextlib.ExitStack()
        self.n = 0

    def din(self, name, shape, dt=F32):
        return self.nc.dram_tensor(name, list(shape), dt, kind="ExternalInput").ap()

    def dout(self, name, shape, dt=F32):
        return self.nc.dram_tensor(name, list(shape), dt, kind="ExternalOutput").ap()

    def sb(self, shape, dt, name=None):
        self.n += 1
        return self.st.enter_context(self.nc.sbuf_tensor(name or f"sb{self.n}", list(shape), dt))

    def ps(self, shape, dt=F32, name=None):
        self.n += 1
        return self.st.enter_context(self.nc.psum_tensor(name or f"ps{self.n}", list(shape), dt))

    def dma(self, q, out, in_, reads=(), writes=(), key="ld", nowaw=False):
        kw = {"max_dma_last_dim": 4096} if q == "pool" else {}
        self.P.op(q, lambda e: e.dma_start(out=out, in_=in_, **kw), reads, writes, dma=key, nowaw=nowaw)

    def mm(self, out, pairs, reads, writes):
        def fn(e):
            n = len(pairs)
            for i, (l, r) in enumerate(pairs):
                ins = e.matmul(out, lhsT=l, rhs=r, start=(i == 0), stop=(i == n - 1))
            return ins
        self.P.op("pe", fn, reads, writes)

    def pe(self, fn, reads, writes):
        self.P.op("pe", fn, reads, writes)

    def act(self, out, in_, func, reads, writes, bias=None, scale=None):
        kw = {}
        if bias is not None:
            kw["bias"] = bias
        if scale is not None:
            kw["scale"] = scale
        self.P.op("act", lambda e: e.activation(out=out, in_=in_, func=func, **kw), reads, writes)

    def ts(self, out, in0, s1, op0, reads, writes, s2=None, op1=None, eng="dve", nowaw=False):
        kw = {}
        if op1 is not None:
            kw["op1"] = op1
        self.P.op(eng, lambda e: e.tensor_scalar(out=out, in0=in0, scalar1=s1, scalar2=s2, op0=op0, **kw),
                  reads, writes, nowaw=nowaw)

    def stt(self, out, in0, scalar, in1, op0, op1, reads, writes):
        self.P.op("dve", lambda e: e.scalar_tensor_tensor(out=out, in0=in0, scalar=scalar, in1=in1,
                                                          op0=op0, op1=op1), reads, writes)

    def tt(self, out, in0, in1, op, reads, writes, eng="dve", nowaw=False):
        self.P.op(eng, lambda e: e.tensor_tensor(out=out, in0=in0, in1=in1, op=op), reads, writes, nowaw=nowaw)

    def copy(self, out, in_, reads, writes, eng="dve"):
        self.P.op(eng, lambda e: e.tensor_copy(out=out, in_=in_), reads, writes)

    def finish(self):
        self.P.emit()
        self.st.close()
        return self.nc


class Rot:
    def __init__(self, items):
        self.items = items
        self.i = 0

    def next(self):
        it = self.items[self.i % len(self.items)]
        self.i += 1
        return it


HALO = 128
TG = 512


def build_P():
    b = Builder()
    nc = b.nc
    W = TPC + HALO
    xT = b.din("xT", [D, W])
    w_in = b.din("w_in", [D, 2560])
    convw = b.din("convw", [128, 16])
    convb = b.din("convb", [128, 4])
    wqk = b.din("wqk", [128, 8 * 128])
    wv = b.din("wv", [128, 4 * 128])
    wif = b.din("wif", [128, 12 * 8])
    bif = b.din("bif", [4, 2])
    o_qa = b.dout("qaT", [512, TPC], BF)
    o_ka = b.dout("kaT", [512, TPC], BF)
    o_va = b.dout("vaT", [512, TPC], BF)
    o_sz = b.dout("sigzT", [512, TPC], F32)
    o_qm = b.dout("qmT", [512, TPC], BF)
    o_km = b.dout("kmT", [512, TPC], BF)
    o_vm = b.dout("vmT", [512, TPC], BF)
    o_ip = b.dout("ipre", [4, TPC], F32)
    o_lf = b.dout("logf", [4, TPC], F32)

    xb = b.sb([128, 8, W], BF)
    t_xb = [Tile() for _ in range(8)]
    xTv = xT.rearrange("(kc p) t -> p kc t", p=128)
    for kc in range(8):
        b.dma("pool", xb[:, kc, :], xTv[:, kc, :], writes=[t_xb[kc]], key="ldx")
    cw = b.sb([128, 16], F32)
    cb = b.sb([128, 4], F32)
    wqk_f = b.sb([128, 1024], BF)
    wv_f = b.sb([128, 512], BF)
    wif_b = b.sb([128, 96], BF)
    bif_s = b.sb([4, 2], F32)
    t_par = Tile()
    b.dma("sp", cw[:], convw[:, :], writes=[t_par], key="ldp", nowaw=True)
    b.dma("sp", cb[:], convb[:, :], writes=[t_par], key="ldp", nowaw=True)
    b.dma("sp", bif_s[:], bif[:, :], writes=[t_par], key="ldp", nowaw=True)
    t_wq = Tile()
    b.dma("pool", wqk_f[:], wqk[:, :], writes=[t_wq], key="ldw2", nowaw=True)
    b.dma("pool", wv_f[:], wv[:, :], writes=[t_wq], key="ldw2", nowaw=True)
    b.dma("pool", wif_b[:], wif[:, :], writes=[t_wq], key="ldw2", nowaw=True)

    wring = Rot([(b.sb([128, 8, 512], BF), Tile(), f"w{i}") for i in range(3)])
    w_inv = w_in.rearrange("(kc p) n -> p kc n", p=128)

    psr = Rot([(b.ps([128, 512]), Tile()) for _ in range(4)])
    st_bf = Rot([(b.sb([128, 512], BF), Tile(), f"sb{i}") for i in range(4)])
    st_f = Rot([(b.sb([128, 512], F32), Tile(), f"sf{i}") for i in range(3)])

    xm = b.sb([128, 4, W], F32)
    t_xm = [[Tile() for _ in range(5)] for _ in range(4)]

    outs_bf = {0: (o_qa, 0.125), 1: (o_ka, None), 2: (o_va, None)}
    for blk in range(5):
        wt, t_w, wkey = wring.next()
        b.dma("pool", wt[:], w_inv[:, :, blk * 512:(blk + 1) * 512], writes=[t_w], key=wkey)
        for cc in range(4):
            groups = [(HALO + g * TG, TG, g + 1) for g in range(4)]
            if blk == 3:
                groups = [(0, HALO, 0)] + groups
            for (c0, n, gi) in groups:
                pt, t_p = psr.next()
                b.mm(pt[:, 0:n], [(wt[:, kc, cc * 128:(cc + 1) * 128], xb[:, kc, c0:c0 + n]) for kc in range(8)],
                     reads=[t_w] + t_xb, writes=[t_p])
                if blk in outs_bf:
                    dst, sc = outs_bf[blk]
                    s, t_s, skey = st_bf.next()
                    b.act(s[:, 0:n], pt[:, 0:n], AF.Copy, [t_p], [t_s], scale=sc)
                    b.dma("sp", dst[cc * 128:(cc + 1) * 128, c0 - HALO:c0 - HALO + n], s[:, 0:n],
                          reads=[t_s], key=skey)
                elif blk == 3:
                    b.copy(xm[:, cc, c0:c0 + n], pt[:, 0:n], [t_p], [t_xm[cc][gi]])
                else:
                    s, t_s, skey = st_f.next()
                    b.act(s[:, 0:n], pt[:, 0:n], AF.Sigmoid, [t_p], [t_s])
                    b.dma("sp", o_sz[cc * 128:(cc + 1) * 128, c0 - HALO:c0 - HALO + n], s[:, 0:n],
                          reads=[t_s], key=skey)

    xc = b.sb([128, 4, TPC], BF)
    xmb = b.sb([128, 4, TPC], BF)
    acc = [(b.sb([128, TPC], F32), Tile()) for _ in range(2)]
    t_xc = [Tile() for _ in range(4)]
    t_xmb = [Tile() for _ in range(4)]
    for ch in range(4):
        a, t_a = acc[ch % 2]
        rd = t_xm[ch] + [t_par]
        b.ts(a[:], xm[:, ch, HALO - 3:HALO - 3 + TPC], cw[:, ch * 4:ch * 4 + 1], ALU.mult, rd, [t_a])
        for j in range(1, 4):
            b.stt(a[:], xm[:, ch, HALO - 3 + j:HALO - 3 + j + TPC], cw[:, ch * 4 + j:ch * 4 + j + 1], a[:],
                  ALU.mult, ALU.add, rd + [t_a], [t_a])
        b.act(xc[:, ch, :], a[:], AF.Silu, [t_a, t_par], [t_xc[ch]], bias=cb[:, ch:ch + 1])
        b.copy(xmb[:, ch, :], xm[:, ch, HALO:HALO + TPC], t_xm[ch], [t_xmb[ch]], eng="pool")

    qkv = [(b.sb([128, 12, TG], BF), Tile()) for _ in range(2)]
    psg = [(b.ps([4, TG]), Tile()) for _ in range(2)]
    g_f = b.sb([4, TG], F32)
    g_e = b.sb([4, TG], F32)
    g_l = b.sb([4, TG], F32)
    g_i = b.sb([4, TG], F32)
    t_gf, t_ge, t_gl, t_gi = Tile(), Tile(), Tile(), Tile()
    SC_M = 128.0 ** -0.5
    for g in range(4):
        c0 = g * TG
        qt, t_q = qkv[g % 2]
        for which in range(3):
            for h in range(4):
                pt, t_p = psr.next()
                if which < 2:
                    lhsT = wqk_f[:, (which * 4 + h) * 128:(which * 4 + h + 1) * 128]
                    rhs = xc[:, h, c0:c0 + TG]
                    rd = [t_wq, t_xc[h]]
                else:
                    lhsT = wv_f[:, h * 128:(h + 1) * 128]
                    rhs = xmb[:, h, c0:c0 + TG]
                    rd = [t_wq, t_xmb[h]]
                b.mm(pt[:, :], [(lhsT, rhs)], rd, [t_p])
                b.act(qt[:, which * 4 + h, :], pt[:, :], AF.Copy, [t_p], [t_q])
                s, t_s, skey = st_bf.next()
                if which == 0:
                    b.ts(s[:, :], qt[:, which * 4 + h, :], SC_M, ALU.mult, [t_q], [t_s])
                else:
                    b.copy(s[:, :], qt[:, which * 4 + h, :], [t_q], [t_s])
                dst = (o_qm, o_km, o_vm)[which]
                b.dma("sp", dst[h * 128:(h + 1) * 128, c0:c0 + TG], s[:, :], reads=[t_s], key=skey)
        pi, t_pi = psg[0]
        pf, t_pf = psg[1]
        b.mm(pi[:, :], [(wif_b[:, j * 8:j * 8 + 4], qt[:, j, :]) for j in range(12)], [t_wq, t_q], [t_pi])
        b.mm(pf[:, :], [(wif_b[:, j * 8 + 4:j * 8 + 8], qt[:, j, :]) for j in range(12)], [t_wq, t_q], [t_pf])
        b.act(g_i[:], pi[:, :], AF.Identity, [t_pi, t_par], [t_gi], bias=bif_s[:, 0:1])
        b.dma("sp", o_ip[:, c0:c0 + TG], g_i[:], reads=[t_gi], key="sgi")
        b.act(g_f[:], pf[:, :], AF.Identity, [t_pf, t_par], [t_gf], bias=bif_s[:, 1:2])
        b.act(g_e[:], g_f[:], AF.Exp, [t_gf], [t_ge], scale=-1.0)
        b.act(g_l[:], g_e[:], AF.Ln, [t_ge], [t_gl], bias=1.0)
        b.ts(g_f[:], g_l[:], -1.0, ALU.mult, [t_gl], [t_gf])
        b.dma("sp", o_lf[:, c0:c0 + TG], g_f[:], reads=[t_gf], key="sgf")
    return b.finish()


def run_P(xT_ext_list, inp, l):
    nc = build_P()
    w_in = np.ascontiguousarray(inp["w_in"][l])
    convw = np.ascontiguousarray(inp["conv_w"][l].T.reshape(4, 128, 4).transpose(1, 0, 2).reshape(128, 16))
    convb = np.ascontiguousarray(inp["conv_b"][l].reshape(4, 128).T)
    wqk = np.ascontiguousarray(inp["w_qk_m"][l].reshape(8, 128, 128).transpose(1, 0, 2).reshape(128, 1024))
    wv = np.ascontiguousarray(inp["w_v_m"][l].transpose(1, 0, 2).reshape(128, 512))
    wif = np.ascontiguousarray(inp["w_if"][l].reshape(12, 128, 8).transpose(1, 0, 2).reshape(128, 96))
    bif = np.ascontiguousarray(inp["b_if"][l].reshape(2, 4).T)
    maps = [{"xT": xT_ext_list[c], "w_in": w_in, "convw": convw, "convb": convb, "wqk": wqk, "wv": wv,
             "wif": wif, "bif": bif} for c in range(NCORES)]
    res = run_bass_kernel_spmd(nc, maps, core_ids=list(range(NCORES)))
    return res.results


def make_xT_ext(x_tok):
    out = []
    for c in range(NCORES):
        a = np.zeros((D, HALO + TPC), np.float32)
        lo = c * TPC - HALO
        if c == 0:
            a[:, HALO:] = x_tok[0:TPC].T
        else:
            a[:, :] = x_tok[lo:lo + HALO + TPC].T
        out.append(a)
    return out


PATTERNS = (1, 4, 16)
NEG = -30000.0


def a_layout():
    lay = []
    koff = 0
    boff = 0
    for d in PATTERNS:
        nq = TPC // d
        nb = nq // 128
        lay.append(dict(d=d, nq=nq, nb=nb, kcls=128 + nq, koff=koff, boff=boff))
        koff += d * (128 + nq)
        boff += d * (nb + 1)
    return lay, koff, boff


def build_A():
    b = Builder()
    lay, KTOT, NBLK = a_layout()
    qh = b.din("qh", [4, 3, 64, 2 * TPC], BF)
    kh = b.din("kh", [4, 64, 2, KTOT], BF)
    vh = b.din("vh", [4, 128, NBLK * 130], BF)
    bmn = b.din("bmn", [12, 128, 512])
    bmf = b.din("bmf", [12, 128, 512])
    ident_in = b.din("ident", [128, 128])
    oa = b.dout("oa", [3, 4, TPC, 130], F32)

    ident = b.sb([128, 128], BF)
    t_c = Tile()
    b.dma("pool", ident[:], ident_in[:, :], writes=[t_c], key="ldc", nowaw=True)
    bn_sb = b.sb([128, 12, 512], BF)
    bf_sb = b.sb([128, 12, 512], BF)
    b.dma("pool", bn_sb[:], bmn.rearrange("n p c -> p n c"), writes=[t_c], key="ldc", nowaw=True)
    b.dma("pool", bf_sb[:], bmf.rearrange("n p c -> p n c"), writes=[t_c], key="ldc", nowaw=True)

    pset = []
    for pi, L in enumerate(lay):
        klen = L["d"] * L["kcls"]
        nblk = L["d"] * (L["nb"] + 1)
        pset.append(dict(q=b.sb([64, 2, TPC], BF), k=b.sb([64, 2, klen], BF), v=b.sb([128, nblk, 130], BF),
                         t=Tile(), key=f"in{pi}", klen=klen, nblk=nblk))
    sbank = Rot([(b.ps([128, 512]), Tile()) for _ in range(3)])
    obank = Rot([(b.ps([128, 512]), Tile()) for _ in range(2)])
    ptr = Rot([(b.sb([128, 512], BF), Tile()) for _ in range(3)])
    ostg = Rot([(b.sb([128, 16, 130], F32), Tile(), f"os{i}") for i in range(2)])

    for hp in range(4):
        for pi, L in enumerate(lay):
            d, nq, nb = L["d"], L["nq"], L["nb"]
            s = pset[pi]
            b.dma("sp", s["q"][:].rearrange("p h t -> p (h t)"), qh[hp, pi], writes=[s["t"]], key=s["key"])
            b.dma("sp", s["k"][:], kh[hp][:, :, L["koff"]:L["koff"] + s["klen"]], writes=[s["t"]], key=s["key"],
                  nowaw=True)
            b.dma("sp", s["v"][:].rearrange("p n c -> p (n c)"),
                  vh[hp][:, L["boff"] * 130:(L["boff"] + s["nblk"]) * 130], writes=[s["t"]], key=s["key"], nowaw=True)
            og, t_og, okey = ostg.next()
            for r in range(d):
                for qb in range(1, nb + 1):
                    qpos = r * nq + (qb - 1) * 128
                    sb_, t_sb = sbank.next()
                    tab = bf_sb if qb == 1 else bn_sb
                    n12 = pi * 4 + hp

                    def fn(e, sb_=sb_, tab=tab, n12=n12, s=s, L=L, r=r, qb=qb, qpos=qpos, pi=pi):
                        e.matmul(sb_[:, :], lhsT=ident[:], rhs=tab[:, n12, :], start=True, stop=False,
                                 skip_group_check=True)
                        for hh in range(2):
                            for w in range(2):
                                kpos = r * L["kcls"] + (qb - 1 + w) * 128
                                c0 = (hh * 2 + w) * 128
                                ins = e.matmul(sb_[:, c0:c0 + 128],
                                               lhsT=s["k"][:, hh, kpos:kpos + 128],
                                               rhs=s["q"][:, hh, qpos:qpos + 128],
                                               start=False, stop=True, skip_group_check=True)
                        return ins
                    b.pe(fn, [t_c, s["t"]], [t_sb])
                    pt, t_pt = ptr.next()
                    b.act(pt[:, :], sb_[:, :], AF.Exp, [t_sb], [t_pt, t_sb])
                    ob, t_ob = obank.next()

                    def fn2(e, ob=ob, pt=pt, s=s, L=L, r=r, qb=qb):
                        for hh in range(2):
                            for w in range(2):
                                blk = r * (L["nb"] + 1) + (qb - 1 + w)
                                c0 = (hh * 2 + w) * 128
                                ins = e.matmul(ob[:, hh * 65:hh * 65 + 65], lhsT=pt[:, c0:c0 + 128],
                                               rhs=s["v"][:, blk, hh * 65:hh * 65 + 65],
                                               start=(w == 0), stop=(w == 1))
                        return ins
                    b.pe(fn2, [t_pt, s["t"]], [t_ob])
                    b.copy(og[:, qpos // 128, :], ob[:, 0:130], [t_ob], [t_og, t_ob])
            b.dma("sp", oa[pi, hp].rearrange("(n q) c -> q n c", q=128), og[:], reads=[t_og], key=okey)
    return b.finish()


def _bucket(dist):
    dist = np.asarray(dist, np.int64)
    dd = np.maximum(dist, 16).astype(np.float32)
    lb = 16 + (np.log(dd / np.float32(16)) / np.float32(np.log(2048.0 / 16.0)) * np.float32(16)).astype(np.int32)
    return np.where(dist < 16, dist, np.minimum(lb, 31))


def a_bias_tables(rel_bias):
    k = np.arange(128)[:, None]
    q = np.arange(128)[None, :]
    tn = np.full((3, 4, 128, 2, 2, 128), NEG, np.float32)
    for pi, d in enumerate(PATTERNS):
        for w in range(2):
            rel = q + 128 - (k + 128 * w)
            valid = (rel >= 0) & (rel <= 128)
            bk = _bucket(np.maximum(rel, 0) * d)
            for h in range(8):
                vals = rel_bias[bk, h]
                tn[pi, h // 2, :, h % 2, w, :] = np.where(valid, vals, NEG)
    tf = tn.copy()
    tf[:, :, :, :, 0, :] = NEG
    return tn.reshape(12, 128, 512), tf.reshape(12, 128, 512)


def a_indices(core):
    lay, KTOT, NBLK = a_layout()
    base = core * TPC
    qidx, kidx = [], []
    for L in lay:
        d, nq = L["d"], L["nq"]
        qi = np.concatenate([base + r + d * np.arange(nq) for r in range(d)])
        ki = np.concatenate([base + r + d * (np.arange(128 + nq) - 128) for r in range(d)])
        qidx.append(qi)
        kidx.append(np.where(ki < 0, -1, ki))
    return qidx, np.concatenate(kidx)


def run_A(qa, ka, va, rel_bias):
    nc = build_A()
    tn, tf = a_bias_tables(rel_bias)
    ident = np.eye(128, dtype=np.float32)
    kz = np.concatenate([ka, np.zeros((1, 512), ka.dtype)], 0)
    vz = np.concatenate([va, np.zeros((1, 512), va.dtype)], 0)
    maps = []
    qidx_all = []
    for c in range(NCORES):
        qidx, kidx = a_indices(c)
        qidx_all.append(qidx)
        qh = np.stack([np.stack([qa[qi][:, hp * 128:(hp + 1) * 128].reshape(-1, 2, 64).transpose(2, 1, 0).reshape(64, -1)
                                 for qi in qidx]) for hp in range(4)])
        kg = kz[kidx]
        kh = np.stack([kg[:, hp * 128:(hp + 1) * 128].reshape(-1, 2, 64).transpose(2, 1, 0) for hp in range(4)])
        vg = vz[kidx].reshape(-1, 128, 4, 2, 64)
        ve = np.concatenate([vg, np.ones(vg.shape[:-1] + (1,), vg.dtype)], -1)
        vh = np.ascontiguousarray(ve.transpose(2, 1, 0, 3, 4)).reshape(4, 128, -1)
        maps.append({"qh": np.ascontiguousarray(qh), "kh": np.ascontiguousarray(kh), "vh": vh,
                     "bmn": tn, "bmf": tf if c == 0 else tn, "ident": ident})
    res = run_bass_kernel_spmd(nc, maps, core_ids=list(range(NCORES))).results
    out = np.zeros((3, S, 4, 130), np.float32)
    for c in range(NCORES):
        o = np.asarray(res[c]["oa"])
        for pi in range(3):
            out[pi, qidx_all[c][pi]] = o[pi].transpose(1, 0, 2)
    return out.reshape(3, S, 8, 65)


NCH = S // 128


def build_M():
    b = Builder()
    qT = b.din("qT", [128, S], BF)
    kT = b.din("kT", [128, S], BF)
    ktok = b.din("ktok", [128, NCH * 128], BF)
    vext = b.din("vext", [128, NCH * 65], BF)
    ipre = b.din("ipre", [128, NCH])
    logf = b.din("logf", [128, NCH])
    U_in = b.din("U", [128, 128])
    NG_in = b.din("NEGM", [128, 128])
    on_in = b.din("ones", [128, 128])
    id_in = b.din("identf", [128, 128])
    ho = b.dout("ho", [128, NCH * 64], F32)

    U = b.sb([128, 128], F32)
    NG = b.sb([128, 128], F32)
    ON = b.sb([128, 128], F32)
    IDF = b.sb([128, 128], F32)
    ip_sb = b.sb([128, NCH], F32)
    lf_sb = b.sb([128, NCH], F32)
    t_c = Tile()
    for dst, src in ((U, U_in), (NG, NG_in), (ON, on_in), (IDF, id_in), (ip_sb, ipre), (lf_sb, logf)):
        b.dma("sp", dst[:], src[:, :], writes=[t_c], key="ldc", nowaw=True)
    q_sb = b.sb([128, S], BF)
    k_sb = b.sb([128, S], BF)
    kt_sb = b.sb([128, NCH, 128], BF)
    v_sb = b.sb([128, NCH, 65], BF)
    NPC = 4
    CPP = NCH // NPC
    t_in = [Tile() for _ in range(NPC)]
    for pc in range(NPC):
        key = f"in{pc}"
        c0, c1 = pc * CPP, (pc + 1) * CPP
        b.dma("sp", q_sb[:, c0 * 128:c1 * 128], qT[:, c0 * 128:c1 * 128], writes=[t_in[pc]], key=key, nowaw=True)
        b.dma("sp", k_sb[:, c0 * 128:c1 * 128], kT[:, c0 * 128:c1 * 128], writes=[t_in[pc]], key=key, nowaw=True)
        b.dma("sp", kt_sb[:, c0:c1, :].rearrange("p n c -> p (n c)"), ktok[:, c0 * 128:c1 * 128],
              writes=[t_in[pc]], key=key, nowaw=True)
        b.dma("sp", v_sb[:, c0:c1, :].rearrange("p n c -> p (n c)"), vext[:, c0 * 65:c1 * 65],
              writes=[t_in[pc]], key=key, nowaw=True)

    ps_b = (b.ps([128, 512]), Tile())
    bcol = b.sb([128, NCH], F32)
    imb = b.sb([128, NCH], F32)
    eb = b.sb([128, NCH], F32)
    t_bcol, t_imb, t_eb = Tile(), Tile(), Tile()
    b.mm(ps_b[0][:, 0:NCH], [(U[:], lf_sb[:])], [t_c], [ps_b[1]])
    b.copy(bcol[:], ps_b[0][:, 0:NCH], [ps_b[1]], [t_bcol, ps_b[1]])
    b.tt(imb[:], ip_sb[:], bcol[:], ALU.subtract, [t_c, t_bcol], [t_imb])
    b.act(eb[:], bcol[:], AF.Exp, [t_bcol], [t_eb])

    C = b.sb([128, 65], F32)
    Cb = b.sb([128, 65], BF)
    t_C, t_Cb = Tile(), Tile()
    b.P.op("dve", lambda e: e.memset(C[:], 0.0), (), [t_C])
    b.P.op("dve", lambda e: e.memset(Cb[:], 0.0), (), [t_Cb])

    def rot(n, shape, dt):
        return Rot([(b.sb(shape, dt), Tile()) for _ in range(n)])
    LUr = rot(2, [128, 128], F32)
    DTr = rot(2, [128, 128], F32)
    dcr = rot(2, [128, 1], F32)
    SWr = rot(2, [128, 128], BF)
    hir = rot(2, [128, 65], F32)
    Htr = rot(2, [128, 65], F32)
    denr = rot(2, [128, 1], F32)
    rdr = rot(2, [128, 1], F32)
    kwr = rot(2, [128, 128], BF)
    Bmr = Rot([(b.ps([128, 512]), Tile()) for _ in range(2)])
    STr = Rot([(b.ps([128, 512]), Tile()) for _ in range(2)])
    Hi = (b.ps([128, 512]), Tile())
    He = (b.ps([128, 512]), Tile())
    dC = (b.ps([128, 512]), Tile())
    GRP = 16
    hout = Rot([(b.sb([128, GRP, 64], F32), Tile(), f"ho{i}") for i in range(2)])
    hcur = None
    for c in range(NCH):
        pc = c // CPP
        tin = t_in[pc]
        cs = slice(c * 128, (c + 1) * 128)
        if c % GRP == 0:
            hcur = hout.next()
        LU, t_LU = LUr.next()
        b.ts(LU[:], U[:], lf_sb[:, c:c + 1], ALU.mult, [t_c], [t_LU])
        Bm, t_Bm = Bmr.next()

        def fnb(e, Bm=Bm, LU=LU):
            e.matmul(Bm[:, 0:128], lhsT=ON[:], rhs=LU[:], start=True, stop=False)
            return e.matmul(Bm[:, 0:128], lhsT=IDF[:], rhs=NG[:], start=False, stop=True)
        b.pe(fnb, [t_c, t_LU], [t_Bm])
        DT, t_DT = DTr.next()
        b.act(DT[:], Bm[:, 0:128], AF.Exp, [t_Bm, t_imb], [t_DT, t_Bm], bias=imb[:, c:c + 1])
        dcol, t_dc = dcr.next()
        b.act(dcol[:], Bm[:, 127:128], AF.Exp, [t_Bm], [t_dc, t_Bm])
        ST, t_ST = STr.next()
        b.mm(ST[:, 0:128], [(k_sb[:, cs], q_sb[:, cs])], [tin], [t_ST])
        SW, t_SW = SWr.next()
        b.tt(SW[:], ST[:, 0:128], DT[:], ALU.mult, [t_ST, t_DT], [t_SW, t_ST])
        b.mm(Hi[0][:, 0:65], [(SW[:], v_sb[:, c, :])], [t_SW, tin], [Hi[1]])
        b.mm(He[0][:, 0:65], [(q_sb[:, cs], Cb[:])], [tin, t_Cb], [He[1]])
        hi, t_hi = hir.next()
        b.act(hi[:], Hi[0][:, 0:65], AF.Copy, [Hi[1]], [t_hi, Hi[1]])
        Ht, t_Ht = Htr.next()
        b.stt(Ht[:], He[0][:, 0:65], eb[:, c:c + 1], hi[:], ALU.mult, ALU.add, [He[1], t_eb, t_hi], [t_Ht, He[1]])
        den, t_den = denr.next()
        b.act(den[:], Ht[:, 64:65], AF.Abs, [t_Ht], [t_den])
        b.ts(den[:], den[:], 1.0, ALU.max, [t_den], [t_den])
        rd, t_rd = rdr.next()
        b.P.op("dve", lambda e, rd=rd, den=den: e.reciprocal(out=rd[:], in_=den[:]), [t_den], [t_rd])
        b.ts(hcur[0][:, c % GRP, :], Ht[:, 0:64], rd[:, 0:1], ALU.mult, [t_Ht, t_rd], [hcur[1]])
        kw, t_kw = kwr.next()
        b.ts(kw[:], kt_sb[:, c, :], DT[:, 127:128], ALU.mult, [tin, t_DT], [t_kw])
        b.mm(dC[0][:, 0:65], [(kw[:], v_sb[:, c, :])], [t_kw, tin], [dC[1]])
        b.stt(C[:], C[:], dcol[:, 0:1], dC[0][:, 0:65], ALU.mult, ALU.add, [t_C, t_dc, dC[1]], [t_C, dC[1]])
        b.act(Cb[:], C[:], AF.Copy, [t_C], [t_Cb])
        if c % GRP == GRP - 1:
            g = c // GRP
            b.dma("sp", ho[:, g * GRP * 64:(g + 1) * GRP * 64], hcur[0][:].rearrange("p n c -> p (n c)"),
                  reads=[hcur[1]], key=hcur[2])
    return b.finish()


def run_M(qm, km, vm, ipre, logf):
    nc = build_M()
    a = np.arange(128)
    U = (a[:, None] <= a[None, :]).astype(np.float32)
    NEGM = np.where(a[:, None] <= a[None, :], 0.0, NEG).astype(np.float32)
    ones = np.ones((128, 128), np.float32)
    identf = np.eye(128, dtype=np.float32)
    maps = []
    for c in range(NCORES):
        h, vh = c // 2, c % 2
        qh = qm[:, h * 128:(h + 1) * 128]
        kh = km[:, h * 128:(h + 1) * 128]
        v = vm[:, h * 128 + vh * 64:h * 128 + vh * 64 + 64].reshape(NCH, 128, 64)
        ve = np.concatenate([v, np.ones((NCH, 128, 1), v.dtype)], -1)
        maps.append({
            "qT": np.ascontiguousarray(qh.T), "kT": np.ascontiguousarray(kh.T),
            "ktok": np.ascontiguousarray(kh.reshape(NCH, 128, 128).transpose(1, 0, 2)).reshape(128, -1),
            "vext": np.ascontiguousarray(ve.transpose(1, 0, 2)).reshape(128, -1),
            "ipre": np.ascontiguousarray(ipre[:, h].reshape(NCH, 128).T),
            "logf": np.ascontiguousarray(logf[:, h].reshape(NCH, 128).T),
            "U": U, "NEGM": NEGM, "ones": ones, "identf": identf})
    res = run_bass_kernel_spmd(nc, maps, core_ids=list(range(NCORES))).results
    out = np.zeros((S, 4, 128), np.float32)
    for c in range(NCORES):
        h, vh = c // 2, c % 2
        o = np.asarray(res[c]["ho"]).reshape(128, NCH, 64)
        out[:, h, vh * 64:(vh + 1) * 64] = o.transpose(1, 0, 2).reshape(S, 64)
    return out


def ln_tile(b, z, t_z, out, t_out, lng, lnb, t_par, scr):
    st, mv, sq, rs = scr
    t_st, t_mv, t_sq, t_rs = Tile(), Tile(), Tile(), Tile()
    b.P.op("dve", lambda e: e.bn_stats(out=st[:, 0:6], in_=z[:, 0:512]), [t_z], [t_st])
    b.P.op("dve", lambda e: e.bn_stats(out=st[:, 6:12], in_=z[:, 512:1024]), [t_z], [t_st], nowaw=True)
    b.P.op("dve", lambda e: e.bn_aggr(out=mv[:, 0:2], in_=st[:, 0:12]), [t_st], [t_mv])
    b.act(sq[:, 0:1], mv[:, 1:2], AF.Sqrt, [t_mv, t_par], [t_sq], bias=EPS_AP[0][:, 0:1])
    b.P.op("dve", lambda e: e.reciprocal(out=rs[:, 0:1], in_=sq[:, 0:1]), [t_sq], [t_rs])
    b.ts(out[:], z[:], mv[:, 0:1], ALU.subtract, [t_z, t_mv, t_rs], [t_out], s2=rs[:, 0:1], op1=ALU.mult)
    b.tt(out[:], out[:], lng[:], ALU.mult, [t_out, t_par], [t_out])
    b.tt(out[:], out[:], lnb[:], ALU.add, [t_out, t_par], [t_out])


EPS_AP = [None]


def build_D1():
    b = Builder()
    xT = b.din("xT", [D, TPC])
    x_tok = b.din("x_tok", [TPC, D])
    oa_tok = b.din("oa_tok", [3, TPC, 520])
    hm_in = b.din("hm", [TPC, 512])
    sz_in = b.din("sigz", [TPC, 512])
    gm_in = b.din("gm_b", [128, 512])
    lng_in = b.din("lng_b", [128, D])
    lnb_in = b.din("lnb_b", [128, D])
    wg_in = b.din("w_gate", [D, 2048])
    bg_in = b.din("bg", [128, 16])
    wbra_in = b.din("w_br_a", [512, D])
    wbrm_in = b.din("w_br_m", [512, D])
    wo_in = b.din("w_o", [D, D])
    id_in = b.din("ident", [128, 128])
    eps_in = b.din("eps", [128, 1])
    x1 = b.dout("x1", [TPC, D], F32)

    t_par = Tile()
    gm = b.sb([128, 512], F32)
    lng = b.sb([128, D], F32)
    lnb = b.sb([128, D], F32)
    bg = b.sb([128, 16], F32)
    eps = b.sb([128, 1], F32)
    EPS_AP[0] = eps
    for dst, src in ((gm, gm_in), (lng, lng_in), (lnb, lnb_in), (bg, bg_in), (eps, eps_in)):
        b.dma("sp", dst[:], src[:, :], writes=[t_par], key="ldp", nowaw=True)
    ident = b.sb([128, 128], BF)
    wbra = b.sb([128, 4, D], BF)
    wbrm = b.sb([128, 4, D], BF)
    wo = b.sb([128, 8, D], BF)
    xb = b.sb([128, 8, TPC], BF)
    t_w = Tile()
    b.dma("pool", ident[:], id_in[:, :], writes=[t_w], key="ldw", nowaw=True)
    t_xb = Tile()
    xTv = xT.rearrange("(kc p) t -> p kc t", p=128)
    for kc in range(8):
        b.dma("pool", xb[:, kc, :], xTv[:, kc, :], writes=[t_xb], key="ldx", nowaw=True)
    b.dma("pool", wbra[:], wbra_in.rearrange("(kc p) n -> p kc n", p=128), writes=[t_w], key="ldw", nowaw=True)
    b.dma("pool", wbrm[:], wbrm_in.rearrange("(kc p) n -> p kc n", p=128), writes=[t_w], key="ldw", nowaw=True)
    wov = wo_in.rearrange("(kc p) n -> p kc n", p=128)
    for kc in range(0, 8, 2):
        b.dma("pool", wo[:, kc:kc + 2, :], wov[:, kc:kc + 2, :], writes=[t_w], key="ldw", nowaw=True)
    wgv = wg_in.rearrange("(kc p) (j n) -> p kc j n", p=128, j=2)
    wgr = Rot([(b.sb([128, 8, 2, 128], BF), Tile(), f"wg{i}") for i in range(2)])

    def rot(n, shape, dt, key=None):
        return Rot([(b.sb(shape, dt), Tile(), (f"{key}{i}" if key else None)) for i in range(n)])
    o3r = rot(1, [128, 3, 520], F32, "o3")
    hmr = rot(2, [128, 512], F32, "hm")
    szr = rot(2, [128, 512], F32, "sz")
    s1 = (b.sb([128, 520], F32), Tile())
    rd = (b.sb([128, 8], F32), Tile())
    yab = (b.sb([128, 512], BF), Tile())
    ymb = (b.sb([128, 512], BF), Tile())
    hn = (b.sb([128, 512], F32), Tile())
    st6 = b.sb([128, 4, 6], F32)
    mv = b.sb([128, 4, 2], F32)
    sq4 = b.sb([128, 4], F32)
    rs4 = b.sb([128, 4], F32)
    t_st6, t_mv, t_sq4, t_rs4 = Tile(), Tile(), Tile(), Tile()
    yTr = Rot([(b.sb([128, 4, TG], BF), b.sb([128, 4, TG], BF), Tile()) for _ in range(2)])
    mgr = Rot([(b.sb([128, 8, TG], BF), Tile()) for _ in range(2)])
    sgr = rot(2, [128, TG], F32)
    t1r = rot(2, [128, TG], F32)
    xtr = rot(2, [128, D], F32, "xt")
    zr = rot(2, [128, D], F32)
    outr = rot(2, [128, D], F32, "ot")
    lscr = (b.sb([128, 12], F32), b.sb([128, 2], F32), b.sb([128, 1], F32), b.sb([128, 1], F32))
    psr = Rot([(b.ps([128, 512]), Tile()) for _ in range(5)])
    tp = (b.ps([128, 8, 128], BF), Tile())
    ybk = Rot([(b.ps([128, 512]), Tile()) for _ in range(2)])

    for tg in range(4):
        yaT, ymT, t_yT = yTr.next()
        for ti in range(4):
            tt = tg * 4 + ti
            rows = slice(tt * 128, (tt + 1) * 128)
            o3, t_o3, k_o3 = o3r.next()
            b.dma("sp", o3[:], oa_tok[:, rows, :].rearrange("n q c -> q n c"), writes=[t_o3], key=k_o3)
            hmt, t_hm, k_hm = hmr.next()
            b.dma("sp", hmt[:], hm_in[rows, :], writes=[t_hm], key=k_hm)
            szt, t_sz, k_sz = szr.next()
            b.dma("sp", szt[:], sz_in[rows, :], writes=[t_sz], key=k_sz)
            b.tt(s1[0][:], o3[:, 0, :], o3[:, 1, :], ALU.add, [t_o3], [s1[1]])
            b.tt(s1[0][:], s1[0][:], o3[:, 2, :], ALU.add, [t_o3, s1[1]], [s1[1]])
            s1v = s1[0][:].rearrange("p (h c) -> p h c", c=65)
            b.P.op("dve", lambda e, s1v=s1v: e.reciprocal(out=rd[0][:], in_=s1v[:, :, 64]), [s1[1]], [rd[1]])
            for h in range(8):
                b.ts(yab[0][:, h * 64:(h + 1) * 64], s1v[:, h, 0:64], rd[0][:, h:h + 1], ALU.mult,
                     [s1[1], rd[1]], [yab[1]])
            for h in range(4):
                b.P.op("dve", lambda e, h=h, hmt=hmt: e.bn_stats(out=st6[:, h, :], in_=hmt[:, h * 128:(h + 1) * 128]),
                       [t_hm], [t_st6])
                b.P.op("dve", lambda e, h=h: e.bn_aggr(out=mv[:, h, :], in_=st6[:, h, :]), [t_st6], [t_mv])
            b.act(sq4[:], mv[:, :, 1], AF.Sqrt, [t_mv, t_par], [t_sq4], bias=eps[:, 0:1])
            b.P.op("dve", lambda e: e.reciprocal(out=rs4[:], in_=sq4[:]), [t_sq4], [t_rs4])
            for h in range(4):
                b.ts(hn[0][:, h * 128:(h + 1) * 128], hmt[:, h * 128:(h + 1) * 128], mv[:, h, 0:1], ALU.subtract,
                     [t_hm, t_mv, t_rs4], [hn[1]], s2=rs4[:, h:h + 1], op1=ALU.mult)
            b.tt(hn[0][:], hn[0][:], gm[:], ALU.mult, [hn[1], t_par], [hn[1]])
            b.tt(ymb[0][:], hn[0][:], szt[:], ALU.mult, [hn[1], t_sz], [ymb[1]])

            def ftp(e):
                for j in range(4):
                    e.transpose(tp[0][:, j, :], yab[0][:, j * 128:(j + 1) * 128], ident[:])
                for j in range(4):
                    ins = e.transpose(tp[0][:, 4 + j, :], ymb[0][:, j * 128:(j + 1) * 128], ident[:])
                return ins
            b.pe(ftp, [yab[1], ymb[1], t_w], [tp[1]])
            cs = slice(ti * 128, (ti + 1) * 128)
            b.copy(yaT[:, :, cs], tp[0][:, 0:4, :], [tp[1]], [t_yT, tp[1]])
            b.act(ymT[:, :, cs], tp[0][:, 4:8, :], AF.Copy, [tp[1]], [t_yT, tp[1]])
        mg, t_mg = mgr.next()
        tcols = slice(tg * TG, (tg + 1) * TG)
        for n in range(8):
            wg, t_wg, k_wg = wgr.next()
            b.dma("pool", wg[:, :, 0, :], wgv[:, :, 0, n * 128:(n + 1) * 128], writes=[t_wg], key=k_wg)
            b.dma("pool", wg[:, :, 1, :], wgv[:, :, 1, n * 128:(n + 1) * 128], writes=[t_wg], key=k_wg, nowaw=True)
            t1, t_t1, _ = t1r.next()
            for j, (wbr, yT) in enumerate(((wbra, yaT), (wbrm, ymT))):
                pa, t_pa = psr.next()
                b.mm(pa[:, :], [(wbr[:, kc, n * 128:(n + 1) * 128], yT[:, kc, :]) for kc in range(4)],
                     [t_w, t_yT], [t_pa])
                ga, t_ga = psr.next()
                b.mm(ga[:, :], [(wg[:, kc, j, :], xb[:, kc, tcols]) for kc in range(8)], [t_wg, t_xb], [t_ga])
                sg, t_sg, _ = sgr.next()
                b.act(sg[:], ga[:, :], AF.Sigmoid, [t_ga, t_par], [t_sg, t_ga], bias=bg[:, j * 8 + n:j * 8 + n + 1])
                if j == 0:
                    b.tt(t1[:], pa[:, :], sg[:], ALU.mult, [t_pa, t_sg], [t_t1, t_pa])
                else:
                    b.tt(sg[:], pa[:, :], sg[:], ALU.mult, [t_pa, t_sg], [t_sg, t_pa])
                    b.tt(mg[:, n, :], t1[:], sg[:], ALU.add, [t_t1, t_sg], [t_mg])
        for ti in range(4):
            tt = tg * 4 + ti
            rows = slice(tt * 128, (tt + 1) * 128)
            xt, t_xt, k_xt = xtr.next()
            b.dma("sp", xt[:], x_tok[rows, :], writes=[t_xt], key=k_xt)
            z, t_z, _ = zr.next()
            for nh in range(2):
                yb, t_yb = ybk.next()
                b.mm(yb[:, :], [(mg[:, kc, ti * 128:(ti + 1) * 128], wo[:, kc, nh * 512:(nh + 1) * 512])
                                for kc in range(8)], [t_mg, t_w], [t_yb])
                b.stt(z[:, nh * 512:(nh + 1) * 512], xt[:, nh * 512:(nh + 1) * 512], ALPHA, yb[:, :],
                      ALU.mult, ALU.add, [t_xt, t_yb], [t_z, t_yb])
            ot, t_ot, k_ot = outr.next()
            ln_tile(b, z, t_z, ot, t_ot, lng, lnb, t_par, lscr)
            b.dma("sp", x1[rows, :], ot[:], reads=[t_ot], key=k_ot)
    return b.finish()


def bcast128(v):
    return np.ascontiguousarray(np.broadcast_to(np.asarray(v, np.float32)[None, :], (128, v.shape[0])))


def run_D1(x_tok, oa_parts, h_raw, sigz_tok, inp, l):
    nc = build_D1()
    com = {"gm_b": bcast128(inp["m_norm_g"][l]), "lng_b": bcast128(inp["ln_g"][l, 0]),
           "lnb_b": bcast128(inp["ln_b"][l, 0]), "w_gate": np.ascontiguousarray(inp["w_gate"][l]),
           "bg": np.ascontiguousarray(inp["b_gate"][l].reshape(16, 128).T),
           "w_br_a": np.ascontiguousarray(inp["w_br_a"][l]), "w_br_m": np.ascontiguousarray(inp["w_br_m"][l]),
           "w_o": np.ascontiguousarray(inp["w_o"][l]), "ident": np.eye(128, dtype=np.float32),
           "eps": np.full((128, 1), LN_EPS, np.float32)}
    maps = []
    for c in range(NCORES):
        sl = slice(c * TPC, (c + 1) * TPC)
        m = dict(com)
        m["xT"] = np.ascontiguousarray(x_tok[sl].T)
        m["x_tok"] = np.ascontiguousarray(x_tok[sl])
        m["oa_tok"] = np.ascontiguousarray(oa_parts[:, sl].reshape(3, TPC, 520))
        m["hm"] = np.ascontiguousarray(h_raw[sl].reshape(TPC, 512))
        m["sigz"] = np.ascontiguousarray(sigz_tok[sl])
        maps.append(m)
    res = run_bass_kernel_spmd(nc, maps, core_ids=list(range(NCORES))).results
    return np.concatenate([np.asarray(res[c]["x1"]) for c in range(NCORES)], 0)


class LNS:
    def __init__(self, b):
        self.st = b.sb([128, 12], F32)
        self.mv = b.sb([128, 2], F32)
        self.sq = b.sb([128, 1], F32)
        self.rs = b.sb([128, 1], F32)
        self.t_st, self.t_mv, self.t_sq, self.t_rs = Tile(), Tile(), Tile(), Tile()


def ln_tile2(b, z, t_z, out, t_out, lng, lnb, eps, t_par, S_):
    b.P.op("dve", lambda e: e.bn_stats(out=S_.st[:, 0:6], in_=z[:, 0:512]), [t_z], [S_.t_st])
    b.P.op("dve", lambda e: e.bn_stats(out=S_.st[:, 6:12], in_=z[:, 512:1024]), [t_z], [S_.t_st], nowaw=True)
    b.P.op("dve", lambda e: e.bn_aggr(out=S_.mv[:, 0:2], in_=S_.st[:, 0:12]), [S_.t_st], [S_.t_mv])
    b.act(S_.sq[:, 0:1], S_.mv[:, 1:2], AF.Sqrt, [S_.t_mv, t_par], [S_.t_sq], bias=eps[:, 0:1])
    b.P.op("dve", lambda e: e.reciprocal(out=S_.rs[:, 0:1], in_=S_.sq[:, 0:1]), [S_.t_sq], [S_.t_rs])
    b.ts(out[:], z[:], S_.mv[:, 0:1], ALU.subtract, [t_z, S_.t_mv, S_.t_rs], [t_out], s2=S_.rs[:, 0:1], op1=ALU.mult)
    b.tt(out[:], out[:], lng[:], ALU.mult, [t_out, t_par], [t_out])
    b.tt(out[:], out[:], lnb[:], ALU.add, [t_out, t_par], [t_out])


def build_F(n_units, cpu, moe):
    b = Builder()
    FU = cpu * 128
    HT = 1024
    xT = b.din("xT", [D, TPC])
    x_tok = b.din("x_tok", [TPC, D])
    w13 = b.din("w13u", [n_units, D, 2, FU])
    w2 = b.din("w2u", [n_units, FU, D])
    lng_in = b.din("lng_b", [128, D])
    lnb_in = b.din("lnb_b", [128, D])
    eps_in = b.din("eps", [128, 1])
    if moe:
        rw_in = b.din("rw", [D, N_EXP])
        rb_in = b.din("rb_b", [128, N_EXP])
        upe = n_units // N_EXP
    x2 = b.dout("x2", [TPC, D], F32)

    t_par = Tile()
    lng = b.sb([128, D], F32)
    lnb = b.sb([128, D], F32)
    eps = b.sb([128, 1], F32)
    par = [(lng, lng_in), (lnb, lnb_in), (eps, eps_in)]
    if moe:
        rb = b.sb([128, N_EXP], F32)
        par.append((rb, rb_in))
        rw = b.sb([128, 8, N_EXP], F32)
    for dst, src in par:
        b.dma("sp", dst[:], src[:, :], writes=[t_par], key="ldp", nowaw=True)
    if moe:
        b.dma("sp", rw[:], rw_in.rearrange("(kc p) e -> p kc e", p=128), writes=[t_par], key="ldp", nowaw=True)

    xb = b.sb([128, 8, HT], BF)
    t_xb = Tile()
    hT = b.sb([128, cpu, HT], BF)
    t_h = [Tile() for _ in range(HT // 512)]
    w2r = Rot([(b.sb([128, cpu, D], BF), Tile(), f"w2{i}") for i in range(2)])
    w13r = Rot([(b.sb([128, 8, 2, 128], BF), Tile(), f"w13{i}") for i in range(3)])
    acc = b.sb([128, HT // 128, D], F32)
    t_acc = [Tile() for _ in range(HT // 128)]
    sar = Rot([(b.sb([128, 512], F32), Tile()) for _ in range(2)])
    xtr = Rot([(b.sb([128, D], F32), Tile(), f"xt{i}") for i in range(2)])
    otr = Rot([(b.sb([128, D], F32), Tile(), f"ot{i}") for i in range(2)])
    lns = LNS(b)
    pa_r = Rot([(b.ps([128, 512]), Tile()) for _ in range(2)])
    pg_r = Rot([(b.ps([128, 512]), Tile()) for _ in range(2)])
    py_r = Rot([(b.ps([128, 512]), Tile()) for _ in range(3)])
    if moe:
        gates = b.sb([128, TPC // 128, N_EXP], F32)
        t_gate = [Tile() for _ in range(TPC // 128)]
        xfr = Rot([(b.sb([128, 8, 128], F32), Tile(), f"xf{i}") for i in range(2)])
        pl = (b.ps([128, 512]), Tile())
        lg = b.sb([128, N_EXP], F32)
        mx = b.sb([128, 8], F32)
        msk = b.sb([128, N_EXP], F32)
        nm1 = b.sb([128, 1], F32)
        ex = b.sb([128, N_EXP], F32)
        den = b.sb([128, 1], F32)
        rden = b.sb([128, 1], F32)
        t_lg, t_mx, t_msk, t_nm1, t_ex, t_den, t_rden = (Tile() for _ in range(7))

    xTv = xT.rearrange("(kc p) t -> p kc t", p=128)
    w13v = w13.rearrange("u (kc p) j f -> u p kc j f", p=128)
    w2v = w2.rearrange("u (fc p) n -> u p fc n", p=128)
    for half in range(TPC // HT):
        t0 = half * HT
        for kc in range(0, 8, 2):
            b.dma("pool", xb[:, kc:kc + 2, :], xTv[:, kc:kc + 2, t0:t0 + HT], writes=[t_xb], key="ldx",
                  nowaw=(kc > 0))
        if moe:
            for ti in range(HT // 128):
                tt = half * (HT // 128) + ti
                xf, t_xf, k_xf = xfr.next()
                b.dma("sp", xf[:], xTv[:, :, tt * 128:(tt + 1) * 128], writes=[t_xf], key=k_xf)
                b.mm(pl[0][:, 0:N_EXP], [(xf[:, kc, :], rw[:, kc, :]) for kc in range(8)], [t_xf, t_par], [pl[1]])
                b.tt(lg[:], pl[0][:, 0:N_EXP], rb[:], ALU.add, [pl[1], t_par], [t_lg, pl[1]])
                b.P.op("dve", lambda e: e.max(out=mx[:], in_=lg[:]), [t_lg], [t_mx])
                b.ts(msk[:], lg[:], mx[:, 1:2], ALU.is_ge, [t_lg, t_mx], [t_msk])
                b.ts(nm1[:], mx[:, 0:1], -1.0, ALU.mult, [t_mx], [t_nm1])
                b.act(ex[:], lg[:], AF.Exp, [t_lg, t_nm1], [t_ex], bias=nm1[:, 0:1])
                b.tt(ex[:], ex[:], msk[:], ALU.mult, [t_ex, t_msk], [t_ex])
                b.P.op("dve", lambda e: e.reduce_sum(out=den[:], in_=ex[:], axis=mybir.AxisListType.X),
                       [t_ex], [t_den])
                b.P.op("dve", lambda e: e.reciprocal(out=rden[:], in_=den[:]), [t_den], [t_rden])
                b.ts(gates[:, tt, :], ex[:], rden[:, 0:1], ALU.mult, [t_ex, t_rden], [t_gate[tt]])
        for u in range(n_units):
            w2t = None
            for fc in range(cpu):
                wt, t_w, k_w = w13r.next()
                b.dma("pool", wt[:, :, 0, :], w13v[u][:, :, 0, fc * 128:(fc + 1) * 128], writes=[t_w], key=k_w)
                b.dma("pool", wt[:, :, 1, :], w13v[u][:, :, 1, fc * 128:(fc + 1) * 128], writes=[t_w], key=k_w,
                      nowaw=True)
                if fc == min(1, cpu - 1):
                    w2t, t_w2, k_w2 = w2r.next()
                    b.dma("pool", w2t[:], w2v[u], writes=[t_w2], key=k_w2)
                for tg in range(HT // 512):
                    cs = slice(tg * 512, (tg + 1) * 512)
                    pa, t_pa = pa_r.next()
                    pg, t_pg = pg_r.next()
                    b.mm(pa[:, :], [(wt[:, kc, 0, :], xb[:, kc, cs]) for kc in range(8)], [t_w, t_xb], [t_pa])
                    b.mm(pg[:, :], [(wt[:, kc, 1, :], xb[:, kc, cs]) for kc in range(8)], [t_w, t_xb], [t_pg])
                    sa, t_sa = sar.next()
                    b.act(sa[:], pa[:, :], AF.Silu, [t_pa], [t_sa, t_pa])
                    b.tt(hT[:, fc, cs], pg[:, :], sa[:], ALU.mult, [t_pg, t_sa], [t_h[tg], t_pg], nowaw=(fc > 0))
            for ti in range(HT // 128):
                tt = half * (HT // 128) + ti
                for nh in range(2):
                    py, t_py = py_r.next()
                    b.mm(py[:, :], [(hT[:, fc, ti * 128:(ti + 1) * 128], w2t[:, fc, nh * 512:(nh + 1) * 512])
                                    for fc in range(cpu)], [t_h[ti // 4], t_w2], [t_py])
                    dst = acc[:, ti, nh * 512:(nh + 1) * 512]
                    if moe:
                        g = gates[:, tt, u // upe:u // upe + 1]
                        rd = [t_py, t_gate[tt]]
                    else:
                        g = 1.0
                        rd = [t_py]
                    if u == 0:
                        b.ts(dst, py[:, :], g, ALU.mult, rd, [t_acc[ti], t_py], nowaw=(nh > 0))
                    else:
                        b.stt(dst, py[:, :], g, dst, ALU.mult, ALU.add, rd + [t_acc[ti]], [t_acc[ti], t_py])
        for ti in range(HT // 128):
            tt = half * (HT // 128) + ti
            rows = slice(tt * 128, (tt + 1) * 128)
            xt, t_xt, k_xt = xtr.next()
            b.dma("sp", xt[:], x_tok[rows, :], writes=[t_xt], key=k_xt)
            b.stt(xt[:], xt[:], ALPHA, acc[:, ti, :], ALU.mult, ALU.add, [t_xt, t_acc[ti]], [t_xt])
            ot, t_ot, k_ot = otr.next()
            ln_tile2(b, xt, t_xt, ot, t_ot, lng, lnb, eps, t_par, lns)
            b.dma("sp", x2[rows, :], ot[:], reads=[t_ot], key=k_ot)
    return b.finish()


def run_F(x1, inp, l):
    j = l // 2
    com = {"lng_b": bcast128(inp["ln_g"][l, 1]), "lnb_b": bcast128(inp["ln_b"][l, 1]),
           "eps": np.full((128, 1), LN_EPS, np.float32)}
    if l % 2 == 0:
        n_units, cpu, moe = 2, 11, False
        w13 = inp["ffn_w13"][j].reshape(D, 2, n_units, cpu * 128).transpose(2, 0, 1, 3)
        w2 = inp["ffn_w2"][j].reshape(n_units, cpu * 128, D)
    else:
        upe, cpu, moe = 4, 7, True
        n_units = N_EXP * upe
        w13 = inp["exp_w13"][j].reshape(N_EXP, D, 2, upe, cpu * 128).transpose(0, 3, 1, 2, 4).reshape(
            n_units, D, 2, cpu * 128)
        w2 = inp["exp_w2"][j].reshape(n_units, cpu * 128, D)
        com["rw"] = np.ascontiguousarray(inp["router_w"][j])
        com["rb_b"] = bcast128(inp["router_b"][j])
    com["w13u"] = np.ascontiguousarray(w13)
    com["w2u"] = np.ascontiguousarray(w2)
    nc = build_F(n_units, cpu, moe)
    maps = []
    for c in range(NCORES):
        sl = slice(c * TPC, (c + 1) * TPC)
        m = dict(com)
        m["xT"] = np.ascontiguousarray(x1[sl].T)
        m["x_tok"] = np.ascontiguousarray(x1[sl])
        maps.append(m)
    res = run_bass_kernel_spmd(nc, maps, core_ids=list(range(NCORES))).results
    return np.concatenate([np.asarray(res[c]["x2"]) for c in range(NCORES)], 0)


def _tok(res, name):
    return np.ascontiguousarray(np.concatenate([np.asarray(res[c][name]) for c in range(NCORES)], axis=1).T)


def kernel(**inp):
    inp = {k: np.asarray(v) for k, v in inp.items()}
    x = np.ascontiguousarray(inp["x"][0], dtype=np.float32)
    for l in range(DEPTH):
        rp = run_P(make_xT_ext(x), inp, l)
        parts = run_A(_tok(rp, "qaT"), _tok(rp, "kaT"), _tok(rp, "vaT"), inp["rel_bias"])
        h_raw = run_M(_tok(rp, "qmT"), _tok(rp, "kmT"), _tok(rp, "vmT"), _tok(rp, "ipre"), _tok(rp, "logf"))
        x1 = run_D1(x, parts, h_raw, _tok(rp, "sigzT"), inp, l)
        x = run_F(x1, inp, l)
    return x[None].astype(np.float32)
```

```python
import contextlib
import numpy as np
import ml_dtypes
import concourse.bass as bass
import concourse.mybir as mybir
from concourse.bass_utils import run_bass_kernel_spmd

F32 = mybir.dt.float32
BF = mybir.dt.bfloat16
AF = mybir.ActivationFunctionType
ALU = mybir.AluOpType
BF_NP = ml_dtypes.bfloat16

NCORES = 8
S = 16384
D = 1024
TPC = S // NCORES
DEPTH = 2
ALPHA = (2.0 * DEPTH) ** 0.25
LN_EPS = 1e-5
D_FF = 2816
D_FF_E = 3584
N_EXP = 8
TRUNC = None


class Tile:
    __slots__ = ("name", "w", "r")

    def __init__(self, name=""):
        self.name = name
        self.w = None
        self.r = []


class Op:
    __slots__ = ("eng", "fn", "deps", "signal", "sem", "val", "dma", "idx")


class Prog:
    ENGS = ("pe", "act", "dve", "pool", "sp")

    def __init__(self, nc):
        self.nc = nc
        self.ops = []

    def op(self, eng, fn, reads=(), writes=(), dma=None, nowaw=False):
        o = Op()
        o.eng, o.fn, o.dma, o.signal = eng, fn, dma, dma is not None
        o.idx = len(self.ops)
        deps = set()
        for t in reads:
            if t.w is not None:
                deps.add(t.w)
        for t in writes:
            if t.w is not None and not nowaw:
                deps.add(t.w)
            deps.update(t.r)
        deps.discard(o.idx)
        o.deps = deps
        self.ops.append(o)
        for t in reads:
            t.r.append(o.idx)
        for t in writes:
            t.w = o.idx
            t.r = []
        return o

    def emit(self, final_wait_eng="sp"):
        nc = self.nc
        if TRUNC is not None:
            self.ops = self.ops[:TRUNC]
        ops = self.ops
        for o in ops:
            for d in o.deps:
                ops[d].signal = True
        with contextlib.ExitStack() as st:
            esem = {e: st.enter_context(nc.semaphore("s_" + e)) for e in self.ENGS}
            dkeys = sorted({o.dma for o in ops if o.dma is not None})
            dsem = {k: st.enter_context(nc.semaphore("d_" + str(k))) for k in dkeys}
            cnt = {}
            for o in ops:
                if o.dma is not None:
                    cnt[("d", o.dma)] = cnt.get(("d", o.dma), 0) + 16
                    o.sem, o.val = dsem[o.dma], cnt[("d", o.dma)]
                elif o.signal:
                    cnt[o.eng] = cnt.get(o.eng, 0) + 1
                    o.sem, o.val = esem[o.eng], cnt[o.eng]
                else:
                    o.sem, o.val = None, None
            finals = [(dsem[k], cnt[("d", k)]) for k in dkeys]
            block = st.enter_context(nc.Block())
            per_eng = {e: [o for o in ops if o.eng == e] for e in self.ENGS}

            def run(e, engobj):
                seen = {}
                for o in per_eng[e]:
                    need = {}
                    for d in o.deps:
                        do = ops[d]
                        key = id(do.sem)
                        if seen.get(key, 0) >= do.val:
                            continue
                        if key not in need or need[key][1] < do.val:
                            need[key] = (do.sem, do.val)
                    for key, (s, v) in need.items():
                        engobj.wait_ge(s, v)
                        seen[key] = v
                    ins = o.fn(engobj)
                    if o.signal:
                        ins.then_inc(o.sem, 16 if o.dma is not None else 1)
                if e == final_wait_eng:
                    for s, v in finals:
                        engobj.wait_ge(s, v)

            @block.tensor
            def _(eng):
                run("pe", eng)

            @block.scalar
            def _(eng):
                run("act", eng)

            @block.vector
            def _(eng):
                run("dve", eng)

            @block.gpsimd
            def _(eng):
                run("pool", eng)

            @block.sync
            def _(eng):
                run("sp", eng)


class Builder:
    def __init__(self):
        self.nc = bass.Bass("TRN2", target_bir_lowering=False)
        self.P = Prog(self.nc)
        self.st = contextlib.ExitStack()
        self.n = 0

    def din(self, name, shape, dt=F32):
        return self.nc.dram_tensor(name, list(shape), dt, kind="ExternalInput").ap()

    def dout(self, name, shape, dt=F32):
        return self.nc.dram_tensor(name, list(shape), dt, kind="ExternalOutput").ap()

    def sb(self, shape, dt, name=None):
        self.n += 1
        return self.st.enter_context(self.nc.sbuf_tensor(name or f"sb{self.n}", list(shape), dt))

    def ps(self, shape, dt=F32, name=None):
        self.n += 1
        return self.st.enter_context(self.nc.psum_tensor(name or f"ps{self.n}", list(shape), dt))

    def dma(self, q, out, in_, reads=(), writes=(), key="ld", nowaw=False):
        kw = {"max_dma_last_dim": 4096} if q == "pool" else {}
        self.P.op(q, lambda e: e.dma_start(out=out, in_=in_, **kw), reads, writes, dma=key, nowaw=nowaw)

    def mm(self, out, pairs, reads, writes):
        def fn(e):
            n = len(pairs)
            for i, (l, r) in enumerate(pairs):
                ins = e.matmul(out, lhsT=l, rhs=r, start=(i == 0), stop=(i == n - 1))
            return ins
        self.P.op("pe", fn, reads, writes)

    def pe(self, fn, reads, writes):
        self.P.op("pe", fn, reads, writes)

    def act(self, out, in_, func, reads, writes, bias=None, scale=None):
        kw = {}
        if bias is not None:
            kw["bias"] = bias
        if scale is not None:
            kw["scale"] = scale
        self.P.op("act", lambda e: e.activation(out=out, in_=in_, func=func, **kw), reads, writes)

    def ts(self, out, in0, s1, op0, reads, writes, s2=None, op1=None, eng="dve", nowaw=False):
        kw = {}
        if op1 is not None:
            kw["op1"] = op1
        self.P.op(eng, lambda e: e.tensor_scalar(out=out, in0=in0, scalar1=s1, scalar2=s2, op0=op0, **kw),
                  reads, writes, nowaw=nowaw)

    def stt(self, out, in0, scalar, in1, op0, op1, reads, writes):
        self.P.op("dve", lambda e: e.scalar_tensor_tensor(out=out, in0=in0, scalar=scalar, in1=in1,
                                                          op0=op0, op1=op1), reads, writes)

    def tt(self, out, in0, in1, op, reads, writes, eng="dve", nowaw=False):
        self.P.op(eng, lambda e: e.tensor_tensor(out=out, in0=in0, in1=in1, op=op), reads, writes, nowaw=nowaw)

    def copy(self, out, in_, reads, writes, eng="dve"):
        self.P.op(eng, lambda e: e.tensor_copy(out=out, in_=in_), reads, writes)

    def finish(self):
        self.P.emit()
        self.st.close()
        return self.nc


class Rot:
    def __init__(self, items):
        self.items = items
        self.i = 0

    def next(self):
        it = self.items[self.i % len(self.items)]
        self.i += 1
        return it


HALO = 128
TG = 512


def build_P():
    b = Builder()
    nc = b.nc
    W = TPC + HALO
    xT = b.din("xT", [D, W])
    w_in = b.din("w_in", [D, 2560])
    convw = b.din("convw", [128, 16])
    convb = b.din("convb", [128, 4])
    wqk = b.din("wqk", [128, 8 * 128])
    wv = b.din("wv", [128, 4 * 128])
    wif = b.din("wif", [128, 12 * 8])
    bif = b.din("bif", [4, 2])
    o_qa = b.dout("qaT", [512, TPC], BF)
    o_ka = b.dout("kaT", [512, TPC], BF)
    o_va = b.dout("vaT", [512, TPC], BF)
    o_sz = b.dout("sigzT", [512, TPC], F32)
    o_qm = b.dout("qmT", [512, TPC], BF)
    o_km = b.dout("kmT", [512, TPC], BF)
    o_vm = b.dout("vmT", [512, TPC], BF)
    o_ip = b.dout("ipre", [4, TPC], F32)
    o_lf = b.dout("logf", [4, TPC], F32)

    xb = b.sb([128, 8, W], BF)
    t_xb = [Tile() for _ in range(8)]
    xTv = xT.rearrange("(kc p) t -> p kc t", p=128)
    for kc in range(8):
        b.dma("pool", xb[:, kc, :], xTv[:, kc, :], writes=[t_xb[kc]], key="ldx")
    cw = b.sb([128, 16], F32)
    cb = b.sb([128, 4], F32)
    wqk_f = b.sb([128, 1024], BF)
    wv_f = b.sb([128, 512], BF)
    wif_b = b.sb([128, 96], BF)
    bif_s = b.sb([4, 2], F32)
    t_par = Tile()
    b.dma("sp", cw[:], convw[:, :], writes=[t_par], key="ldp", nowaw=True)
    b.dma("sp", cb[:], convb[:, :], writes=[t_par], key="ldp", nowaw=True)
    b.dma("sp", bif_s[:], bif[:, :], writes=[t_par], key="ldp", nowaw=True)
    t_wq = Tile()
    b.dma("pool", wqk_f[:], wqk[:, :], writes=[t_wq], key="ldw2", nowaw=True)
    b.dma("pool", wv_f[:], wv[:, :], writes=[t_wq], key="ldw2", nowaw=True)
    b.dma("pool", wif_b[:], wif[:, :], writes=[t_wq], key="ldw2", nowaw=True)

    wring = Rot([(b.sb([128, 8, 512], BF), Tile(), f"w{i}") for i in range(3)])
    w_inv = w_in.rearrange("(kc p) n -> p kc n", p=128)

    psr = Rot([(b.ps([128, 512]), Tile()) for _ in range(4)])
    st_bf = Rot([(b.sb([128, 512], BF), Tile(), f"sb{i}") for i in range(4)])
    st_f = Rot([(b.sb([128, 512], F32), Tile(), f"sf{i}") for i in range(3)])

    xm = b.sb([128, 4, W], F32)
    t_xm = [[Tile() for _ in range(5)] for _ in range(4)]

    outs_bf = {0: (o_qa, 0.125), 1: (o_ka, None), 2: (o_va, None)}
    for blk in range(5):
        wt, t_w, wkey = wring.next()
        b.dma("pool", wt[:], w_inv[:, :, blk * 512:(blk + 1) * 512], writes=[t_w], key=wkey)
        for cc in range(4):
            groups = [(HALO + g * TG, TG, g + 1) for g in range(4)]
            if blk == 3:
                groups = [(0, HALO, 0)] + groups
            for (c0, n, gi) in groups:
                pt, t_p = psr.next()
                b.mm(pt[:, 0:n], [(wt[:, kc, cc * 128:(cc + 1) * 128], xb[:, kc, c0:c0 + n]) for kc in range(8)],
                     reads=[t_w] + t_xb, writes=[t_p])
                if blk in outs_bf:
                    dst, sc = outs_bf[blk]
                    s, t_s, skey = st_bf.next()
                    b.act(s[:, 0:n], pt[:, 0:n], AF.Copy, [t_p], [t_s], scale=sc)
                    b.dma("sp", dst[cc * 128:(cc + 1) * 128, c0 - HALO:c0 - HALO + n], s[:, 0:n],
                          reads=[t_s], key=skey)
                elif blk == 3:
                    b.copy(xm[:, cc, c0:c0 + n], pt[:, 0:n], [t_p], [t_xm[cc][gi]])
                else:
                    s, t_s, skey = st_f.next()
                    b.act(s[:, 0:n], pt[:, 0:n], AF.Sigmoid, [t_p], [t_s])
                    b.dma("sp", o_sz[cc * 128:(cc + 1) * 128, c0 - HALO:c0 - HALO + n], s[:, 0:n],
                          reads=[t_s], key=skey)

    xc = b.sb([128, 4, TPC], BF)
    xmb = b.sb([128, 4, TPC], BF)
    acc = [(b.sb([128, TPC], F32), Tile()) for _ in range(2)]
    t_xc = [Tile() for _ in range(4)]
    t_xmb = [Tile() for _ in range(4)]
    for ch in range(4):
        a, t_a = acc[ch % 2]
        rd = t_xm[ch] + [t_par]
        b.ts(a[:], xm[:, ch, HALO - 3:HALO - 3 + TPC], cw[:, ch * 4:ch * 4 + 1], ALU.mult, rd, [t_a])
        for j in range(1, 4):
            b.stt(a[:], xm[:, ch, HALO - 3 + j:HALO - 3 + j + TPC], cw[:, ch * 4 + j:ch * 4 + j + 1], a[:],
                  ALU.mult, ALU.add, rd + [t_a], [t_a])
        b.act(xc[:, ch, :], a[:], AF.Silu, [t_a, t_par], [t_xc[ch]], bias=cb[:, ch:ch + 1])
        b.copy(xmb[:, ch, :], xm[:, ch, HALO:HALO + TPC], t_xm[ch], [t_xmb[ch]], eng="pool")

    qkv = [(b.sb([128, 12, TG], BF), Tile()) for _ in range(2)]
    psg = [(b.ps([4, TG]), Tile()) for _ in range(2)]
    g_f = b.sb([4, TG], F32)
    g_e = b.sb([4, TG], F32)
    g_l = b.sb([4, TG], F32)
    g_i = b.sb([4, TG], F32)
    t_gf, t_ge, t_gl, t_gi = Tile(), Tile(), Tile(), Tile()
    SC_M = 128.0 ** -0.5
    for g in range(4):
        c0 = g * TG
        qt, t_q = qkv[g % 2]
        for which in range(3):
            for h in range(4):
                pt, t_p = psr.next()
                if which < 2:
                    lhsT = wqk_f[:, (which * 4 + h) * 128:(which * 4 + h + 1) * 128]
                    rhs = xc[:, h, c0:c0 + TG]
                    rd = [t_wq, t_xc[h]]
                else:
                    lhsT = wv_f[:, h * 128:(h + 1) * 128]
                    rhs = xmb[:, h, c0:c0 + TG]
                    rd = [t_wq, t_xmb[h]]
                b.mm(pt[:, :], [(lhsT, rhs)], rd, [t_p])
                b.act(qt[:, which * 4 + h, :], pt[:, :], AF.Copy, [t_p], [t_q])
                s, t_s, skey = st_bf.next()
                if which == 0:
                    b.ts(s[:, :], qt[:, which * 4 + h, :], SC_M, ALU.mult, [t_q], [t_s])
                else:
                    b.copy(s[:, :], qt[:, which * 4 + h, :], [t_q], [t_s])
                dst = (o_qm, o_km, o_vm)[which]
                b.dma("sp", dst[h * 128:(h + 1) * 128, c0:c0 + TG], s[:, :], reads=[t_s], key=skey)
        pi, t_pi = psg[0]
        pf, t_pf = psg[1]
        b.mm(pi[:, :], [(wif_b[:, j * 8:j * 8 + 4], qt[:, j, :]) for j in range(12)], [t_wq, t_q], [t_pi])
        b.mm(pf[:, :], [(wif_b[:, j * 8 + 4:j * 8 + 8], qt[:, j, :]) for j in range(12)], [t_wq, t_q], [t_pf])
        b.act(g_i[:], pi[:, :], AF.Identity, [t_pi, t_par], [t_gi], bias=bif_s[:, 0:1])
        b.dma("sp", o_ip[:, c0:c0 + TG], g_i[:], reads=[t_gi], key="sgi")
        b.act(g_f[:], pf[:, :], AF.Identity, [t_pf, t_par], [t_gf], bias=bif_s[:, 1:2])
        b.act(g_e[:], g_f[:], AF.Exp, [t_gf], [t_ge], scale=-1.0)
        b.act(g_l[:], g_e[:], AF.Ln, [t_ge], [t_gl], bias=1.0)
        b.ts(g_f[:], g_l[:], -1.0, ALU.mult, [t_gl], [t_gf])
        b.dma("sp", o_lf[:, c0:c0 + TG], g_f[:], reads=[t_gf], key="sgf")
    return b.finish()


def run_P(xT_ext_list, inp, l):
    nc = build_P()
    w_in = np.ascontiguousarray(inp["w_in"][l])
    convw = np.ascontiguousarray(inp["conv_w"][l].T.reshape(4, 128, 4).transpose(1, 0, 2).reshape(128, 16))
    convb = np.ascontiguousarray(inp["conv_b"][l].reshape(4, 128).T)
    wqk = np.ascontiguousarray(inp["w_qk_m"][l].reshape(8, 128, 128).transpose(1, 0, 2).reshape(128, 1024))
    wv = np.ascontiguousarray(inp["w_v_m"][l].transpose(1, 0, 2).reshape(128, 512))
    wif = np.ascontiguousarray(inp["w_if"][l].reshape(12, 128, 8).transpose(1, 0, 2).reshape(128, 96))
    bif = np.ascontiguousarray(inp["b_if"][l].reshape(2, 4).T)
    maps = [{"xT": xT_ext_list[c], "w_in": w_in, "convw": convw, "convb": convb, "wqk": wqk, "wv": wv,
             "wif": wif, "bif": bif} for c in range(NCORES)]
    res = run_bass_kernel_spmd(nc, maps, core_ids=list(range(NCORES)))
    return res.results


def make_xT_ext(x_tok):
    out = []
    for c in range(NCORES):
        a = np.zeros((D, HALO + TPC), np.float32)
        lo = c * TPC - HALO
        if c == 0:
            a[:, HALO:] = x_tok[0:TPC].T
        else:
            a[:, :] = x_tok[lo:lo + HALO + TPC].T
        out.append(a)
    return out


PATTERNS = (1, 4, 16)
NEG = -30000.0


def a_layout():
    lay = []
    koff = 0
    boff = 0
    for d in PATTERNS:
        nq = TPC // d
        nb = nq // 128
        lay.append(dict(d=d, nq=nq, nb=nb, kcls=128 + nq, koff=koff, boff=boff))
        koff += d * (128 + nq)
        boff += d * (nb + 1)
    return lay, koff, boff


def build_A():
    b = Builder()
    lay, KTOT, NBLK = a_layout()
    qh = b.din("qh", [4, 3, 64, 2 * TPC], BF)
    kh = b.din("kh", [4, 64, 2, KTOT], BF)
    vh = b.din("vh", [4, 128, NBLK * 130], BF)
    bmn = b.din("bmn", [12, 128, 512])
    bmf = b.din("bmf", [12, 128, 512])
    ident_in = b.din("ident", [128, 128])
    oa = b.dout("oa", [3, 4, TPC, 130], F32)

    ident = b.sb([128, 128], BF)
    t_c = Tile()
    b.dma("pool", ident[:], ident_in[:, :], writes=[t_c], key="ldc", nowaw=True)
    bn_sb = b.sb([128, 12, 512], BF)
    bf_sb = b.sb([128, 12, 512], BF)
    b.dma("pool", bn_sb[:], bmn.rearrange("n p c -> p n c"), writes=[t_c], key="ldc", nowaw=True)
    b.dma("pool", bf_sb[:], bmf.rearrange("n p c -> p n c"), writes=[t_c], key="ldc", nowaw=True)

    pset = []
    for pi, L in enumerate(lay):
        klen = L["d"] * L["kcls"]
        nblk = L["d"] * (L["nb"] + 1)
        pset.append(dict(q=b.sb([64, 2, TPC], BF), k=b.sb([64, 2, klen], BF), v=b.sb([128, nblk, 130], BF),
                         t=Tile(), key=f"in{pi}", klen=klen, nblk=nblk))
    sbank = Rot([(b.ps([128, 512]), Tile()) for _ in range(3)])
    obank = Rot([(b.ps([128, 512]), Tile()) for _ in range(2)])
    ptr = Rot([(b.sb([128, 512], BF), Tile()) for _ in range(3)])
    ostg = Rot([(b.sb([128, 16, 130], F32), Tile(), f"os{i}") for i in range(2)])

    for hp in range(4):
        for pi, L in enumerate(lay):
            d, nq, nb = L["d"], L["nq"], L["nb"]
            s = pset[pi]
            b.dma("sp", s["q"][:].rearrange("p h t -> p (h t)"), qh[hp, pi], writes=[s["t"]], key=s["key"])
            b.dma("sp", s["k"][:], kh[hp][:, :, L["koff"]:L["koff"] + s["klen"]], writes=[s["t"]], key=s["key"],
                  nowaw=True)
            b.dma("sp", s["v"][:].rearrange("p n c -> p (n c)"),
                  vh[hp][:, L["boff"] * 130:(L["boff"] + s["nblk"]) * 130], writes=[s["t"]], key=s["key"], nowaw=True)
            og, t_og, okey = ostg.next()
            for r in range(d):
                for qb in range(1, nb + 1):
                    qpos = r * nq + (qb - 1) * 128
                    sb_, t_sb = sbank.next()
                    tab = bf_sb if qb == 1 else bn_sb
                    n12 = pi * 4 + hp

                    def fn(e, sb_=sb_, tab=tab, n12=n12, s=s, L=L, r=r, qb=qb, qpos=qpos, pi=pi):
                        e.matmul(sb_[:, :], lhsT=ident[:], rhs=tab[:, n12, :], start=True, stop=False,
                                 skip_group_check=True)
                        for hh in range(2):
                            for w in range(2):
                                kpos = r * L["kcls"] + (qb - 1 + w) * 128
                                c0 = (hh * 2 + w) * 128
                                ins = e.matmul(sb_[:, c0:c0 + 128],
                                               lhsT=s["k"][:, hh, kpos:kpos + 128],
                                               rhs=s["q"][:, hh, qpos:qpos + 128],
                                               start=False, stop=True, skip_group_check=True)
                        return ins
                    b.pe(fn, [t_c, s["t"]], [t_sb])
                    pt, t_pt = ptr.next()
                    b.act(pt[:, :], sb_[:, :], AF.Exp, [t_sb], [t_pt, t_sb])
                    ob, t_ob = obank.next()

                    def fn2(e, ob=ob, pt=pt, s=s, L=L, r=r, qb=qb):
                        for hh in range(2):
                            for w in range(2):
                                blk = r * (L["nb"] + 1) + (qb - 1 + w)
                                c0 = (hh * 2 + w) * 128
                                ins = e.matmul(ob[:, hh * 65:hh * 65 + 65], lhsT=pt[:, c0:c0 + 128],
                                               rhs=s["v"][:, blk, hh * 65:hh * 65 + 65],
                                               start=(w == 0), stop=(w == 1))
                        return ins
                    b.pe(fn2, [t_pt, s["t"]], [t_ob])
                    b.copy(og[:, qpos // 128, :], ob[:, 0:130], [t_ob], [t_og, t_ob])
            b.dma("sp", oa[pi, hp].rearrange("(n q) c -> q n c", q=128), og[:], reads=[t_og], key=okey)
    return b.finish()


def _bucket(dist):
    dist = np.asarray(dist, np.int64)
    dd = np.maximum(dist, 16).astype(np.float32)
    lb = 16 + (np.log(dd / np.float32(16)) / np.float32(np.log(2048.0 / 16.0)) * np.float32(16)).astype(np.int32)
    return np.where(dist < 16, dist, np.minimum(lb, 31))


def a_bias_tables(rel_bias):
    k = np.arange(128)[:, None]
    q = np.arange(128)[None, :]
    tn = np.full((3, 4, 128, 2, 2, 128), NEG, np.float32)
    for pi, d in enumerate(PATTERNS):
        for w in range(2):
            rel = q + 128 - (k + 128 * w)
            valid = (rel >= 0) & (rel <= 128)
            bk = _bucket(np.maximum(rel, 0) * d)
            for h in range(8):
                vals = rel_bias[bk, h]
                tn[pi, h // 2, :, h % 2, w, :] = np.where(valid, vals, NEG)
    tf = tn.copy()
    tf[:, :, :, :, 0, :] = NEG
    return tn.reshape(12, 128, 512), tf.reshape(12, 128, 512)


def a_indices(core):
    lay, KTOT, NBLK = a_layout()
    base = core * TPC
    qidx, kidx = [], []
    for L in lay:
        d, nq = L["d"], L["nq"]
        qi = np.concatenate([base + r + d * np.arange(nq) for r in range(d)])
        ki = np.concatenate([base + r + d * (np.arange(128 + nq) - 128) for r in range(d)])
        qidx.append(qi)
        kidx.append(np.where(ki < 0, -1, ki))
    return qidx, np.concatenate(kidx)


def run_A(qa, ka, va, rel_bias):
    nc = build_A()
    tn, tf = a_bias_tables(rel_bias)
    ident = np.eye(128, dtype=np.float32)
    kz = np.concatenate([ka, np.zeros((1, 512), ka.dtype)], 0)
    vz = np.concatenate([va, np.zeros((1, 512), va.dtype)], 0)
    maps = []
    qidx_all = []
    for c in range(NCORES):
        qidx, kidx = a_indices(c)
        qidx_all.append(qidx)
        qh = np.stack([np.stack([qa[qi][:, hp * 128:(hp + 1) * 128].reshape(-1, 2, 64).transpose(2, 1, 0).reshape(64, -1)
                                 for qi in qidx]) for hp in range(4)])
        kg = kz[kidx]
        kh = np.stack([kg[:, hp * 128:(hp + 1) * 128].reshape(-1, 2, 64).transpose(2, 1, 0) for hp in range(4)])
        vg = vz[kidx].reshape(-1, 128, 4, 2, 64)
        ve = np.concatenate([vg, np.ones(vg.shape[:-1] + (1,), vg.dtype)], -1)
        vh = np.ascontiguousarray(ve.transpose(2, 1, 0, 3, 4)).reshape(4, 128, -1)
        maps.append({"qh": np.ascontiguousarray(qh), "kh": np.ascontiguousarray(kh), "vh": vh,
                     "bmn": tn, "bmf": tf if c == 0 else tn, "ident": ident})
    res = run_bass_kernel_spmd(nc, maps, core_ids=list(range(NCORES))).results
    out = np.zeros((3, S, 4, 130), np.float32)
    for c in range(NCORES):
        o = np.asarray(res[c]["oa"])
        for pi in range(3):
            out[pi, qidx_all[c][pi]] = o[pi].transpose(1, 0, 2)
    return out.reshape(3, S, 8, 65)


NCH = S // 128


def build_M():
    b = Builder()
    qT = b.din("qT", [128, S], BF)
    kT = b.din("kT", [128, S], BF)
    ktok = b.din("ktok", [128, NCH * 128], BF)
    vext = b.din("vext", [128, NCH * 65], BF)
    ipre = b.din("ipre", [128, NCH])
    logf = b.din("logf", [128, NCH])
    U_in = b.din("U", [128, 128])
    NG_in = b.din("NEGM", [128, 128])
    on_in = b.din("ones", [128, 128])
    id_in = b.din("identf", [128, 128])
    ho = b.dout("ho", [128, NCH * 64], F32)

    U = b.sb([128, 128], F32)
    NG = b.sb([128, 128], F32)
    ON = b.sb([128, 128], F32)
    IDF = b.sb([128, 128], F32)
    ip_sb = b.sb([128, NCH], F32)
    lf_sb = b.sb([128, NCH], F32)
    t_c = Tile()
    for dst, src in ((U, U_in), (NG, NG_in), (ON, on_in), (IDF, id_in), (ip_sb, ipre), (lf_sb, logf)):
        b.dma("sp", dst[:], src[:, :], writes=[t_c], key="ldc", nowaw=True)
    q_sb = b.sb([128, S], BF)
    k_sb = b.sb([128, S], BF)
    kt_sb = b.sb([128, NCH, 128], BF)
    v_sb = b.sb([128, NCH, 65], BF)
    NPC = 4
    CPP = NCH // NPC
    t_in = [Tile() for _ in range(NPC)]
    for pc in range(NPC):
        key = f"in{pc}"
        c0, c1 = pc * CPP, (pc + 1) * CPP
        b.dma("sp", q_sb[:, c0 * 128:c1 * 128], qT[:, c0 * 128:c1 * 128], writes=[t_in[pc]], key=key, nowaw=True)
        b.dma("sp", k_sb[:, c0 * 128:c1 * 128], kT[:, c0 * 128:c1 * 128], writes=[t_in[pc]], key=key, nowaw=True)
        b.dma("sp", kt_sb[:, c0:c1, :].rearrange("p n c -> p (n c)"), ktok[:, c0 * 128:c1 * 128],
              writes=[t_in[pc]], key=key, nowaw=True)
        b.dma("sp", v_sb[:, c0:c1, :].rearrange("p n c -> p (n c)"), vext[:, c0 * 65:c1 * 65],
              writes=[t_in[pc]], key=key, nowaw=True)

    ps_b = (b.ps([128, 512]), Tile())
    bcol = b.sb([128, NCH], F32)
    imb = b.sb([128, NCH], F32)
    eb = b.sb([128, NCH], F32)
    t_bcol, t_imb, t_eb = Tile(), Tile(), Tile()
    b.mm(ps_b[0][:, 0:NCH], [(U[:], lf_sb[:])], [t_c], [ps_b[1]])
    b.copy(bcol[:], ps_b[0][:, 0:NCH], [ps_b[1]], [t_bcol, ps_b[1]])
    b.tt(imb[:], ip_sb[:], bcol[:], ALU.subtract, [t_c, t_bcol], [t_imb])
    b.act(eb[:], bcol[:], AF.Exp, [t_bcol], [t_eb])

    C = b.sb([128, 65], F32)
    Cb = b.sb([128, 65], BF)
    t_C, t_Cb = Tile(), Tile()
    b.P.op("dve", lambda e: e.memset(C[:], 0.0), (), [t_C])
    b.P.op("dve", lambda e: e.memset(Cb[:], 0.0), (), [t_Cb])

    def rot(n, shape, dt):
        return Rot([(b.sb(shape, dt), Tile()) for _ in range(n)])
    LUr = rot(2, [128, 128], F32)
    DTr = rot(2, [128, 128], F32)
    dcr = rot(2, [128, 1], F32)
    SWr = rot(2, [128, 128], BF)
    hir = rot(2, [128, 65], F32)
    Htr = rot(2, [128, 65], F32)
    denr = rot(2, [128, 1], F32)
    rdr = rot(2, [128, 1], F32)
    kwr = rot(2, [128, 128], BF)
    Bmr = Rot([(b.ps([128, 512]), Tile()) for _ in range(2)])
    STr = Rot([(b.ps([128, 512]), Tile()) for _ in range(2)])
    Hi = (b.ps([128, 512]), Tile())
    He = (b.ps([128, 512]), Tile())
    dC = (b.ps([128, 512]), Tile())
    GRP = 16
    hout = Rot([(b.sb([128, GRP, 64], F32), Tile(), f"ho{i}") for i in range(2)])
    hcur = None
    for c in range(NCH):
        pc = c // CPP
        tin = t_in[pc]
        cs = slice(c * 128, (c + 1) * 128)
        if c % GRP == 0:
            hcur = hout.next()
        LU, t_LU = LUr.next()
        b.ts(LU[:], U[:], lf_sb[:, c:c + 1], ALU.mult, [t_c], [t_LU])
        Bm, t_Bm = Bmr.next()

        def fnb(e, Bm=Bm, LU=LU):
            e.matmul(Bm[:, 0:128], lhsT=ON[:], rhs=LU[:], start=True, stop=False)
            return e.matmul(Bm[:, 0:128], lhsT=IDF[:], rhs=NG[:], start=False, stop=True)
        b.pe(fnb, [t_c, t_LU], [t_Bm])
        DT, t_DT = DTr.next()
        b.act(DT[:], Bm[:, 0:128], AF.Exp, [t_Bm, t_imb], [t_DT, t_Bm], bias=imb[:, c:c + 1])
        dcol, t_dc = dcr.next()
        b.act(dcol[:], Bm[:, 127:128], AF.Exp, [t_Bm], [t_dc, t_Bm])
        ST, t_ST = STr.next()
        b.mm(ST[:, 0:128], [(k_sb[:, cs], q_sb[:, cs])], [tin], [t_ST])
        SW, t_SW = SWr.next()
        b.tt(SW[:], ST[:, 0:128], DT[:], ALU.mult, [t_ST, t_DT], [t_SW, t_ST])
        b.mm(Hi[0][:, 0:65], [(SW[:], v_sb[:, c, :])], [t_SW, tin], [Hi[1]])
        b.mm(He[0][:, 0:65], [(q_sb[:, cs], Cb[:])], [tin, t_Cb], [He[1]])
        hi, t_hi = hir.next()
        b.act(hi[:], Hi[0][:, 0:65], AF.Copy, [Hi[1]], [t_hi, Hi[1]])
        Ht, t_Ht = Htr.next()
        b.stt(Ht[:], He[0][:, 0:65], eb[:, c:c + 1], hi[:], ALU.mult, ALU.add, [He[1], t_eb, t_hi], [t_Ht, He[1]])
        den, t_den = denr.next()
        b.act(den[:], Ht[:, 64:65], AF.Abs, [t_Ht], [t_den])
        b.ts(den[:], den[:], 1.0, ALU.max, [t_den], [t_den])
        rd, t_rd = rdr.next()
        b.P.op("dve", lambda e, rd=rd, den=den: e.reciprocal(out=rd[:], in_=den[:]), [t_den], [t_rd])
        b.ts(hcur[0][:, c % GRP, :], Ht[:, 0:64], rd[:, 0:1], ALU.mult, [t_Ht, t_rd], [hcur[1]])
        kw, t_kw = kwr.next()
        b.ts(kw[:], kt_sb[:, c, :], DT[:, 127:128], ALU.mult, [tin, t_DT], [t_kw])
        b.mm(dC[0][:, 0:65], [(kw[:], v_sb[:, c, :])], [t_kw, tin], [dC[1]])
        b.stt(C[:], C[:], dcol[:, 0:1], dC[0][:, 0:65], ALU.mult, ALU.add, [t_C, t_dc, dC[1]], [t_C, dC[1]])
        b.act(Cb[:], C[:], AF.Copy, [t_C], [t_Cb])
        if c % GRP == GRP - 1:
            g = c // GRP
            b.dma("sp", ho[:, g * GRP * 64:(g + 1) * GRP * 64], hcur[0][:].rearrange("p n c -> p (n c)"),
                  reads=[hcur[1]], key=hcur[2])
    return b.finish()


def run_M(qm, km, vm, ipre, logf):
    nc = build_M()
    a = np.arange(128)
    U = (a[:, None] <= a[None, :]).astype(np.float32)
    NEGM = np.where(a[:, None] <= a[None, :], 0.0, NEG).astype(np.float32)
    ones = np.ones((128, 128), np.float32)
    identf = np.eye(128, dtype=np.float32)
    maps = []
    for c in range(NCORES):
        h, vh = c // 2, c % 2
        qh = qm[:, h * 128:(h + 1) * 128]
        kh = km[:, h * 128:(h + 1) * 128]
        v = vm[:, h * 128 + vh * 64:h * 128 + vh * 64 + 64].reshape(NCH, 128, 64)
        ve = np.concatenate([v, np.ones((NCH, 128, 1), v.dtype)], -1)
        maps.append({
            "qT": np.ascontiguousarray(qh.T), "kT": np.ascontiguousarray(kh.T),
            "ktok": np.ascontiguousarray(kh.reshape(NCH, 128, 128).transpose(1, 0, 2)).reshape(128, -1),
            "vext": np.ascontiguousarray(ve.transpose(1, 0, 2)).reshape(128, -1),
            "ipre": np.ascontiguousarray(ipre[:, h].reshape(NCH, 128).T),
            "logf": np.ascontiguousarray(logf[:, h].reshape(NCH, 128).T),
            "U": U, "NEGM": NEGM, "ones": ones, "identf": identf})
    res = run_bass_kernel_spmd(nc, maps, core_ids=list(range(NCORES))).results
    out = np.zeros((S, 4, 128), np.float32)
    for c in range(NCORES):
        h, vh = c // 2, c % 2
        o = np.asarray(res[c]["ho"]).reshape(128, NCH, 64)
        out[:, h, vh * 64:(vh + 1) * 64] = o.transpose(1, 0, 2).reshape(S, 64)
    return out


def ln_tile(b, z, t_z, out, t_out, lng, lnb, t_par, scr):
    st, mv, sq, rs = scr
    t_st, t_mv, t_sq, t_rs = Tile(), Tile(), Tile(), Tile()
    b.P.op("dve", lambda e: e.bn_stats(out=st[:, 0:6], in_=z[:, 0:512]), [t_z], [t_st])
    b.P.op("dve", lambda e: e.bn_stats(out=st[:, 6:12], in_=z[:, 512:1024]), [t_z], [t_st], nowaw=True)
    b.P.op("dve", lambda e: e.bn_aggr(out=mv[:, 0:2], in_=st[:, 0:12]), [t_st], [t_mv])
    b.act(sq[:, 0:1], mv[:, 1:2], AF.Sqrt, [t_mv, t_par], [t_sq], bias=EPS_AP[0][:, 0:1])
    b.P.op("dve", lambda e: e.reciprocal(out=rs[:, 0:1], in_=sq[:, 0:1]), [t_sq], [t_rs])
    b.ts(out[:], z[:], mv[:, 0:1], ALU.subtract, [t_z, t_mv, t_rs], [t_out], s2=rs[:, 0:1], op1=ALU.mult)
    b.tt(out[:], out[:], lng[:], ALU.mult, [t_out, t_par], [t_out])
    b.tt(out[:], out[:], lnb[:], ALU.add, [t_out, t_par], [t_out])


EPS_AP = [None]


def build_D1():
    b = Builder()
    xT = b.din("xT", [D, TPC])
    x_tok = b.din("x_tok", [TPC, D])
    oa_tok = b.din("oa_tok", [3, TPC, 520])
    hm_in = b.din("hm", [TPC, 512])
    sz_in = b.din("sigz", [TPC, 512])
    gm_in = b.din("gm_b", [128, 512])
    lng_in = b.din("lng_b", [128, D])
    lnb_in = b.din("lnb_b", [128, D])
    wg_in = b.din("w_gate", [D, 2048])
    bg_in = b.din("bg", [128, 16])
    wbra_in = b.din("w_br_a", [512, D])
    wbrm_in = b.din("w_br_m", [512, D])
    wo_in = b.din("w_o", [D, D])
    id_in = b.din("ident", [128, 128])
    eps_in = b.din("eps", [128, 1])
    x1 = b.dout("x1", [TPC, D], F32)

    t_par = Tile()
    gm = b.sb([128, 512], F32)
    lng = b.sb([128, D], F32)
    lnb = b.sb([128, D], F32)
    bg = b.sb([128, 16], F32)
    eps = b.sb([128, 1], F32)
    EPS_AP[0] = eps
    for dst, src in ((gm, gm_in), (lng, lng_in), (lnb, lnb_in), (bg, bg_in), (eps, eps_in)):
        b.dma("sp", dst[:], src[:, :], writes=[t_par], key="ldp", nowaw=True)
    ident = b.sb([128, 128], BF)
    wbra = b.sb([128, 4, D], BF)
    wbrm = b.sb([128, 4, D], BF)
    wo = b.sb([128, 8, D], BF)
    xb = b.sb([128, 8, TPC], BF)
    t_w = Tile()
    b.dma("pool", ident[:], id_in[:, :], writes=[t_w], key="ldw", nowaw=True)
    t_xb = Tile()
    xTv = xT.rearrange("(kc p) t -> p kc t", p=128)
    for kc in range(8):
        b.dma("pool", xb[:, kc, :], xTv[:, kc, :], writes=[t_xb], key="ldx", nowaw=True)
    b.dma("pool", wbra[:], wbra_in.rearrange("(kc p) n -> p kc n", p=128), writes=[t_w], key="ldw", nowaw=True)
    b.dma("pool", wbrm[:], wbrm_in.rearrange("(kc p) n -> p kc n", p=128), writes=[t_w], key="ldw", nowaw=True)
    wov = wo_in.rearrange("(kc p) n -> p kc n", p=128)
    for kc in range(0, 8, 2):
        b.dma("pool", wo[:, kc:kc + 2, :], wov[:, kc:kc + 2, :], writes=[t_w], key="ldw", nowaw=True)
    wgv = wg_in.rearrange("(kc p) (j n) -> p kc j n", p=128, j=2)
    wgr = Rot([(b.sb([128, 8, 2, 128], BF), Tile(), f"wg{i}") for i in range(2)])

    def rot(n, shape, dt, key=None):
        return Rot([(b.sb(shape, dt), Tile(), (f"{key}{i}" if key else None)) for i in range(n)])
    o3r = rot(1, [128, 3, 520], F32, "o3")
    hmr = rot(2, [128, 512], F32, "hm")
    szr = rot(2, [128, 512], F32, "sz")
    s1 = (b.sb([128, 520], F32), Tile())
    rd = (b.sb([128, 8], F32), Tile())
    yab = (b.sb([128, 512], BF), Tile())
    ymb = (b.sb([128, 512], BF), Tile())
    hn = (b.sb([128, 512], F32), Tile())
    st6 = b.sb([128, 4, 6], F32)
    mv = b.sb([128, 4, 2], F32)
    sq4 = b.sb([128, 4], F32)
    rs4 = b.sb([128, 4], F32)
    t_st6, t_mv, t_sq4, t_rs4 = Tile(), Tile(), Tile(), Tile()
    yTr = Rot([(b.sb([128, 4, TG], BF), b.sb([128, 4, TG], BF), Tile()) for _ in range(2)])
    mgr = Rot([(b.sb([128, 8, TG], BF), Tile()) for _ in range(2)])
    sgr = rot(2, [128, TG], F32)
    t1r = rot(2, [128, TG], F32)
    xtr = rot(2, [128, D], F32, "xt")
    zr = rot(2, [128, D], F32)
    outr = rot(2, [128, D], F32, "ot")
    lscr = (b.sb([128, 12], F32), b.sb([128, 2], F32), b.sb([128, 1], F32), b.sb([128, 1], F32))
    psr = Rot([(b.ps([128, 512]), Tile()) for _ in range(5)])
    tp = (b.ps([128, 8, 128], BF), Tile())
    ybk = Rot([(b.ps([128, 512]), Tile()) for _ in range(2)])

    for tg in range(4):
        yaT, ymT, t_yT = yTr.next()
        for ti in range(4):
            tt = tg * 4 + ti
            rows = slice(tt * 128, (tt + 1) * 128)
            o3, t_o3, k_o3 = o3r.next()
            b.dma("sp", o3[:], oa_tok[:, rows, :].rearrange("n q c -> q n c"), writes=[t_o3], key=k_o3)
            hmt, t_hm, k_hm = hmr.next()
            b.dma("sp", hmt[:], hm_in[rows, :], writes=[t_hm], key=k_hm)
            szt, t_sz, k_sz = szr.next()
            b.dma("sp", szt[:], sz_in[rows, :], writes=[t_sz], key=k_sz)
            b.tt(s1[0][:], o3[:, 0, :], o3[:, 1, :], ALU.add, [t_o3], [s1[1]])
            b.tt(s1[0][:], s1[0][:], o3[:, 2, :], ALU.add, [t_o3, s1[1]], [s1[1]])
            s1v = s1[0][:].rearrange("p (h c) -> p h c", c=65)
            b.P.op("dve", lambda e, s1v=s1v: e.reciprocal(out=rd[0][:], in_=s1v[:, :, 64]), [s1[1]], [rd[1]])
            for h in range(8):
                b.ts(yab[0][:, h * 64:(h + 1) * 64], s1v[:, h, 0:64], rd[0][:, h:h + 1], ALU.mult,
                     [s1[1], rd[1]], [yab[1]])
            for h in range(4):
                b.P.op("dve", lambda e, h=h, hmt=hmt: e.bn_stats(out=st6[:, h, :], in_=hmt[:, h * 128:(h + 1) * 128]),
                       [t_hm], [t_st6])
                b.P.op("dve", lambda e, h=h: e.bn_aggr(out=mv[:, h, :], in_=st6[:, h, :]), [t_st6], [t_mv])
            b.act(sq4[:], mv[:, :, 1], AF.Sqrt, [t_mv, t_par], [t_sq4], bias=eps[:, 0:1])
            b.P.op("dve", lambda e: e.reciprocal(out=rs4[:], in_=sq4[:]), [t_sq4], [t_rs4])
            for h in range(4):
                b.ts(hn[0][:, h * 128:(h + 1) * 128], hmt[:, h * 128:(h + 1) * 128], mv[:, h, 0:1], ALU.subtract,
                     [t_hm, t_mv, t_rs4], [hn[1]], s2=rs4[:, h:h + 1], op1=ALU.mult)
            b.tt(hn[0][:], hn[0][:], gm[:], ALU.mult, [hn[1], t_par], [hn[1]])
            b.tt(ymb[0][:], hn[0][:], szt[:], ALU.mult, [hn[1], t_sz], [ymb[1]])

            def ftp(e):
                for j in range(4):
                    e.transpose(tp[0][:, j, :], yab[0][:, j * 128:(j + 1) * 128], ident[:])
                for j in range(4):
                    ins = e.transpose(tp[0][:, 4 + j, :], ymb[0][:, j * 128:(j + 1) * 128], ident[:])
                return ins
            b.pe(ftp, [yab[1], ymb[1], t_w], [tp[1]])
            cs = slice(ti * 128, (ti + 1) * 128)
            b.copy(yaT[:, :, cs], tp[0][:, 0:4, :], [tp[1]], [t_yT, tp[1]])
            b.act(ymT[:, :, cs], tp[0][:, 4:8, :], AF.Copy, [tp[1]], [t_yT, tp[1]])
        mg, t_mg = mgr.next()
        tcols = slice(tg * TG, (tg + 1) * TG)
        for n in range(8):
            wg, t_wg, k_wg = wgr.next()
            b.dma("pool", wg[:, :, 0, :], wgv[:, :, 0, n * 128:(n + 1) * 128], writes=[t_wg], key=k_wg)
            b.dma("pool", wg[:, :, 1, :], wgv[:, :, 1, n * 128:(n + 1) * 128], writes=[t_wg], key=k_wg, nowaw=True)
            t1, t_t1, _ = t1r.next()
            for j, (wbr, yT) in enumerate(((wbra, yaT), (wbrm, ymT))):
                pa, t_pa = psr.next()
                b.mm(pa[:, :], [(wbr[:, kc, n * 128:(n + 1) * 128], yT[:, kc, :]) for kc in range(4)],
                     [t_w, t_yT], [t_pa])
                ga, t_ga = psr.next()
                b.mm(ga[:, :], [(wg[:, kc, j, :], xb[:, kc, tcols]) for kc in range(8)], [t_wg, t_xb], [t_ga])
                sg, t_sg, _ = sgr.next()
                b.act(sg[:], ga[:, :], AF.Sigmoid, [t_ga, t_par], [t_sg, t_ga], bias=bg[:, j * 8 + n:j * 8 + n + 1])
                if j == 0:
                    b.tt(t1[:], pa[:, :], sg[:], ALU.mult, [t_pa, t_sg], [t_t1, t_pa])
                else:
                    b.tt(sg[:], pa[:, :], sg[:], ALU.mult, [t_pa, t_sg], [t_sg, t_pa])
                    b.tt(mg[:, n, :], t1[:], sg[:], ALU.add, [t_t1, t_sg], [t_mg])
        for ti in range(4):
            tt = tg * 4 + ti
            rows = slice(tt * 128, (tt + 1) * 128)
            xt, t_xt, k_xt = xtr.next()
            b.dma("sp", xt[:], x_tok[rows, :], writes=[t_xt], key=k_xt)
            z, t_z, _ = zr.next()
            for nh in range(2):
                yb, t_yb = ybk.next()
                b.mm(yb[:, :], [(mg[:, kc, ti * 128:(ti + 1) * 128], wo[:, kc, nh * 512:(nh + 1) * 512])
                                for kc in range(8)], [t_mg, t_w], [t_yb])
                b.stt(z[:, nh * 512:(nh + 1) * 512], xt[:, nh * 512:(nh + 1) * 512], ALPHA, yb[:, :],
                      ALU.mult, ALU.add, [t_xt, t_yb], [t_z, t_yb])
            ot, t_ot, k_ot = outr.next()
            ln_tile(b, z, t_z, ot, t_ot, lng, lnb, t_par, lscr)
            b.dma("sp", x1[rows, :], ot[:], reads=[t_ot], key=k_ot)
    return b.finish()


def bcast128(v):
    return np.ascontiguousarray(np.broadcast_to(np.asarray(v, np.float32)[None, :], (128, v.shape[0])))


def run_D1(x_tok, oa_parts, h_raw, sigz_tok, inp, l):
    nc = build_D1()
    com = {"gm_b": bcast128(inp["m_norm_g"][l]), "lng_b": bcast128(inp["ln_g"][l, 0]),
           "lnb_b": bcast128(inp["ln_b"][l, 0]), "w_gate": np.ascontiguousarray(inp["w_gate"][l]),
           "bg": np.ascontiguousarray(inp["b_gate"][l].reshape(16, 128).T),
           "w_br_a": np.ascontiguousarray(inp["w_br_a"][l]), "w_br_m": np.ascontiguousarray(inp["w_br_m"][l]),
           "w_o": np.ascontiguousarray(inp["w_o"][l]), "ident": np.eye(128, dtype=np.float32),
           "eps": np.full((128, 1), LN_EPS, np.float32)}
    maps = []
    for c in range(NCORES):
        sl = slice(c * TPC, (c + 1) * TPC)
        m = dict(com)
        m["xT"] = np.ascontiguousarray(x_tok[sl].T)
        m["x_tok"] = np.ascontiguousarray(x_tok[sl])
        m["oa_tok"] = np.ascontiguousarray(oa_parts[:, sl].reshape(3, TPC, 520))
        m["hm"] = np.ascontiguousarray(h_raw[sl].reshape(TPC, 512))
        m["sigz"] = np.ascontiguousarray(sigz_tok[sl])
        maps.append(m)
    res = run_bass_kernel_spmd(nc, maps, core_ids=list(range(NCORES))).results
    return np.concatenate([np.asarray(res[c]["x1"]) for c in range(NCORES)], 0)


class LNS:
    def __init__(self, b):
        self.st = b.sb([128, 12], F32)
        self.mv = b.sb([128, 2], F32)
        self.sq = b.sb([128, 1], F32)
        self.rs = b.sb([128, 1], F32)
        self.t_st, self.t_mv, self.t_sq, self.t_rs = Tile(), Tile(), Tile(), Tile()


def ln_tile2(b, z, t_z, out, t_out, lng, lnb, eps, t_par, S_):
    b.P.op("dve", lambda e: e.bn_stats(out=S_.st[:, 0:6], in_=z[:, 0:512]), [t_z], [S_.t_st])
    b.P.op("dve", lambda e: e.bn_stats(out=S_.st[:, 6:12], in_=z[:, 512:1024]), [t_z], [S_.t_st], nowaw=True)
    b.P.op("dve", lambda e: e.bn_aggr(out=S_.mv[:, 0:2], in_=S_.st[:, 0:12]), [S_.t_st], [S_.t_mv])
    b.act(S_.sq[:, 0:1], S_.mv[:, 1:2], AF.Sqrt, [S_.t_mv, t_par], [S_.t_sq], bias=eps[:, 0:1])
    b.P.op("dve", lambda e: e.reciprocal(out=S_.rs[:, 0:1], in_=S_.sq[:, 0:1]), [S_.t_sq], [S_.t_rs])
    b.ts(out[:], z[:], S_.mv[:, 0:1], ALU.subtract, [t_z, S_.t_mv, S_.t_rs], [t_out], s2=S_.rs[:, 0:1], op1=ALU.mult)
    b.tt(out[:], out[:], lng[:], ALU.mult, [t_out, t_par], [t_out])
    b.tt(out[:], out[:], lnb[:], ALU.add, [t_out, t_par], [t_out])


def build_F(n_units, cpu, moe):
    b = Builder()
    FU = cpu * 128
    HT = 1024
    xT = b.din("xT", [D, TPC])
    x_tok = b.din("x_tok", [TPC, D])
    w13 = b.din("w13u", [n_units, D, 2, FU])
    w2 = b.din("w2u", [n_units, FU, D])
    lng_in = b.din("lng_b", [128, D])
    lnb_in = b.din("lnb_b", [128, D])
    eps_in = b.din("eps", [128, 1])
    if moe:
        rw_in = b.din("rw", [D, N_EXP])
        rb_in = b.din("rb_b", [128, N_EXP])
        upe = n_units // N_EXP
    x2 = b.dout("x2", [TPC, D], F32)

    t_par = Tile()
    lng = b.sb([128, D], F32)
    lnb = b.sb([128, D], F32)
    eps = b.sb([128, 1], F32)
    par = [(lng, lng_in), (lnb, lnb_in), (eps, eps_in)]
    if moe:
        rb = b.sb([128, N_EXP], F32)
        par.append((rb, rb_in))
        rw = b.sb([128, 8, N_EXP], F32)
    for dst, src in par:
        b.dma("sp", dst[:], src[:, :], writes=[t_par], key="ldp", nowaw=True)
    if moe:
        b.dma("sp", rw[:], rw_in.rearrange("(kc p) e -> p kc e", p=128), writes=[t_par], key="ldp", nowaw=True)

    xb = b.sb([128, 8, HT], BF)
    t_xb = Tile()
    hT = b.sb([128, cpu, HT], BF)
    t_h = [Tile() for _ in range(HT // 512)]
    w2r = Rot([(b.sb([128, cpu, D], BF), Tile(), f"w2{i}") for i in range(2)])
    w13r = Rot([(b.sb([128, 8, 2, 128], BF), Tile(), f"w13{i}") for i in range(3)])
    acc = b.sb([128, HT // 128, D], F32)
    t_acc = [Tile() for _ in range(HT // 128)]
    sar = Rot([(b.sb([128, 512], F32), Tile()) for _ in range(2)])
    xtr = Rot([(b.sb([128, D], F32), Tile(), f"xt{i}") for i in range(2)])
    otr = Rot([(b.sb([128, D], F32), Tile(), f"ot{i}") for i in range(2)])
    lns = LNS(b)
    pa_r = Rot([(b.ps([128, 512]), Tile()) for _ in range(2)])
    pg_r = Rot([(b.ps([128, 512]), Tile()) for _ in range(2)])
    py_r = Rot([(b.ps([128, 512]), Tile()) for _ in range(3)])
    if moe:
        gates = b.sb([128, TPC // 128, N_EXP], F32)
        t_gate = [Tile() for _ in range(TPC // 128)]
        xfr = Rot([(b.sb([128, 8, 128], F32), Tile(), f"xf{i}") for i in range(2)])
        pl = (b.ps([128, 512]), Tile())
        lg = b.sb([128, N_EXP], F32)
        mx = b.sb([128, 8], F32)
        msk = b.sb([128, N_EXP], F32)
        nm1 = b.sb([128, 1], F32)
        ex = b.sb([128, N_EXP], F32)
        den = b.sb([128, 1], F32)
        rden = b.sb([128, 1], F32)
        t_lg, t_mx, t_msk, t_nm1, t_ex, t_den, t_rden = (Tile() for _ in range(7))

    xTv = xT.rearrange("(kc p) t -> p kc t", p=128)
    w13v = w13.rearrange("u (kc p) j f -> u p kc j f", p=128)
    w2v = w2.rearrange("u (fc p) n -> u p fc n", p=128)
    for half in range(TPC // HT):
        t0 = half * HT
        for kc in range(0, 8, 2):
            b.dma("pool", xb[:, kc:kc + 2, :], xTv[:, kc:kc + 2, t0:t0 + HT], writes=[t_xb], key="ldx",
                  nowaw=(kc > 0))
        if moe:
            for ti in range(HT // 128):
                tt = half * (HT // 128) + ti
                xf, t_xf, k_xf = xfr.next()
                b.dma("sp", xf[:], xTv[:, :, tt * 128:(tt + 1) * 128], writes=[t_xf], key=k_xf)
                b.mm(pl[0][:, 0:N_EXP], [(xf[:, kc, :], rw[:, kc, :]) for kc in range(8)], [t_xf, t_par], [pl[1]])
                b.tt(lg[:], pl[0][:, 0:N_EXP], rb[:], ALU.add, [pl[1], t_par], [t_lg, pl[1]])
                b.P.op("dve", lambda e: e.max(out=mx[:], in_=lg[:]), [t_lg], [t_mx])
                b.ts(msk[:], lg[:], mx[:, 1:2], ALU.is_ge, [t_lg, t_mx], [t_msk])
                b.ts(nm1[:], mx[:, 0:1], -1.0, ALU.mult, [t_mx], [t_nm1])
                b.act(ex[:], lg[:], AF.Exp, [t_lg, t_nm1], [t_ex], bias=nm1[:, 0:1])
                b.tt(ex[:], ex[:], msk[:], ALU.mult, [t_ex, t_msk], [t_ex])
                b.P.op("dve", lambda e: e.reduce_sum(out=den[:], in_=ex[:], axis=mybir.AxisListType.X),
                       [t_ex], [t_den])
                b.P.op("dve", lambda e: e.reciprocal(out=rden[:], in_=den[:]), [t_den], [t_rden])
                b.ts(gates[:, tt, :], ex[:], rden[:, 0:1], ALU.mult, [t_ex, t_rden], [t_gate[tt]])
        for u in range(n_units):
            w2t = None
            for fc in range(cpu):
                wt, t_w, k_w = w13r.next()
                b.dma("pool", wt[:, :, 0, :], w13v[u][:, :, 0, fc * 128:(fc + 1) * 128], writes=[t_w], key=k_w)
                b.dma("pool", wt[:, :, 1, :], w13v[u][:, :, 1, fc * 128:(fc + 1) * 128], writes=[t_w], key=k_w,
                      nowaw=True)
                if fc == min(1, cpu - 1):
                    w2t, t_w2, k_w2 = w2r.next()
                    b.dma("pool", w2t[:], w2v[u], writes=[t_w2], key=k_w2)
                for tg in range(HT // 512):
                    cs = slice(tg * 512, (tg + 1) * 512)
                    pa, t_pa = pa_r.next()
                    pg, t_pg = pg_r.next()
                    b.mm(pa[:, :], [(wt[:, kc, 0, :], xb[:, kc, cs]) for kc in range(8)], [t_w, t_xb], [t_pa])
                    b.mm(pg[:, :], [(wt[:, kc, 1, :], xb[:, kc, cs]) for kc in range(8)], [t_w, t_xb], [t_pg])
                    sa, t_sa = sar.next()
                    b.act(sa[:], pa[:, :], AF.Silu, [t_pa], [t_sa, t_pa])
                    b.tt(hT[:, fc, cs], pg[:, :], sa[:], ALU.mult, [t_pg, t_sa], [t_h[tg], t_pg], nowaw=(fc > 0))
            for ti in range(HT // 128):
                tt = half * (HT // 128) + ti
                for nh in range(2):
                    py, t_py = py_r.next()
                    b.mm(py[:, :], [(hT[:, fc, ti * 128:(ti + 1) * 128], w2t[:, fc, nh * 512:(nh + 1) * 512])
                                    for fc in range(cpu)], [t_h[ti // 4], t_w2], [t_py])
                    dst = acc[:, ti, nh * 512:(nh + 1) * 512]
                    if moe:
                        g = gates[:, tt, u // upe:u // upe + 1]
                        rd = [t_py, t_gate[tt]]
                    else:
                        g = 1.0
                        rd = [t_py]
                    if u == 0:
                        b.ts(dst, py[:, :], g, ALU.mult, rd, [t_acc[ti], t_py], nowaw=(nh > 0))
                    else:
                        b.stt(dst, py[:, :], g, dst, ALU.mult, ALU.add, rd + [t_acc[ti]], [t_acc[ti], t_py])
        for ti in range(HT // 128):
            tt = half * (HT // 128) + ti
            rows = slice(tt * 128, (tt + 1) * 128)
            xt, t_xt, k_xt = xtr.next()
            b.dma("sp", xt[:], x_tok[rows, :], writes=[t_xt], key=k_xt)
            b.stt(xt[:], xt[:], ALPHA, acc[:, ti, :], ALU.mult, ALU.add, [t_xt, t_acc[ti]], [t_xt])
            ot, t_ot, k_ot = otr.next()
            ln_tile2(b, xt, t_xt, ot, t_ot, lng, lnb, eps, t_par, lns)
            b.dma("sp", x2[rows, :], ot[:], reads=[t_ot], key=k_ot)
    return b.finish()


def run_F(x1, inp, l):
    j = l // 2
    com = {"lng_b": bcast128(inp["ln_g"][l, 1]), "lnb_b": bcast128(inp["ln_b"][l, 1]),
           "eps": np.full((128, 1), LN_EPS, np.float32)}
    if l % 2 == 0:
        n_units, cpu, moe = 2, 11, False
        w13 = inp["ffn_w13"][j].reshape(D, 2, n_units, cpu * 128).transpose(2, 0, 1, 3)
        w2 = inp["ffn_w2"][j].reshape(n_units, cpu * 128, D)
    else:
        upe, cpu, moe = 4, 7, True
        n_units = N_EXP * upe
        w13 = inp["exp_w13"][j].reshape(N_EXP, D, 2, upe, cpu * 128).transpose(0, 3, 1, 2, 4).reshape(
            n_units, D, 2, cpu * 128)
        w2 = inp["exp_w2"][j].reshape(n_units, cpu * 128, D)
        com["rw"] = np.ascontiguousarray(inp["router_w"][j])
        com["rb_b"] = bcast128(inp["router_b"][j])
    com["w13u"] = np.ascontiguousarray(w13)
    com["w2u"] = np.ascontiguousarray(w2)
    nc = build_F(n_units, cpu, moe)
    maps = []
    for c in range(NCORES):
        sl = slice(c * TPC, (c + 1) * TPC)
        m = dict(com)
        m["xT"] = np.ascontiguousarray(x1[sl].T)
        m["x_tok"] = np.ascontiguousarray(x1[sl])
        maps.append(m)
    res = run_bass_kernel_spmd(nc, maps, core_ids=list(range(NCORES))).results
    return np.concatenate([np.asarray(res[c]["x2"]) for c in range(NCORES)], 0)


CAP = 384
NSB = CAP // 128


def build_FS():
    b = Builder()
    HT = 1024
    NT = HT // 128
    NHALF = TPC // HT
    NFC = D_FF_E // 128
    xT = b.din("xT", [D, TPC])
    x_tok = b.din("x_tok", [TPC, D])
    w13 = b.din("w13e", [N_EXP, D, 2, D_FF_E])
    w2 = b.din("w2e", [N_EXP, D_FF_E, D])
    lng_in = b.din("lng_b", [128, D])
    lnb_in = b.din("lnb_b", [128, D])
    eps_in = b.din("eps", [128, 1])
    rw_in = b.din("rw", [D, N_EXP])
    rb_in = b.din("rb_b", [128, N_EXP])
    ones_in = b.din("ones", [128, 128])
    us_in = b.din("ustrict", [128, 128])
    iota_in = b.din("iota", [128, CAP])
    id_in = b.din("ident", [128, 128])
    x2 = b.dout("x2", [TPC, D], F32)
    cnt_out = b.dout("cnt", [1, NHALF * N_EXP], F32)

    t_par = Tile()
    lng = b.sb([128, D], F32)
    lnb = b.sb([128, D], F32)
    eps = b.sb([128, 1], F32)
    rb = b.sb([128, N_EXP], F32)
    rw = b.sb([128, 8, N_EXP], F32)
    ONES = b.sb([128, 128], F32)
    US = b.sb([128, 128], F32)
    iota = b.sb([128, CAP], F32)
    ident = b.sb([128, 128], BF)
    for dst, src in ((lng, lng_in), (lnb, lnb_in), (eps, eps_in), (rb, rb_in), (ONES, ones_in), (US, us_in),
                     (iota, iota_in)):
        b.dma("sp", dst[:], src[:, :], writes=[t_par], key="ldp", nowaw=True)
    b.dma("sp", rw[:], rw_in.rearrange("(kc p) e -> p kc e", p=128), writes=[t_par], key="ldp", nowaw=True)
    t_id = Tile()
    b.dma("pool", ident[:], id_in[:, :], writes=[t_id], key="ldi")

    xtb = b.sb([128, NT, D], BF)
    t_xtb = Tile()
    Sel = b.sb([128, NT, CAP], BF)
    t_sel = Tile()
    SelT = b.sb([128, NSB, HT], BF)
    t_selT = [Tile() for _ in range(NSB)]
    xg = b.sb([128, 8, CAP], BF)
    t_xg = Tile()
    oe = b.sb([128, NSB, D], BF)
    t_oe = [Tile() for _ in range(NSB)]
    hT = b.sb([128, NFC, CAP], BF)
    t_hT = Tile()
    w13r = Rot([(b.sb([128, 8, 2, 128], BF), Tile(), f"w13{i}") for i in range(3)])
    w2r = Rot([(b.sb([128, NFC, 256], BF), Tile(), f"w2{i}") for i in range(2)])
    acc = b.sb([128, NT, D], F32)
    t_acc = [Tile() for _ in range(NT)]
    sar = Rot([(b.sb([128, CAP], F32), Tile()) for _ in range(2)])
    xtr = Rot([(b.sb([128, D], F32), Tile(), f"xt{i}") for i in range(2)])
    otr = Rot([(b.sb([128, D], F32), Tile(), f"ot{i}") for i in range(2)])
    lns = LNS(b)
    psr = Rot([(b.ps([128, 512]), Tile()) for _ in range(4)])
    pcr = Rot([(b.ps([128, 512]), Tile()) for _ in range(2)])
    tp = (b.ps([128, NT, 128], BF), Tile())
    pm = (b.ps([128, 512]), Tile())
    gates = b.sb([128, TPC // 128, N_EXP], F32)
    mska = b.sb([128, TPC // 128, N_EXP], F32)
    possb = b.sb([128, NT, N_EXP], F32)
    cntsb = b.sb([128, NHALF * N_EXP], F32)
    t_gate = [Tile() for _ in range(TPC // 128)]
    t_mska = [Tile() for _ in range(TPC // 128)]
    t_pos = [Tile() for _ in range(NT)]
    t_cnt = Tile()
    xfr = Rot([(b.sb([128, 8, 128], F32), Tile(), f"xf{i}") for i in range(2)])
    lg = b.sb([128, N_EXP], F32)
    mx = b.sb([128, 8], F32)
    nm1 = b.sb([128, 1], F32)
    ex = b.sb([128, N_EXP], F32)
    den = b.sb([128, 1], F32)
    rden = b.sb([128, 1], F32)
    t_lg, t_mx, t_nm1, t_ex, t_den, t_rden = (Tile() for _ in range(6))

    xTv = xT.rearrange("(kc p) t -> p kc t", p=128)
    w13v = w13.rearrange("e (kc p) j f -> e p kc j f", p=128)
    w2v = w2.rearrange("e (fc p) n -> e p fc n", p=128)
    for half in range(NHALF):
        t0 = half * HT
        xtv = x_tok[t0:t0 + HT, :].rearrange("(n p) d -> p n d", p=128)
        for n0 in range(0, NT, 2):
            b.dma("pool", xtb[:, n0:n0 + 2, :], xtv[:, n0:n0 + 2, :], writes=[t_xtb], key="ldx", nowaw=(n0 > 0))
        for ti in range(NT):
            tt = half * NT + ti
            xf, t_xf, k_xf = xfr.next()
            b.dma("sp", xf[:], xTv[:, :, tt * 128:(tt + 1) * 128], writes=[t_xf], key=k_xf)
            b.mm(pm[0][:, 0:N_EXP], [(xf[:, kc, :], rw[:, kc, :]) for kc in range(8)], [t_xf, t_par], [pm[1]])
            b.tt(lg[:], pm[0][:, 0:N_EXP], rb[:], ALU.add, [pm[1], t_par], [t_lg, pm[1]])
            b.P.op("dve", lambda e: e.max(out=mx[:], in_=lg[:]), [t_lg], [t_mx])
            b.ts(mska[:, tt, :], lg[:], mx[:, 1:2], ALU.is_ge, [t_lg, t_mx], [t_mska[tt]])
            b.ts(nm1[:], mx[:, 0:1], -1.0, ALU.mult, [t_mx], [t_nm1])
            b.act(ex[:], lg[:], AF.Exp, [t_lg, t_nm1], [t_ex], bias=nm1[:, 0:1])
            b.tt(ex[:], ex[:], mska[:, tt, :], ALU.mult, [t_ex, t_mska[tt]], [t_ex])
            b.P.op("dve", lambda e: e.reduce_sum(out=den[:], in_=ex[:], axis=mybir.AxisListType.X), [t_ex], [t_den])
            b.P.op("dve", lambda e: e.reciprocal(out=rden[:], in_=den[:]), [t_den], [t_rden])
            b.ts(gates[:, tt, :], ex[:], rden[:, 0:1], ALU.mult, [t_ex, t_rden], [t_gate[tt]])
        mrd = [t_mska[half * NT + ti] for ti in range(NT)]
        for ti in range(NT):
            pairs = [(ONES[:], mska[:, half * NT + tp_, :]) for tp_ in range(ti)] + [(US[:], mska[:, half * NT + ti, :])]
            b.mm(pm[0][:, 0:N_EXP], pairs, mrd[:ti + 1] + [t_par], [pm[1]])
            b.copy(possb[:, ti, :], pm[0][:, 0:N_EXP], [pm[1]], [t_pos[ti], pm[1]])
        b.mm(pm[0][:, 0:N_EXP], [(ONES[:], mska[:, half * NT + ti, :]) for ti in range(NT)], mrd + [t_par], [pm[1]])
        b.copy(cntsb[:, half * N_EXP:(half + 1) * N_EXP], pm[0][:, 0:N_EXP], [pm[1]], [t_cnt, pm[1]])
        for e_ in range(N_EXP):
            for ti in range(NT):
                tt = half * NT + ti
                b.ts(Sel[:, ti, :], iota[:, :], possb[:, ti, e_:e_ + 1], ALU.is_equal,
                     [t_par, t_pos[ti], t_mska[tt]], [t_sel], s2=mska[:, tt, e_:e_ + 1], op1=ALU.mult, nowaw=(ti > 0))
            for kc in range(8):
                pg_, t_pg_ = psr.next()
                b.mm(pg_[:, 0:CAP], [(xtb[:, ti, kc * 128:(kc + 1) * 128], Sel[:, ti, :]) for ti in range(NT)],
                     [t_xtb, t_sel], [t_pg_])
                b.act(xg[:, kc, :], pg_[:, 0:CAP], AF.Copy, [t_pg_], [t_xg, t_pg_])
            for sb_ in range(NSB):
                def ftp(e, sb_=sb_):
                    for ti in range(NT):
                        ins = e.transpose(tp[0][:, ti, :], Sel[:, ti, sb_ * 128:(sb_ + 1) * 128], ident[:])
                    return ins
                b.pe(ftp, [t_sel, t_id], [tp[1]])
                b.copy(SelT[:, sb_, :], tp[0][:].rearrange("p n c -> p (n c)"), [tp[1]], [t_selT[sb_], tp[1]])
            w2q = {}
            for fc in range(NFC):
                wt, t_w, k_w = w13r.next()
                b.dma("pool", wt[:, :, 0, :], w13v[e_][:, :, 0, fc * 128:(fc + 1) * 128], writes=[t_w], key=k_w)
                b.dma("pool", wt[:, :, 1, :], w13v[e_][:, :, 1, fc * 128:(fc + 1) * 128], writes=[t_w], key=k_w,
                      nowaw=True)
                if fc in (2, 4):
                    q = 0 if fc == 2 else 1
                    w2q[q] = w2r.next()
                    b.dma("pool", w2q[q][0][:], w2v[e_][:, :, q * 256:(q + 1) * 256], writes=[w2q[q][1]], key=w2q[q][2])
                pa, t_pa = psr.next()
                pg, t_pg = psr.next()
                b.mm(pa[:, 0:CAP], [(wt[:, kc, 0, :], xg[:, kc, :]) for kc in range(8)], [t_w, t_xg], [t_pa])
                b.mm(pg[:, 0:CAP], [(wt[:, kc, 1, :], xg[:, kc, :]) for kc in range(8)], [t_w, t_xg], [t_pg])
                sa, t_sa = sar.next()
                b.act(sa[:], pa[:, 0:CAP], AF.Silu, [t_pa], [t_sa, t_pa])
                b.tt(hT[:, fc, :], pg[:, 0:CAP], sa[:], ALU.mult, [t_pg, t_sa], [t_hT, t_pg], nowaw=(fc > 0))
            for q in range(4):
                if q >= 2:
                    w2q[q] = w2r.next()
                    b.dma("pool", w2q[q][0][:], w2v[e_][:, :, q * 256:(q + 1) * 256], writes=[w2q[q][1]], key=w2q[q][2])
                wq, t_wq, _ = w2q[q]
                for sb_ in range(NSB):
                    py, t_py = psr.next()
                    b.mm(py[:, 0:256], [(hT[:, fc, sb_ * 128:(sb_ + 1) * 128], wq[:, fc, :]) for fc in range(NFC)],
                         [t_hT, t_wq], [t_py])
                    b.act(oe[:, sb_, q * 256:(q + 1) * 256], py[:, 0:256], AF.Copy, [t_py], [t_oe[sb_], t_py],
                          )
            for ti in range(NT):
                tt = half * NT + ti
                for nh in range(2):
                    pc, t_pc = pcr.next()
                    b.mm(pc[:, :], [(SelT[:, sb_, ti * 128:(ti + 1) * 128], oe[:, sb_, nh * 512:(nh + 1) * 512])
                                    for sb_ in range(NSB)], t_selT + t_oe, [t_pc])
                    dst = acc[:, ti, nh * 512:(nh + 1) * 512]
                    g = gates[:, tt, e_:e_ + 1]
                    if e_ == 0:
                        b.ts(dst, pc[:, :], g, ALU.mult, [t_pc, t_gate[tt]], [t_acc[ti], t_pc], nowaw=(nh > 0))
                    else:
                        b.stt(dst, pc[:, :], g, dst, ALU.mult, ALU.add, [t_pc, t_gate[tt], t_acc[ti]],
                              [t_acc[ti], t_pc])
        for ti in range(NT):
            tt = half * NT + ti
            rows = slice(tt * 128, (tt + 1) * 128)
            xt, t_xt, k_xt = xtr.next()
            b.dma("sp", xt[:], x_tok[rows, :], writes=[t_xt], key=k_xt)
            b.stt(xt[:], xt[:], ALPHA, acc[:, ti, :], ALU.mult, ALU.add, [t_xt, t_acc[ti]], [t_xt])
            ot, t_ot, k_ot = otr.next()
            ln_tile2(b, xt, t_xt, ot, t_ot, lng, lnb, eps, t_par, lns)
            b.dma("sp", x2[rows, :], ot[:], reads=[t_ot], key=k_ot)
    b.dma("sp", cnt_out[:, :], cntsb[0:1, :], reads=[t_cnt], key="stc")
    return b.finish()


def run_FS(x1, inp, l):
    j = l // 2
    a = np.arange(128)
    com = {"lng_b": bcast128(inp["ln_g"][l, 1]), "lnb_b": bcast128(inp["ln_b"][l, 1]),
           "eps": np.full((128, 1), LN_EPS, np.float32),
           "rw": np.ascontiguousarray(inp["router_w"][j]), "rb_b": bcast128(inp["router_b"][j]),
           "w13e": np.ascontiguousarray(inp["exp_w13"][j]).reshape(N_EXP, D, 2, D_FF_E),
           "w2e": np.ascontiguousarray(inp["exp_w2"][j]),
           "ones": np.ones((128, 128), np.float32),
           "ustrict": (a[:, None] < a[None, :]).astype(np.float32),
           "iota": bcast128(np.arange(CAP, dtype=np.float32)),
           "ident": np.eye(128, dtype=np.float32)}
    nc = build_FS()
    maps = []
    for c in range(NCORES):
        sl = slice(c * TPC, (c + 1) * TPC)
        m = dict(com)
        m["xT"] = np.ascontiguousarray(x1[sl].T)
        m["x_tok"] = np.ascontiguousarray(x1[sl])
        maps.append(m)
    res = run_bass_kernel_spmd(nc, maps, core_ids=list(range(NCORES))).results
    x2 = np.concatenate([np.asarray(res[c]["x2"]) for c in range(NCORES)], 0)
    cnt = max(float(np.asarray(res[c]["cnt"]).max()) for c in range(NCORES))
    return x2, cnt


def _tok(res, name):
    return np.ascontiguousarray(np.concatenate([np.asarray(res[c][name]) for c in range(NCORES)], axis=1).T)


def kernel(**inp):
    inp = {k: np.asarray(v) for k, v in inp.items()}
    x = np.ascontiguousarray(inp["x"][0], dtype=np.float32)
    for l in range(DEPTH):
        rp = run_P(make_xT_ext(x), inp, l)
        parts = run_A(_tok(rp, "qaT"), _tok(rp, "kaT"), _tok(rp, "vaT"), inp["rel_bias"])
        h_raw = run_M(_tok(rp, "qmT"), _tok(rp, "kmT"), _tok(rp, "vmT"), _tok(rp, "ipre"), _tok(rp, "logf"))
        x1 = run_D1(x, parts, h_raw, _tok(rp, "sigzT"), inp, l)
        if l % 2 == 0:
            x = run_F(x1, inp, l)
        else:
            x2, cnt = run_FS(x1, inp, l)
            x = x2 if cnt <= CAP else run_F(x1, inp, l)
    return x[None].astype(np.float32)
```

```python
import contextlib
import numpy as np
import ml_dtypes
import concourse.bass as bass
import concourse.mybir as mybir
from concourse.bass_utils import run_bass_kernel_spmd

F32 = mybir.dt.float32
BF = mybir.dt.bfloat16
AF = mybir.ActivationFunctionType
ALU = mybir.AluOpType
BF_NP = ml_dtypes.bfloat16

NCORES = 8
S = 16384
D = 1024
TPC = S // NCORES
DEPTH = 2
ALPHA = (2.0 * DEPTH) ** 0.25
LN_EPS = 1e-5
D_FF = 2816
D_FF_E = 3584
N_EXP = 8
TRUNC = None


class Tile:
    __slots__ = ("name", "w", "r")

    def __init__(self, name=""):
        self.name = name
        self.w = None
        self.r = []


class Op:
    __slots__ = ("eng", "fn", "deps", "signal", "sem", "val", "dma", "idx")


class Prog:
    ENGS = ("pe", "act", "dve", "pool", "sp")

    def __init__(self, nc):
        self.nc = nc
        self.ops = []

    def op(self, eng, fn, reads=(), writes=(), dma=None, nowaw=False):
        o = Op()
        o.eng, o.fn, o.dma, o.signal = eng, fn, dma, dma is not None
        o.idx = len(self.ops)
        deps = set()
        for t in reads:
            if t.w is not None:
                deps.add(t.w)
        for t in writes:
            if t.w is not None and not nowaw:
                deps.add(t.w)
            deps.update(t.r)
        deps.discard(o.idx)
        o.deps = deps
        self.ops.append(o)
        for t in reads:
            t.r.append(o.idx)
        for t in writes:
            t.w = o.idx
            t.r = []
        return o

    def emit(self, final_wait_eng="sp"):
        nc = self.nc
        if TRUNC is not None:
            self.ops = self.ops[:TRUNC]
        ops = self.ops
        for o in ops:
            for d in o.deps:
                ops[d].signal = True
        with contextlib.ExitStack() as st:
            esem = {e: st.enter_context(nc.semaphore("s_" + e)) for e in self.ENGS}
            dkeys = sorted({o.dma for o in ops if o.dma is not None})
            dsem = {k: st.enter_context(nc.semaphore("d_" + str(k))) for k in dkeys}
            cnt = {}
            for o in ops:
                if o.dma is not None:
                    cnt[("d", o.dma)] = cnt.get(("d", o.dma), 0) + 16
                    o.sem, o.val = dsem[o.dma], cnt[("d", o.dma)]
                elif o.signal:
                    cnt[o.eng] = cnt.get(o.eng, 0) + 1
                    o.sem, o.val = esem[o.eng], cnt[o.eng]
                else:
                    o.sem, o.val = None, None
            finals = [(dsem[k], cnt[("d", k)]) for k in dkeys]
            block = st.enter_context(nc.Block())
            per_eng = {e: [o for o in ops if o.eng == e] for e in self.ENGS}

            def run(e, engobj):
                seen = {}
                for o in per_eng[e]:
                    need = {}
                    for d in o.deps:
                        do = ops[d]
                        key = id(do.sem)
                        if seen.get(key, 0) >= do.val:
                            continue
                        if key not in need or need[key][1] < do.val:
                            need[key] = (do.sem, do.val)
                    for key, (s, v) in need.items():
                        engobj.wait_ge(s, v)
                        seen[key] = v
                    ins = o.fn(engobj)
                    if o.signal:
                        ins.then_inc(o.sem, 16 if o.dma is not None else 1)
                if e == final_wait_eng:
                    for s, v in finals:
                        engobj.wait_ge(s, v)

            @block.tensor
            def _(eng):
                run("pe", eng)

            @block.scalar
            def _(eng):
                run("act", eng)

            @block.vector
            def _(eng):
                run("dve", eng)

            @block.gpsimd
            def _(eng):
                run("pool", eng)

            @block.sync
            def _(eng):
                run("sp", eng)


class Builder:
    def __init__(self):
        self.nc = bass.Bass("TRN2", target_bir_lowering=False)
        self.P = Prog(self.nc)
        self.st = contextlib.ExitStack()
        self.n = 0

    def din(self, name, shape, dt=F32):
        return self.nc.dram_tensor(name, list(shape), dt, kind="ExternalInput").ap()

    def dout(self, name, shape, dt=F32):
        return self.nc.dram_tensor(name, list(shape), dt, kind="ExternalOutput").ap()

    def sb(self, shape, dt, name=None):
        self.n += 1
        return self.st.enter_context(self.nc.sbuf_tensor(name or f"sb{self.n}", list(shape), dt))

    def ps(self, shape, dt=F32, name=None):
        self.n += 1
        return self.st.enter_context(self.nc.psum_tensor(name or f"ps{self.n}", list(shape), dt))

    def dma(self, q, out, in_, reads=(), writes=(), key="ld", nowaw=False):
        kw = {"max_dma_last_dim": 4096} if q == "pool" else {}
        self.P.op(q, lambda e: e.dma_start(out=out, in_=in_, **kw), reads, writes, dma=key, nowaw=nowaw)

    def mm(self, out, pairs, reads, writes):
        def fn(e):
            n = len(pairs)
            for i, (l, r) in enumerate(pairs):
                ins = e.matmul(out, lhsT=l, rhs=r, start=(i == 0), stop=(i == n - 1))
            return ins
        self.P.op("pe", fn, reads, writes)

    def pe(self, fn, reads, writes):
        self.P.op("pe", fn, reads, writes)

    def act(self, out, in_, func, reads, writes, bias=None, scale=None):
        kw = {}
        if bias is not None:
            kw["bias"] = bias
        if scale is not None:
            kw["scale"] = scale
        self.P.op("act", lambda e: e.activation(out=out, in_=in_, func=func, **kw), reads, writes)

    def ts(self, out, in0, s1, op0, reads, writes, s2=None, op1=None, eng="dve", nowaw=False):
        kw = {}
        if op1 is not None:
            kw["op1"] = op1
        self.P.op(eng, lambda e: e.tensor_scalar(out=out, in0=in0, scalar1=s1, scalar2=s2, op0=op0, **kw),
                  reads, writes, nowaw=nowaw)

    def stt(self, out, in0, scalar, in1, op0, op1, reads, writes):
        self.P.op("dve", lambda e: e.scalar_tensor_tensor(out=out, in0=in0, scalar=scalar, in1=in1,
                                                          op0=op0, op1=op1), reads, writes)

    def tt(self, out, in0, in1, op, reads, writes, eng="dve", nowaw=False):
        self.P.op(eng, lambda e: e.tensor_tensor(out=out, in0=in0, in1=in1, op=op), reads, writes, nowaw=nowaw)

    def copy(self, out, in_, reads, writes, eng="dve"):
        self.P.op(eng, lambda e: e.tensor_copy(out=out, in_=in_), reads, writes)

    def finish(self):
        self.P.emit()
        self.st.close()
        return self.nc


class Rot:
    def __init__(self, items):
        self.items = items
        self.i = 0

    def next(self):
        it = self.items[self.i % len(self.items)]
        self.i += 1
        return it


HALO = 128
TG = 512


def build_P():
    b = Builder()
    nc = b.nc
    W = TPC + HALO
    xT = b.din("xT", [D, W])
    w_in = b.din("w_in", [D, 2560])
    convw = b.din("convw", [128, 16])
    convb = b.din("convb", [128, 4])
    wqk = b.din("wqk", [128, 8 * 128])
    wv = b.din("wv", [128, 4 * 128])
    wif = b.din("wif", [128, 12 * 8])
    bif = b.din("bif", [4, 2])
    o_qa = b.dout("qaT", [512, TPC], BF)
    o_ka = b.dout("kaT", [512, TPC], BF)
    o_va = b.dout("vaT", [512, TPC], BF)
    o_sz = b.dout("sigzT", [512, TPC], F32)
    o_qm = b.dout("qmT", [512, TPC], BF)
    o_km = b.dout("kmT", [512, TPC], BF)
    o_vm = b.dout("vmT", [512, TPC], BF)
    o_ip = b.dout("ipre", [4, TPC], F32)
    o_lf = b.dout("logf", [4, TPC], F32)

    xb = b.sb([128, 8, W], BF)
    t_xb = [Tile() for _ in range(8)]
    xTv = xT.rearrange("(kc p) t -> p kc t", p=128)
    for kc in range(8):
        b.dma("pool", xb[:, kc, :], xTv[:, kc, :], writes=[t_xb[kc]], key="ldx")
    cw = b.sb([128, 16], F32)
    cb = b.sb([128, 4], F32)
    wqk_f = b.sb([128, 1024], BF)
    wv_f = b.sb([128, 512], BF)
    wif_b = b.sb([128, 96], BF)
    bif_s = b.sb([4, 2], F32)
    t_par = Tile()
    b.dma("sp", cw[:], convw[:, :], writes=[t_par], key="ldp", nowaw=True)
    b.dma("sp", cb[:], convb[:, :], writes=[t_par], key="ldp", nowaw=True)
    b.dma("sp", bif_s[:], bif[:, :], writes=[t_par], key="ldp", nowaw=True)
    t_wq = Tile()
    b.dma("pool", wqk_f[:], wqk[:, :], writes=[t_wq], key="ldw2", nowaw=True)
    b.dma("pool", wv_f[:], wv[:, :], writes=[t_wq], key="ldw2", nowaw=True)
    b.dma("pool", wif_b[:], wif[:, :], writes=[t_wq], key="ldw2", nowaw=True)

    wring = Rot([(b.sb([128, 8, 512], BF), Tile(), f"w{i}") for i in range(3)])
    w_inv = w_in.rearrange("(kc p) n -> p kc n", p=128)

    psr = Rot([(b.ps([128, 512]), Tile()) for _ in range(4)])
    st_bf = Rot([(b.sb([128, 512], BF), Tile(), f"sb{i}") for i in range(4)])
    st_f = Rot([(b.sb([128, 512], F32), Tile(), f"sf{i}") for i in range(3)])

    xm = b.sb([128, 4, W], F32)
    t_xm = [[Tile() for _ in range(5)] for _ in range(4)]

    outs_bf = {0: (o_qa, 0.125), 1: (o_ka, None), 2: (o_va, None)}
    for blk in range(5):
        wt, t_w, wkey = wring.next()
        b.dma("pool", wt[:], w_inv[:, :, blk * 512:(blk + 1) * 512], writes=[t_w], key=wkey)
        for cc in range(4):
            groups = [(HALO + g * TG, TG, g + 1) for g in range(4)]
            if blk == 3:
                groups = [(0, HALO, 0)] + groups
            for (c0, n, gi) in groups:
                pt, t_p = psr.next()
                b.mm(pt[:, 0:n], [(wt[:, kc, cc * 128:(cc + 1) * 128], xb[:, kc, c0:c0 + n]) for kc in range(8)],
                     reads=[t_w] + t_xb, writes=[t_p])
                if blk in outs_bf:
                    dst, sc = outs_bf[blk]
                    s, t_s, skey = st_bf.next()
                    b.act(s[:, 0:n], pt[:, 0:n], AF.Copy, [t_p], [t_s], scale=sc)
                    b.dma("sp", dst[cc * 128:(cc + 1) * 128, c0 - HALO:c0 - HALO + n], s[:, 0:n],
                          reads=[t_s], key=skey)
                elif blk == 3:
                    b.copy(xm[:, cc, c0:c0 + n], pt[:, 0:n], [t_p], [t_xm[cc][gi]])
                else:
                    s, t_s, skey = st_f.next()
                    b.act(s[:, 0:n], pt[:, 0:n], AF.Sigmoid, [t_p], [t_s])
                    b.dma("sp", o_sz[cc * 128:(cc + 1) * 128, c0 - HALO:c0 - HALO + n], s[:, 0:n],
                          reads=[t_s], key=skey)

    xc = b.sb([128, 4, TPC], BF)
    xmb = b.sb([128, 4, TPC], BF)
    acc = [(b.sb([128, TPC], F32), Tile()) for _ in range(2)]
    t_xc = [Tile() for _ in range(4)]
    t_xmb = [Tile() for _ in range(4)]
    for ch in range(4):
        a, t_a = acc[ch % 2]
        rd = t_xm[ch] + [t_par]
        b.ts(a[:], xm[:, ch, HALO - 3:HALO - 3 + TPC], cw[:, ch * 4:ch * 4 + 1], ALU.mult, rd, [t_a])
        for j in range(1, 4):
            b.stt(a[:], xm[:, ch, HALO - 3 + j:HALO - 3 + j + TPC], cw[:, ch * 4 + j:ch * 4 + j + 1], a[:],
                  ALU.mult, ALU.add, rd + [t_a], [t_a])
        b.act(xc[:, ch, :], a[:], AF.Silu, [t_a, t_par], [t_xc[ch]], bias=cb[:, ch:ch + 1])
        b.copy(xmb[:, ch, :], xm[:, ch, HALO:HALO + TPC], t_xm[ch], [t_xmb[ch]], eng="pool")

    qkv = [(b.sb([128, 12, TG], BF), Tile()) for _ in range(2)]
    psg = [(b.ps([4, TG]), Tile()) for _ in range(2)]
    g_f = b.sb([4, TG], F32)
    g_e = b.sb([4, TG], F32)
    g_l = b.sb([4, TG], F32)
    g_i = b.sb([4, TG], F32)
    t_gf, t_ge, t_gl, t_gi = Tile(), Tile(), Tile(), Tile()
    SC_M = 128.0 ** -0.5
    for g in range(4):
        c0 = g * TG
        qt, t_q = qkv[g % 2]
        for which in range(3):
            for h in range(4):
                pt, t_p = psr.next()
                if which < 2:
                    lhsT = wqk_f[:, (which * 4 + h) * 128:(which * 4 + h + 1) * 128]
                    rhs = xc[:, h, c0:c0 + TG]
                    rd = [t_wq, t_xc[h]]
                else:
                    lhsT = wv_f[:, h * 128:(h + 1) * 128]
                    rhs = xmb[:, h, c0:c0 + TG]
                    rd = [t_wq, t_xmb[h]]
                b.mm(pt[:, :], [(lhsT, rhs)], rd, [t_p])
                b.act(qt[:, which * 4 + h, :], pt[:, :], AF.Copy, [t_p], [t_q])
                s, t_s, skey = st_bf.next()
                if which == 0:
                    b.ts(s[:, :], qt[:, which * 4 + h, :], SC_M, ALU.mult, [t_q], [t_s])
                else:
                    b.copy(s[:, :], qt[:, which * 4 + h, :], [t_q], [t_s])
                dst = (o_qm, o_km, o_vm)[which]
                b.dma("sp", dst[h * 128:(h + 1) * 128, c0:c0 + TG], s[:, :], reads=[t_s], key=skey)
        pi, t_pi = psg[0]
        pf, t_pf = psg[1]
        b.mm(pi[:, :], [(wif_b[:, j * 8:j * 8 + 4], qt[:, j, :]) for j in range(12)], [t_wq, t_q], [t_pi])
        b.mm(pf[:, :], [(wif_b[:, j * 8 + 4:j * 8 + 8], qt[:, j, :]) for j in range(12)], [t_wq, t_q], [t_pf])
        b.act(g_i[:], pi[:, :], AF.Identity, [t_pi, t_par], [t_gi], bias=bif_s[:, 0:1])
        b.dma("sp", o_ip[:, c0:c0 + TG], g_i[:], reads=[t_gi], key="sgi")
        b.act(g_f[:], pf[:, :], AF.Identity, [t_pf, t_par], [t_gf], bias=bif_s[:, 1:2])
        b.act(g_e[:], g_f[:], AF.Exp, [t_gf], [t_ge], scale=-1.0)
        b.act(g_l[:], g_e[:], AF.Ln, [t_ge], [t_gl], bias=1.0)
        b.ts(g_f[:], g_l[:], -1.0, ALU.mult, [t_gl], [t_gf])
        b.dma("sp", o_lf[:, c0:c0 + TG], g_f[:], reads=[t_gf], key="sgf")
    return b.finish()


def run_P(xT_ext_list, inp, l):
    nc = build_P()
    w_in = np.ascontiguousarray(inp["w_in"][l])
    convw = np.ascontiguousarray(inp["conv_w"][l].T.reshape(4, 128, 4).transpose(1, 0, 2).reshape(128, 16))
    convb = np.ascontiguousarray(inp["conv_b"][l].reshape(4, 128).T)
    wqk = np.ascontiguousarray(inp["w_qk_m"][l].reshape(8, 128, 128).transpose(1, 0, 2).reshape(128, 1024))
    wv = np.ascontiguousarray(inp["w_v_m"][l].transpose(1, 0, 2).reshape(128, 512))
    wif = np.ascontiguousarray(inp["w_if"][l].reshape(12, 128, 8).transpose(1, 0, 2).reshape(128, 96))
    bif = np.ascontiguousarray(inp["b_if"][l].reshape(2, 4).T)
    maps = [{"xT": xT_ext_list[c], "w_in": w_in, "convw": convw, "convb": convb, "wqk": wqk, "wv": wv,
             "wif": wif, "bif": bif} for c in range(NCORES)]
    res = run_bass_kernel_spmd(nc, maps, core_ids=list(range(NCORES)))
    return res.results


def make_xT_ext(x_tok):
    out = []
    for c in range(NCORES):
        a = np.zeros((D, HALO + TPC), np.float32)
        lo = c * TPC - HALO
        if c == 0:
            a[:, HALO:] = x_tok[0:TPC].T
        else:
            a[:, :] = x_tok[lo:lo + HALO + TPC].T
        out.append(a)
    return out


PATTERNS = (1, 4, 16)
NEG = -30000.0


def a_layout():
    lay = []
    koff = 0
    boff = 0
    for d in PATTERNS:
        nq = TPC // d
        nb = nq // 128
        lay.append(dict(d=d, nq=nq, nb=nb, kcls=128 + nq, koff=koff, boff=boff))
        koff += d * (128 + nq)
        boff += d * (nb + 1)
    return lay, koff, boff


def build_A():
    b = Builder()
    lay, KTOT, NBLK = a_layout()
    qh = b.din("qh", [4, 3, 64, 2 * TPC], BF)
    kh = b.din("kh", [4, 64, 2, KTOT], BF)
    vh = b.din("vh", [4, 128, NBLK * 130], BF)
    bmn = b.din("bmn", [12, 128, 512])
    bmf = b.din("bmf", [12, 128, 512])
    ident_in = b.din("ident", [128, 128])
    oa = b.dout("oa", [3, 4, TPC, 130], F32)

    ident = b.sb([128, 128], BF)
    t_c = Tile()
    b.dma("pool", ident[:], ident_in[:, :], writes=[t_c], key="ldc", nowaw=True)
    bn_sb = b.sb([128, 12, 512], BF)
    bf_sb = b.sb([128, 12, 512], BF)
    b.dma("pool", bn_sb[:], bmn.rearrange("n p c -> p n c"), writes=[t_c], key="ldc", nowaw=True)
    b.dma("pool", bf_sb[:], bmf.rearrange("n p c -> p n c"), writes=[t_c], key="ldc", nowaw=True)

    pset = []
    for pi, L in enumerate(lay):
        klen = L["d"] * L["kcls"]
        nblk = L["d"] * (L["nb"] + 1)
        pset.append(dict(q=b.sb([64, 2, TPC], BF), k=b.sb([64, 2, klen], BF), v=b.sb([128, nblk, 130], BF),
                         t=Tile(), key=f"in{pi}", klen=klen, nblk=nblk))
    sbank = Rot([(b.ps([128, 512]), Tile()) for _ in range(3)])
    obank = Rot([(b.ps([128, 512]), Tile()) for _ in range(2)])
    ptr = Rot([(b.sb([128, 512], BF), Tile()) for _ in range(3)])
    ostg = Rot([(b.sb([128, 16, 130], F32), Tile(), f"os{i}") for i in range(2)])

    for hp in range(4):
        for pi, L in enumerate(lay):
            d, nq, nb = L["d"], L["nq"], L["nb"]
            s = pset[pi]
            b.dma("sp", s["q"][:].rearrange("p h t -> p (h t)"), qh[hp, pi], writes=[s["t"]], key=s["key"])
            b.dma("sp", s["k"][:], kh[hp][:, :, L["koff"]:L["koff"] + s["klen"]], writes=[s["t"]], key=s["key"],
                  nowaw=True)
            b.dma("sp", s["v"][:].rearrange("p n c -> p (n c)"),
                  vh[hp][:, L["boff"] * 130:(L["boff"] + s["nblk"]) * 130], writes=[s["t"]], key=s["key"], nowaw=True)
            og, t_og, okey = ostg.next()
            for r in range(d):
                for qb in range(1, nb + 1):
                    qpos = r * nq + (qb - 1) * 128
                    sb_, t_sb = sbank.next()
                    tab = bf_sb if qb == 1 else bn_sb
                    n12 = pi * 4 + hp

                    def fn(e, sb_=sb_, tab=tab, n12=n12, s=s, L=L, r=r, qb=qb, qpos=qpos, pi=pi):
                        e.matmul(sb_[:, :], lhsT=ident[:], rhs=tab[:, n12, :], start=True, stop=False,
                                 skip_group_check=True)
                        for hh in range(2):
                            for w in range(2):
                                kpos = r * L["kcls"] + (qb - 1 + w) * 128
                                c0 = (hh * 2 + w) * 128
                                ins = e.matmul(sb_[:, c0:c0 + 128],
                                               lhsT=s["k"][:, hh, kpos:kpos + 128],
                                               rhs=s["q"][:, hh, qpos:qpos + 128],
                                               start=False, stop=True, skip_group_check=True)
                        return ins
                    b.pe(fn, [t_c, s["t"]], [t_sb])
                    pt, t_pt = ptr.next()
                    b.act(pt[:, :], sb_[:, :], AF.Exp, [t_sb], [t_pt, t_sb])
                    ob, t_ob = obank.next()

                    def fn2(e, ob=ob, pt=pt, s=s, L=L, r=r, qb=qb):
                        for hh in range(2):
                            for w in range(2):
                                blk = r * (L["nb"] + 1) + (qb - 1 + w)
                                c0 = (hh * 2 + w) * 128
                                ins = e.matmul(ob[:, hh * 65:hh * 65 + 65], lhsT=pt[:, c0:c0 + 128],
                                               rhs=s["v"][:, blk, hh * 65:hh * 65 + 65],
                                               start=(w == 0), stop=(w == 1))
                        return ins
                    b.pe(fn2, [t_pt, s["t"]], [t_ob])
                    b.copy(og[:, qpos // 128, :], ob[:, 0:130], [t_ob], [t_og, t_ob])
            b.dma("sp", oa[pi, hp].rearrange("(n q) c -> q n c", q=128), og[:], reads=[t_og], key=okey)
    return b.finish()


def _bucket(dist):
    dist = np.asarray(dist, np.int64)
    dd = np.maximum(dist, 16).astype(np.float32)
    lb = 16 + (np.log(dd / np.float32(16)) / np.float32(np.log(2048.0 / 16.0)) * np.float32(16)).astype(np.int32)
    return np.where(dist < 16, dist, np.minimum(lb, 31))


def a_bias_tables(rel_bias):
    k = np.arange(128)[:, None]
    q = np.arange(128)[None, :]
    tn = np.full((3, 4, 128, 2, 2, 128), NEG, np.float32)
    for pi, d in enumerate(PATTERNS):
        for w in range(2):
            rel = q + 128 - (k + 128 * w)
            valid = (rel >= 0) & (rel <= 128)
            bk = _bucket(np.maximum(rel, 0) * d)
            for h in range(8):
                vals = rel_bias[bk, h]
                tn[pi, h // 2, :, h % 2, w, :] = np.where(valid, vals, NEG)
    tf = tn.copy()
    tf[:, :, :, :, 0, :] = NEG
    return tn.reshape(12, 128, 512), tf.reshape(12, 128, 512)


def a_indices(core):
    lay, KTOT, NBLK = a_layout()
    base = core * TPC
    qidx, kidx = [], []
    for L in lay:
        d, nq = L["d"], L["nq"]
        qi = np.concatenate([base + r + d * np.arange(nq) for r in range(d)])
        ki = np.concatenate([base + r + d * (np.arange(128 + nq) - 128) for r in range(d)])
        qidx.append(qi)
        kidx.append(np.where(ki < 0, -1, ki))
    return qidx, np.concatenate(kidx)


def run_A(qa, ka, va, rel_bias):
    nc = build_A()
    tn, tf = a_bias_tables(rel_bias)
    ident = np.eye(128, dtype=np.float32)
    kz = np.concatenate([ka, np.zeros((1, 512), ka.dtype)], 0)
    vz = np.concatenate([va, np.zeros((1, 512), va.dtype)], 0)
    maps = []
    qidx_all = []
    for c in range(NCORES):
        qidx, kidx = a_indices(c)
        qidx_all.append(qidx)
        qh = np.stack([np.stack([qa[qi][:, hp * 128:(hp + 1) * 128].reshape(-1, 2, 64).transpose(2, 1, 0).reshape(64, -1)
                                 for qi in qidx]) for hp in range(4)])
        kg = kz[kidx]
        kh = np.stack([kg[:, hp * 128:(hp + 1) * 128].reshape(-1, 2, 64).transpose(2, 1, 0) for hp in range(4)])
        vg = vz[kidx].reshape(-1, 128, 4, 2, 64)
        ve = np.concatenate([vg, np.ones(vg.shape[:-1] + (1,), vg.dtype)], -1)
        vh = np.ascontiguousarray(ve.transpose(2, 1, 0, 3, 4)).reshape(4, 128, -1)
        maps.append({"qh": np.ascontiguousarray(qh), "kh": np.ascontiguousarray(kh), "vh": vh,
                     "bmn": tn, "bmf": tf if c == 0 else tn, "ident": ident})
    res = run_bass_kernel_spmd(nc, maps, core_ids=list(range(NCORES))).results
    out = np.zeros((3, S, 4, 130), np.float32)
    for c in range(NCORES):
        o = np.asarray(res[c]["oa"])
        for pi in range(3):
            out[pi, qidx_all[c][pi]] = o[pi].transpose(1, 0, 2)
    return out.reshape(3, S, 8, 65)


NCH = S // 128


def build_M():
    b = Builder()
    qT = b.din("qT", [128, S], BF)
    kT = b.din("kT", [128, S], BF)
    ktok = b.din("ktok", [128, NCH * 128], BF)
    vext = b.din("vext", [128, NCH * 65], BF)
    ipre = b.din("ipre", [128, NCH])
    logf = b.din("logf", [128, NCH])
    U_in = b.din("U", [128, 128])
    NG_in = b.din("NEGM", [128, 128])
    on_in = b.din("ones", [128, 128])
    id_in = b.din("identf", [128, 128])
    ho = b.dout("ho", [128, NCH * 64], F32)

    U = b.sb([128, 128], F32)
    NG = b.sb([128, 128], F32)
    ON = b.sb([128, 128], F32)
    IDF = b.sb([128, 128], F32)
    ip_sb = b.sb([128, NCH], F32)
    lf_sb = b.sb([128, NCH], F32)
    t_c = Tile()
    for dst, src in ((U, U_in), (NG, NG_in), (ON, on_in), (IDF, id_in), (ip_sb, ipre), (lf_sb, logf)):
        b.dma("sp", dst[:], src[:, :], writes=[t_c], key="ldc", nowaw=True)
    q_sb = b.sb([128, S], BF)
    k_sb = b.sb([128, S], BF)
    kt_sb = b.sb([128, NCH, 128], BF)
    v_sb = b.sb([128, NCH, 65], BF)
    NPC = 4
    CPP = NCH // NPC
    t_in = [Tile() for _ in range(NPC)]
    for pc in range(NPC):
        key = f"in{pc}"
        c0, c1 = pc * CPP, (pc + 1) * CPP
        b.dma("sp", q_sb[:, c0 * 128:c1 * 128], qT[:, c0 * 128:c1 * 128], writes=[t_in[pc]], key=key, nowaw=True)
        b.dma("sp", k_sb[:, c0 * 128:c1 * 128], kT[:, c0 * 128:c1 * 128], writes=[t_in[pc]], key=key, nowaw=True)
        b.dma("sp", kt_sb[:, c0:c1, :].rearrange("p n c -> p (n c)"), ktok[:, c0 * 128:c1 * 128],
              writes=[t_in[pc]], key=key, nowaw=True)
        b.dma("sp", v_sb[:, c0:c1, :].rearrange("p n c -> p (n c)"), vext[:, c0 * 65:c1 * 65],
              writes=[t_in[pc]], key=key, nowaw=True)

    ps_b = (b.ps([128, 512]), Tile())
    bcol = b.sb([128, NCH], F32)
    imb = b.sb([128, NCH], F32)
    eb = b.sb([128, NCH], F32)
    t_bcol, t_imb, t_eb = Tile(), Tile(), Tile()
    b.mm(ps_b[0][:, 0:NCH], [(U[:], lf_sb[:])], [t_c], [ps_b[1]])
    b.copy(bcol[:], ps_b[0][:, 0:NCH], [ps_b[1]], [t_bcol, ps_b[1]])
    b.tt(imb[:], ip_sb[:], bcol[:], ALU.subtract, [t_c, t_bcol], [t_imb])
    b.act(eb[:], bcol[:], AF.Exp, [t_bcol], [t_eb])

    C = b.sb([128, 65], F32)
    Cb = b.sb([128, 65], BF)
    t_C, t_Cb = Tile(), Tile()
    b.P.op("dve", lambda e: e.memset(C[:], 0.0), (), [t_C])
    b.P.op("dve", lambda e: e.memset(Cb[:], 0.0), (), [t_Cb])

    def rot(n, shape, dt):
        return Rot([(b.sb(shape, dt), Tile()) for _ in range(n)])
    LUr = rot(2, [128, 128], F32)
    DTr = rot(2, [128, 128], F32)
    dcr = rot(2, [128, 1], F32)
    SWr = rot(2, [128, 128], BF)
    hir = rot(2, [128, 65], F32)
    Htr = rot(2, [128, 65], F32)
    denr = rot(2, [128, 1], F32)
    rdr = rot(2, [128, 1], F32)
    kwr = rot(2, [128, 128], BF)
    Bmr = Rot([(b.ps([128, 512]), Tile()) for _ in range(2)])
    STr = Rot([(b.ps([128, 512]), Tile()) for _ in range(2)])
    Hi = (b.ps([128, 512]), Tile())
    He = (b.ps([128, 512]), Tile())
    dC = (b.ps([128, 512]), Tile())
    GRP = 16
    hout = Rot([(b.sb([128, GRP, 64], F32), Tile(), f"ho{i}") for i in range(2)])
    hcur = None
    for c in range(NCH):
        pc = c // CPP
        tin = t_in[pc]
        cs = slice(c * 128, (c + 1) * 128)
        if c % GRP == 0:
            hcur = hout.next()
        LU, t_LU = LUr.next()
        b.ts(LU[:], U[:], lf_sb[:, c:c + 1], ALU.mult, [t_c], [t_LU])
        Bm, t_Bm = Bmr.next()

        def fnb(e, Bm=Bm, LU=LU):
            e.matmul(Bm[:, 0:128], lhsT=ON[:], rhs=LU[:], start=True, stop=False)
            return e.matmul(Bm[:, 0:128], lhsT=IDF[:], rhs=NG[:], start=False, stop=True)
        b.pe(fnb, [t_c, t_LU], [t_Bm])
        DT, t_DT = DTr.next()
        b.act(DT[:], Bm[:, 0:128], AF.Exp, [t_Bm, t_imb], [t_DT, t_Bm], bias=imb[:, c:c + 1])
        dcol, t_dc = dcr.next()
        b.act(dcol[:], Bm[:, 127:128], AF.Exp, [t_Bm], [t_dc, t_Bm])
        ST, t_ST = STr.next()
        b.mm(ST[:, 0:128], [(k_sb[:, cs], q_sb[:, cs])], [tin], [t_ST])
        SW, t_SW = SWr.next()
        b.tt(SW[:], ST[:, 0:128], DT[:], ALU.mult, [t_ST, t_DT], [t_SW, t_ST])
        b.mm(Hi[0][:, 0:65], [(SW[:], v_sb[:, c, :])], [t_SW, tin], [Hi[1]])
        b.mm(He[0][:, 0:65], [(q_sb[:, cs], Cb[:])], [tin, t_Cb], [He[1]])
        hi, t_hi = hir.next()
        b.act(hi[:], Hi[0][:, 0:65], AF.Copy, [Hi[1]], [t_hi, Hi[1]])
        Ht, t_Ht = Htr.next()
        b.stt(Ht[:], He[0][:, 0:65], eb[:, c:c + 1], hi[:], ALU.mult, ALU.add, [He[1], t_eb, t_hi], [t_Ht, He[1]])
        den, t_den = denr.next()
        b.act(den[:], Ht[:, 64:65], AF.Abs, [t_Ht], [t_den])
        b.ts(den[:], den[:], 1.0, ALU.max, [t_den], [t_den])
        rd, t_rd = rdr.next()
        b.P.op("dve", lambda e, rd=rd, den=den: e.reciprocal(out=rd[:], in_=den[:]), [t_den], [t_rd])
        b.ts(hcur[0][:, c % GRP, :], Ht[:, 0:64], rd[:, 0:1], ALU.mult, [t_Ht, t_rd], [hcur[1]])
        kw, t_kw = kwr.next()
        b.ts(kw[:], kt_sb[:, c, :], DT[:, 127:128], ALU.mult, [tin, t_DT], [t_kw])
        b.mm(dC[0][:, 0:65], [(kw[:], v_sb[:, c, :])], [t_kw, tin], [dC[1]])
        b.stt(C[:], C[:], dcol[:, 0:1], dC[0][:, 0:65], ALU.mult, ALU.add, [t_C, t_dc, dC[1]], [t_C, dC[1]])
        b.act(Cb[:], C[:], AF.Copy, [t_C], [t_Cb])
        if c % GRP == GRP - 1:
            g = c // GRP
            b.dma("sp", ho[:, g * GRP * 64:(g + 1) * GRP * 64], hcur[0][:].rearrange("p n c -> p (n c)"),
                  reads=[hcur[1]], key=hcur[2])
    return b.finish()


def run_M(qm, km, vm, ipre, logf):
    nc = build_M()
    a = np.arange(128)
    U = (a[:, None] <= a[None, :]).astype(np.float32)
    NEGM = np.where(a[:, None] <= a[None, :], 0.0, NEG).astype(np.float32)
    ones = np.ones((128, 128), np.float32)
    identf = np.eye(128, dtype=np.float32)
    maps = []
    for c in range(NCORES):
        h, vh = c // 2, c % 2
        qh = qm[:, h * 128:(h + 1) * 128]
        kh = km[:, h * 128:(h + 1) * 128]
        v = vm[:, h * 128 + vh * 64:h * 128 + vh * 64 + 64].reshape(NCH, 128, 64)
        ve = np.concatenate([v, np.ones((NCH, 128, 1), v.dtype)], -1)
        maps.append({
            "qT": np.ascontiguousarray(qh.T), "kT": np.ascontiguousarray(kh.T),
            "ktok": np.ascontiguousarray(kh.reshape(NCH, 128, 128).transpose(1, 0, 2)).reshape(128, -1),
            "vext": np.ascontiguousarray(ve.transpose(1, 0, 2)).reshape(128, -1),
            "ipre": np.ascontiguousarray(ipre[:, h].reshape(NCH, 128).T),
            "logf": np.ascontiguousarray(logf[:, h].reshape(NCH, 128).T),
            "U": U, "NEGM": NEGM, "ones": ones, "identf": identf})
    res = run_bass_kernel_spmd(nc, maps, core_ids=list(range(NCORES))).results
    out = np.zeros((S, 4, 128), np.float32)
    for c in range(NCORES):
        h, vh = c // 2, c % 2
        o = np.asarray(res[c]["ho"]).reshape(128, NCH, 64)
        out[:, h, vh * 64:(vh + 1) * 64] = o.transpose(1, 0, 2).reshape(S, 64)
    return out


def ln_tile(b, z, t_z, out, t_out, lng, lnb, t_par, scr):
    st, mv, sq, rs = scr
    t_st, t_mv, t_sq, t_rs = Tile(), Tile(), Tile(), Tile()
    b.P.op("dve", lambda e: e.bn_stats(out=st[:, 0:6], in_=z[:, 0:512]), [t_z], [t_st])
    b.P.op("dve", lambda e: e.bn_stats(out=st[:, 6:12], in_=z[:, 512:1024]), [t_z], [t_st], nowaw=True)
    b.P.op("dve", lambda e: e.bn_aggr(out=mv[:, 0:2], in_=st[:, 0:12]), [t_st], [t_mv])
    b.act(sq[:, 0:1], mv[:, 1:2], AF.Sqrt, [t_mv, t_par], [t_sq], bias=EPS_AP[0][:, 0:1])
    b.P.op("dve", lambda e: e.reciprocal(out=rs[:, 0:1], in_=sq[:, 0:1]), [t_sq], [t_rs])
    b.ts(out[:], z[:], mv[:, 0:1], ALU.subtract, [t_z, t_mv, t_rs], [t_out], s2=rs[:, 0:1], op1=ALU.mult)
    b.tt(out[:], out[:], lng[:], ALU.mult, [t_out, t_par], [t_out])
    b.tt(out[:], out[:], lnb[:], ALU.add, [t_out, t_par], [t_out])


EPS_AP = [None]


def build_D1():
    b = Builder()
    xT = b.din("xT", [D, TPC])
    x_tok = b.din("x_tok", [TPC, D])
    oa_tok = b.din("oa_tok", [3, TPC, 520])
    hm_in = b.din("hm", [TPC, 512])
    sz_in = b.din("sigz", [TPC, 512])
    gm_in = b.din("gm_b", [128, 512])
    lng_in = b.din("lng_b", [128, D])
    lnb_in = b.din("lnb_b", [128, D])
    wg_in = b.din("w_gate", [D, 2048])
    bg_in = b.din("bg", [128, 16])
    wbra_in = b.din("w_br_a", [512, D])
    wbrm_in = b.din("w_br_m", [512, D])
    wo_in = b.din("w_o", [D, D])
    id_in = b.din("ident", [128, 128])
    eps_in = b.din("eps", [128, 1])
    x1 = b.dout("x1", [TPC, D], F32)

    t_par = Tile()
    gm = b.sb([128, 512], F32)
    lng = b.sb([128, D], F32)
    lnb = b.sb([128, D], F32)
    bg = b.sb([128, 16], F32)
    eps = b.sb([128, 1], F32)
    EPS_AP[0] = eps
    for dst, src in ((gm, gm_in), (lng, lng_in), (lnb, lnb_in), (bg, bg_in), (eps, eps_in)):
        b.dma("sp", dst[:], src[:, :], writes=[t_par], key="ldp", nowaw=True)
    ident = b.sb([128, 128], BF)
    wbra = b.sb([128, 4, D], BF)
    wbrm = b.sb([128, 4, D], BF)
    wo = b.sb([128, 8, D], BF)
    xb = b.sb([128, 8, TPC], BF)
    t_w = Tile()
    b.dma("pool", ident[:], id_in[:, :], writes=[t_w], key="ldw", nowaw=True)
    t_xb = Tile()
    xTv = xT.rearrange("(kc p) t -> p kc t", p=128)
    for kc in range(8):
        b.dma("pool", xb[:, kc, :], xTv[:, kc, :], writes=[t_xb], key="ldx", nowaw=True)
    b.dma("pool", wbra[:], wbra_in.rearrange("(kc p) n -> p kc n", p=128), writes=[t_w], key="ldw", nowaw=True)
    b.dma("pool", wbrm[:], wbrm_in.rearrange("(kc p) n -> p kc n", p=128), writes=[t_w], key="ldw", nowaw=True)
    wov = wo_in.rearrange("(kc p) n -> p kc n", p=128)
    for kc in range(0, 8, 2):
        b.dma("pool", wo[:, kc:kc + 2, :], wov[:, kc:kc + 2, :], writes=[t_w], key="ldw", nowaw=True)
    wgv = wg_in.rearrange("(kc p) (j n) -> p kc j n", p=128, j=2)
    wgr = Rot([(b.sb([128, 8, 2, 128], BF), Tile(), f"wg{i}") for i in range(2)])

    def rot(n, shape, dt, key=None):
        return Rot([(b.sb(shape, dt), Tile(), (f"{key}{i}" if key else None)) for i in range(n)])
    o3r = rot(1, [128, 3, 520], F32, "o3")
    hmr = rot(2, [128, 512], F32, "hm")
    szr = rot(2, [128, 512], F32, "sz")
    s1 = (b.sb([128, 520], F32), Tile())
    rd = (b.sb([128, 8], F32), Tile())
    yab = (b.sb([128, 512], BF), Tile())
    ymb = (b.sb([128, 512], BF), Tile())
    hn = (b.sb([128, 512], F32), Tile())
    st6 = b.sb([128, 4, 6], F32)
    mv = b.sb([128, 4, 2], F32)
    sq4 = b.sb([128, 4], F32)
    rs4 = b.sb([128, 4], F32)
    t_st6, t_mv, t_sq4, t_rs4 = Tile(), Tile(), Tile(), Tile()
    yTr = Rot([(b.sb([128, 4, TG], BF), b.sb([128, 4, TG], BF), Tile()) for _ in range(2)])
    mgr = Rot([(b.sb([128, 8, TG], BF), Tile()) for _ in range(2)])
    sgr = rot(2, [128, TG], F32)
    t1r = rot(2, [128, TG], F32)
    xtr = rot(2, [128, D], F32, "xt")
    zr = rot(2, [128, D], F32)
    outr = rot(2, [128, D], F32, "ot")
    lscr = (b.sb([128, 12], F32), b.sb([128, 2], F32), b.sb([128, 1], F32), b.sb([128, 1], F32))
    psr = Rot([(b.ps([128, 512]), Tile()) for _ in range(5)])
    tp = (b.ps([128, 8, 128], BF), Tile())
    ybk = Rot([(b.ps([128, 512]), Tile()) for _ in range(2)])

    for tg in range(4):
        yaT, ymT, t_yT = yTr.next()
        for ti in range(4):
            tt = tg * 4 + ti
            rows = slice(tt * 128, (tt + 1) * 128)
            o3, t_o3, k_o3 = o3r.next()
            b.dma("sp", o3[:], oa_tok[:, rows, :].rearrange("n q c -> q n c"), writes=[t_o3], key=k_o3)
            hmt, t_hm, k_hm = hmr.next()
            b.dma("sp", hmt[:], hm_in[rows, :], writes=[t_hm], key=k_hm)
            szt, t_sz, k_sz = szr.next()
            b.dma("sp", szt[:], sz_in[rows, :], writes=[t_sz], key=k_sz)
            b.tt(s1[0][:], o3[:, 0, :], o3[:, 1, :], ALU.add, [t_o3], [s1[1]])
            b.tt(s1[0][:], s1[0][:], o3[:, 2, :], ALU.add, [t_o3, s1[1]], [s1[1]])
            s1v = s1[0][:].rearrange("p (h c) -> p h c", c=65)
            b.P.op("dve", lambda e, s1v=s1v: e.reciprocal(out=rd[0][:], in_=s1v[:, :, 64]), [s1[1]], [rd[1]])
            for h in range(8):
                b.ts(yab[0][:, h * 64:(h + 1) * 64], s1v[:, h, 0:64], rd[0][:, h:h + 1], ALU.mult,
                     [s1[1], rd[1]], [yab[1]])
            for h in range(4):
                b.P.op("dve", lambda e, h=h, hmt=hmt: e.bn_stats(out=st6[:, h, :], in_=hmt[:, h * 128:(h + 1) * 128]),
                       [t_hm], [t_st6])
                b.P.op("dve", lambda e, h=h: e.bn_aggr(out=mv[:, h, :], in_=st6[:, h, :]), [t_st6], [t_mv])
            b.act(sq4[:], mv[:, :, 1], AF.Sqrt, [t_mv, t_par], [t_sq4], bias=eps[:, 0:1])
            b.P.op("dve", lambda e: e.reciprocal(out=rs4[:], in_=sq4[:]), [t_sq4], [t_rs4])
            for h in range(4):
                b.ts(hn[0][:, h * 128:(h + 1) * 128], hmt[:, h * 128:(h + 1) * 128], mv[:, h, 0:1], ALU.subtract,
                     [t_hm, t_mv, t_rs4], [hn[1]], s2=rs4[:, h:h + 1], op1=ALU.mult)
            b.tt(hn[0][:], hn[0][:], gm[:], ALU.mult, [hn[1], t_par], [hn[1]])
            b.tt(ymb[0][:], hn[0][:], szt[:], ALU.mult, [hn[1], t_sz], [ymb[1]])

            def ftp(e):
                for j in range(4):
                    e.transpose(tp[0][:, j, :], yab[0][:, j * 128:(j + 1) * 128], ident[:])
                for j in range(4):
                    ins = e.transpose(tp[0][:, 4 + j, :], ymb[0][:, j * 128:(j + 1) * 128], ident[:])
                return ins
            b.pe(ftp, [yab[1], ymb[1], t_w], [tp[1]])
            cs = slice(ti * 128, (ti + 1) * 128)
            b.copy(yaT[:, :, cs], tp[0][:, 0:4, :], [tp[1]], [t_yT, tp[1]])
            b.act(ymT[:, :, cs], tp[0][:, 4:8, :], AF.Copy, [tp[1]], [t_yT, tp[1]])
        mg, t_mg = mgr.next()
        tcols = slice(tg * TG, (tg + 1) * TG)
        for n in range(8):
            wg, t_wg, k_wg = wgr.next()
            b.dma("pool", wg[:, :, 0, :], wgv[:, :, 0, n * 128:(n + 1) * 128], writes=[t_wg], key=k_wg)
            b.dma("pool", wg[:, :, 1, :], wgv[:, :, 1, n * 128:(n + 1) * 128], writes=[t_wg], key=k_wg, nowaw=True)
            t1, t_t1, _ = t1r.next()
            for j, (wbr, yT) in enumerate(((wbra, yaT), (wbrm, ymT))):
                pa, t_pa = psr.next()
                b.mm(pa[:, :], [(wbr[:, kc, n * 128:(n + 1) * 128], yT[:, kc, :]) for kc in range(4)],
                     [t_w, t_yT], [t_pa])
                ga, t_ga = psr.next()
                b.mm(ga[:, :], [(wg[:, kc, j, :], xb[:, kc, tcols]) for kc in range(8)], [t_wg, t_xb], [t_ga])
                sg, t_sg, _ = sgr.next()
                b.act(sg[:], ga[:, :], AF.Sigmoid, [t_ga, t_par], [t_sg, t_ga], bias=bg[:, j * 8 + n:j * 8 + n + 1])
                if j == 0:
                    b.tt(t1[:], pa[:, :], sg[:], ALU.mult, [t_pa, t_sg], [t_t1, t_pa])
                else:
                    b.tt(sg[:], pa[:, :], sg[:], ALU.mult, [t_pa, t_sg], [t_sg, t_pa])
                    b.tt(mg[:, n, :], t1[:], sg[:], ALU.add, [t_t1, t_sg], [t_mg])
        for ti in range(4):
            tt = tg * 4 + ti
            rows = slice(tt * 128, (tt + 1) * 128)
            xt, t_xt, k_xt = xtr.next()
            b.dma("sp", xt[:], x_tok[rows, :], writes=[t_xt], key=k_xt)
            z, t_z, _ = zr.next()
            for nh in range(2):
                yb, t_yb = ybk.next()
                b.mm(yb[:, :], [(mg[:, kc, ti * 128:(ti + 1) * 128], wo[:, kc, nh * 512:(nh + 1) * 512])
                                for kc in range(8)], [t_mg, t_w], [t_yb])
                b.stt(z[:, nh * 512:(nh + 1) * 512], xt[:, nh * 512:(nh + 1) * 512], ALPHA, yb[:, :],
                      ALU.mult, ALU.add, [t_xt, t_yb], [t_z, t_yb])
            ot, t_ot, k_ot = outr.next()
            ln_tile(b, z, t_z, ot, t_ot, lng, lnb, t_par, lscr)
            b.dma("sp", x1[rows, :], ot[:], reads=[t_ot], key=k_ot)
    return b.finish()


def bcast128(v):
    return np.ascontiguousarray(np.broadcast_to(np.asarray(v, np.float32)[None, :], (128, v.shape[0])))


def run_D1(x_tok, oa_parts, h_raw, sigz_tok, inp, l):
    nc = build_D1()
    com = {"gm_b": bcast128(inp["m_norm_g"][l]), "lng_b": bcast128(inp["ln_g"][l, 0]),
           "lnb_b": bcast128(inp["ln_b"][l, 0]), "w_gate": np.ascontiguousarray(inp["w_gate"][l]),
           "bg": np.ascontiguousarray(inp["b_gate"][l].reshape(16, 128).T),
           "w_br_a": np.ascontiguousarray(inp["w_br_a"][l]), "w_br_m": np.ascontiguousarray(inp["w_br_m"][l]),
           "w_o": np.ascontiguousarray(inp["w_o"][l]), "ident": np.eye(128, dtype=np.float32),
           "eps": np.full((128, 1), LN_EPS, np.float32)}
    maps = []
    for c in range(NCORES):
        sl = slice(c * TPC, (c + 1) * TPC)
        m = dict(com)
        m["xT"] = np.ascontiguousarray(x_tok[sl].T)
        m["x_tok"] = np.ascontiguousarray(x_tok[sl])
        m["oa_tok"] = np.ascontiguousarray(oa_parts[:, sl].reshape(3, TPC, 520))
        m["hm"] = np.ascontiguousarray(h_raw[sl].reshape(TPC, 512))
        m["sigz"] = np.ascontiguousarray(sigz_tok[sl])
        maps.append(m)
    res = run_bass_kernel_spmd(nc, maps, core_ids=list(range(NCORES))).results
    return np.concatenate([np.asarray(res[c]["x1"]) for c in range(NCORES)], 0)


class LNS:
    def __init__(self, b):
        self.st = b.sb([128, 12], F32)
        self.mv = b.sb([128, 2], F32)
        self.sq = b.sb([128, 1], F32)
        self.rs = b.sb([128, 1], F32)
        self.t_st, self.t_mv, self.t_sq, self.t_rs = Tile(), Tile(), Tile(), Tile()


def ln_tile2(b, z, t_z, out, t_out, lng, lnb, eps, t_par, S_):
    b.P.op("dve", lambda e: e.bn_stats(out=S_.st[:, 0:6], in_=z[:, 0:512]), [t_z], [S_.t_st])
    b.P.op("dve", lambda e: e.bn_stats(out=S_.st[:, 6:12], in_=z[:, 512:1024]), [t_z], [S_.t_st], nowaw=True)
    b.P.op("dve", lambda e: e.bn_aggr(out=S_.mv[:, 0:2], in_=S_.st[:, 0:12]), [S_.t_st], [S_.t_mv])
    b.act(S_.sq[:, 0:1], S_.mv[:, 1:2], AF.Sqrt, [S_.t_mv, t_par], [S_.t_sq], bias=eps[:, 0:1])
    b.P.op("dve", lambda e: e.reciprocal(out=S_.rs[:, 0:1], in_=S_.sq[:, 0:1]), [S_.t_sq], [S_.t_rs])
    b.ts(out[:], z[:], S_.mv[:, 0:1], ALU.subtract, [t_z, S_.t_mv, S_.t_rs], [t_out], s2=S_.rs[:, 0:1], op1=ALU.mult)
    b.tt(out[:], out[:], lng[:], ALU.mult, [t_out, t_par], [t_out])
    b.tt(out[:], out[:], lnb[:], ALU.add, [t_out, t_par], [t_out])


def build_F(n_units, cpu, moe):
    b = Builder()
    FU = cpu * 128
    HT = 1024
    xT = b.din("xT", [D, TPC])
    x_tok = b.din("x_tok", [TPC, D])
    w13 = b.din("w13u", [n_units, D, 2, FU])
    w2 = b.din("w2u", [n_units, FU, D])
    lng_in = b.din("lng_b", [128, D])
    lnb_in = b.din("lnb_b", [128, D])
    eps_in = b.din("eps", [128, 1])
    if moe:
        rw_in = b.din("rw", [D, N_EXP])
        rb_in = b.din("rb_b", [128, N_EXP])
        upe = n_units // N_EXP
    x2 = b.dout("x2", [TPC, D], F32)

    t_par = Tile()
    lng = b.sb([128, D], F32)
    lnb = b.sb([128, D], F32)
    eps = b.sb([128, 1], F32)
    par = [(lng, lng_in), (lnb, lnb_in), (eps, eps_in)]
    if moe:
        rb = b.sb([128, N_EXP], F32)
        par.append((rb, rb_in))
        rw = b.sb([128, 8, N_EXP], F32)
    for dst, src in par:
        b.dma("sp", dst[:], src[:, :], writes=[t_par], key="ldp", nowaw=True)
    if moe:
        b.dma("sp", rw[:], rw_in.rearrange("(kc p) e -> p kc e", p=128), writes=[t_par], key="ldp", nowaw=True)

    xb = b.sb([128, 8, HT], BF)
    t_xb = Tile()
    hT = b.sb([128, cpu, HT], BF)
    t_h = [Tile() for _ in range(HT // 512)]
    w2r = Rot([(b.sb([128, cpu, D], BF), Tile(), f"w2{i}") for i in range(2)])
    w13r = Rot([(b.sb([128, 8, 2, 128], BF), Tile(), f"w13{i}") for i in range(3)])
    acc = b.sb([128, HT // 128, D], F32)
    t_acc = [Tile() for _ in range(HT // 128)]
    sar = Rot([(b.sb([128, 512], F32), Tile()) for _ in range(2)])
    xtr = Rot([(b.sb([128, D], F32), Tile(), f"xt{i}") for i in range(2)])
    otr = Rot([(b.sb([128, D], F32), Tile(), f"ot{i}") for i in range(2)])
    lns = LNS(b)
    pa_r = Rot([(b.ps([128, 512]), Tile()) for _ in range(2)])
    pg_r = Rot([(b.ps([128, 512]), Tile()) for _ in range(2)])
    py_r = Rot([(b.ps([128, 512]), Tile()) for _ in range(3)])
    if moe:
        gates = b.sb([128, TPC // 128, N_EXP], F32)
        t_gate = [Tile() for _ in range(TPC // 128)]
        xfr = Rot([(b.sb([128, 8, 128], F32), Tile(), f"xf{i}") for i in range(2)])
        pl = (b.ps([128, 512]), Tile())
        lg = b.sb([128, N_EXP], F32)
        mx = b.sb([128, 8], F32)
        msk = b.sb([128, N_EXP], F32)
        nm1 = b.sb([128, 1], F32)
        ex = b.sb([128, N_EXP], F32)
        den = b.sb([128, 1], F32)
        rden = b.sb([128, 1], F32)
        t_lg, t_mx, t_msk, t_nm1, t_ex, t_den, t_rden = (Tile() for _ in range(7))

    xTv = xT.rearrange("(kc p) t -> p kc t", p=128)
    w13v = w13.rearrange("u (kc p) j f -> u p kc j f", p=128)
    w2v = w2.rearrange("u (fc p) n -> u p fc n", p=128)
    for half in range(TPC // HT):
        t0 = half * HT
        for kc in range(0, 8, 2):
            b.dma("pool", xb[:, kc:kc + 2, :], xTv[:, kc:kc + 2, t0:t0 + HT], writes=[t_xb], key="ldx",
                  nowaw=(kc > 0))
        if moe:
            for ti in range(HT // 128):
                tt = half * (HT // 128) + ti
                xf, t_xf, k_xf = xfr.next()
                b.dma("sp", xf[:], xTv[:, :, tt * 128:(tt + 1) * 128], writes=[t_xf], key=k_xf)
                b.mm(pl[0][:, 0:N_EXP], [(xf[:, kc, :], rw[:, kc, :]) for kc in range(8)], [t_xf, t_par], [pl[1]])
                b.tt(lg[:], pl[0][:, 0:N_EXP], rb[:], ALU.add, [pl[1], t_par], [t_lg, pl[1]])
                b.P.op("dve", lambda e: e.max(out=mx[:], in_=lg[:]), [t_lg], [t_mx])
                b.ts(msk[:], lg[:], mx[:, 1:2], ALU.is_ge, [t_lg, t_mx], [t_msk])
                b.ts(nm1[:], mx[:, 0:1], -1.0, ALU.mult, [t_mx], [t_nm1])
                b.act(ex[:], lg[:], AF.Exp, [t_lg, t_nm1], [t_ex], bias=nm1[:, 0:1])
                b.tt(ex[:], ex[:], msk[:], ALU.mult, [t_ex, t_msk], [t_ex])
                b.P.op("dve", lambda e: e.reduce_sum(out=den[:], in_=ex[:], axis=mybir.AxisListType.X),
                       [t_ex], [t_den])
                b.P.op("dve", lambda e: e.reciprocal(out=rden[:], in_=den[:]), [t_den], [t_rden])
                b.ts(gates[:, tt, :], ex[:], rden[:, 0:1], ALU.mult, [t_ex, t_rden], [t_gate[tt]])
        for u in range(n_units):
            w2t = None
            for fc in range(cpu):
                wt, t_w, k_w = w13r.next()
                b.dma("pool", wt[:, :, 0, :], w13v[u][:, :, 0, fc * 128:(fc + 1) * 128], writes=[t_w], key=k_w)
                b.dma("pool", wt[:, :, 1, :], w13v[u][:, :, 1, fc * 128:(fc + 1) * 128], writes=[t_w], key=k_w,
                      nowaw=True)
                if fc == min(1, cpu - 1):
                    w2t, t_w2, k_w2 = w2r.next()
                    b.dma("pool", w2t[:], w2v[u], writes=[t_w2], key=k_w2)
                for tg in range(HT // 512):
                    cs = slice(tg * 512, (tg + 1) * 512)
                    pa, t_pa = pa_r.next()
                    pg, t_pg = pg_r.next()
                    b.mm(pa[:, :], [(wt[:, kc, 0, :], xb[:, kc, cs]) for kc in range(8)], [t_w, t_xb], [t_pa])
                    b.mm(pg[:, :], [(wt[:, kc, 1, :], xb[:, kc, cs]) for kc in range(8)], [t_w, t_xb], [t_pg])
                    sa, t_sa = sar.next()
                    b.act(sa[:], pa[:, :], AF.Silu, [t_pa], [t_sa, t_pa])
                    b.tt(hT[:, fc, cs], pg[:, :], sa[:], ALU.mult, [t_pg, t_sa], [t_h[tg], t_pg], nowaw=(fc > 0))
            for ti in range(HT // 128):
                tt = half * (HT // 128) + ti
                for nh in range(2):
                    py, t_py = py_r.next()
                    b.mm(py[:, :], [(hT[:, fc, ti * 128:(ti + 1) * 128], w2t[:, fc, nh * 512:(nh + 1) * 512])
                                    for fc in range(cpu)], [t_h[ti // 4], t_w2], [t_py])
                    dst = acc[:, ti, nh * 512:(nh + 1) * 512]
                    if moe:
                        g = gates[:, tt, u // upe:u // upe + 1]
                        rd = [t_py, t_gate[tt]]
                    else:
                        g = 1.0
                        rd = [t_py]
                    if u == 0:
                        b.ts(dst, py[:, :], g, ALU.mult, rd, [t_acc[ti], t_py], nowaw=(nh > 0))
                    else:
                        b.stt(dst, py[:, :], g, dst, ALU.mult, ALU.add, rd + [t_acc[ti]], [t_acc[ti], t_py])
        for ti in range(HT // 128):
            tt = half * (HT // 128) + ti
            rows = slice(tt * 128, (tt + 1) * 128)
            xt, t_xt, k_xt = xtr.next()
            b.dma("sp", xt[:], x_tok[rows, :], writes=[t_xt], key=k_xt)
            b.stt(xt[:], xt[:], ALPHA, acc[:, ti, :], ALU.mult, ALU.add, [t_xt, t_acc[ti]], [t_xt])
            ot, t_ot, k_ot = otr.next()
            ln_tile2(b, xt, t_xt, ot, t_ot, lng, lnb, eps, t_par, lns)
            b.dma("sp", x2[rows, :], ot[:], reads=[t_ot], key=k_ot)
    return b.finish()


def run_F(x1, inp, l):
    j = l // 2
    com = {"lng_b": bcast128(inp["ln_g"][l, 1]), "lnb_b": bcast128(inp["ln_b"][l, 1]),
           "eps": np.full((128, 1), LN_EPS, np.float32)}
    if l % 2 == 0:
        n_units, cpu, moe = 2, 11, False
        w13 = inp["ffn_w13"][j].reshape(D, 2, n_units, cpu * 128).transpose(2, 0, 1, 3)
        w2 = inp["ffn_w2"][j].reshape(n_units, cpu * 128, D)
    else:
        upe, cpu, moe = 4, 7, True
        n_units = N_EXP * upe
        w13 = inp["exp_w13"][j].reshape(N_EXP, D, 2, upe, cpu * 128).transpose(0, 3, 1, 2, 4).reshape(
            n_units, D, 2, cpu * 128)
        w2 = inp["exp_w2"][j].reshape(n_units, cpu * 128, D)
        com["rw"] = np.ascontiguousarray(inp["router_w"][j])
        com["rb_b"] = bcast128(inp["router_b"][j])
    com["w13u"] = np.ascontiguousarray(w13)
    com["w2u"] = np.ascontiguousarray(w2)
    nc = build_F(n_units, cpu, moe)
    maps = []
    for c in range(NCORES):
        sl = slice(c * TPC, (c + 1) * TPC)
        m = dict(com)
        m["xT"] = np.ascontiguousarray(x1[sl].T)
        m["x_tok"] = np.ascontiguousarray(x1[sl])
        maps.append(m)
    res = run_bass_kernel_spmd(nc, maps, core_ids=list(range(NCORES))).results
    return np.concatenate([np.asarray(res[c]["x2"]) for c in range(NCORES)], 0)


def build_W():
    b = Builder()
    NFC = D_FF_E // 128
    w13 = b.din("w13", [D, 2, D_FF_E])
    w2 = b.din("w2", [D_FF_E, D])
    w13b = b.dout("w13b", [NFC, 128, 8, 2, 128], BF)
    w2b = b.dout("w2b", [4, 128, NFC, 256], BF)
    ring = Rot([(b.sb([128, 2 * D_FF_E], BF), Tile(), f"r{i}") for i in range(3)])
    for kc in range(8):
        t, t_t, k = ring.next()
        tv = t[:].rearrange("p (j n) -> p j n", j=2)
        b.dma("pool", tv, w13[kc * 128:(kc + 1) * 128, :, :], writes=[t_t], key=k)
        for j in range(2):
            b.dma("sp", w13b[:, :, kc, j, :].rearrange("c p f -> p c f"),
                  tv[:, j, :].rearrange("p (c f) -> p c f", f=128), reads=[t_t], key="s" + k)
    w2v = w2.rearrange("(fc p) n -> p fc n", p=128)
    for g in range(4):
        t, t_t, k = ring.next()
        tv = t[:].rearrange("p (c n) -> p c n", n=D)
        b.dma("pool", tv, w2v[:, g * 7:(g + 1) * 7, :], writes=[t_t], key=k)
        for q in range(4):
            b.dma("sp", w2b[q][:, g * 7:(g + 1) * 7, :], tv[:, :, q * 256:(q + 1) * 256], reads=[t_t], key="s" + k)
    return b.finish()


def run_W(inp, j):
    nc = build_W()
    maps = [{"w13": np.ascontiguousarray(inp["exp_w13"][j][e]).reshape(D, 2, D_FF_E),
             "w2": np.ascontiguousarray(inp["exp_w2"][j][e])} for e in range(N_EXP)]
    res = run_bass_kernel_spmd(nc, maps, core_ids=list(range(N_EXP))).results
    w13b = np.stack([np.asarray(res[e]["w13b"]) for e in range(N_EXP)])
    w2b = np.stack([np.asarray(res[e]["w2b"]) for e in range(N_EXP)])
    return w13b, w2b


CAP = 384
NSB = CAP // 128


def build_FS():
    b = Builder()
    HT = 1024
    NT = HT // 128
    NHALF = TPC // HT
    NFC = D_FF_E // 128
    xT = b.din("xT", [D, TPC])
    x_tok = b.din("x_tok", [TPC, D])
    w13 = b.din("w13e", [N_EXP, NFC, 128, 8 * 2 * 128], BF)
    w2 = b.din("w2e", [N_EXP, 4, 128, NFC * 256], BF)
    lng_in = b.din("lng_b", [128, D])
    lnb_in = b.din("lnb_b", [128, D])
    eps_in = b.din("eps", [128, 1])
    rw_in = b.din("rw", [D, N_EXP])
    rb_in = b.din("rb_b", [128, N_EXP])
    ones_in = b.din("ones", [128, 128])
    us_in = b.din("ustrict", [128, 128])
    iota_in = b.din("iota", [128, CAP])
    id_in = b.din("ident", [128, 128])
    x2 = b.dout("x2", [TPC, D], F32)
    cnt_out = b.dout("cnt", [1, NHALF * N_EXP], F32)

    t_par = Tile()
    lng = b.sb([128, D], F32)
    lnb = b.sb([128, D], F32)
    eps = b.sb([128, 1], F32)
    rb = b.sb([128, N_EXP], F32)
    rw = b.sb([128, 8, N_EXP], F32)
    ONES = b.sb([128, 128], F32)
    US = b.sb([128, 128], F32)
    iota = b.sb([128, CAP], F32)
    ident = b.sb([128, 128], BF)
    for dst, src in ((lng, lng_in), (lnb, lnb_in), (eps, eps_in), (rb, rb_in), (ONES, ones_in), (US, us_in),
                     (iota, iota_in)):
        b.dma("sp", dst[:], src[:, :], writes=[t_par], key="ldp", nowaw=True)
    b.dma("sp", rw[:], rw_in.rearrange("(kc p) e -> p kc e", p=128), writes=[t_par], key="ldp", nowaw=True)
    t_id = Tile()
    b.dma("pool", ident[:], id_in[:, :], writes=[t_id], key="ldi")

    xtb = b.sb([128, NT, D], BF)
    t_xtb = Tile()
    Sel = b.sb([128, NT, CAP], BF)
    t_sel = Tile()
    SelT = b.sb([128, NSB, HT], BF)
    t_selT = [Tile() for _ in range(NSB)]
    xg = b.sb([128, 8, CAP], BF)
    t_xg = Tile()
    oe = b.sb([128, NSB, D], BF)
    t_oe = [Tile() for _ in range(NSB)]
    hT = b.sb([128, NFC, CAP], BF)
    t_hT = Tile()
    w13r = Rot([(b.sb([128, 8, 2, 128], BF), Tile(), f"w13{i}") for i in range(3)])
    w2r = Rot([(b.sb([128, NFC, 256], BF), Tile(), f"w2{i}") for i in range(2)])
    acc = b.sb([128, NT, D], F32)
    t_acc = [Tile() for _ in range(NT)]
    sar = Rot([(b.sb([128, CAP], F32), Tile()) for _ in range(2)])
    xtr = Rot([(b.sb([128, D], F32), Tile(), f"xt{i}") for i in range(2)])
    otr = Rot([(b.sb([128, D], F32), Tile(), f"ot{i}") for i in range(2)])
    lns = LNS(b)
    psr = Rot([(b.ps([128, 512]), Tile()) for _ in range(4)])
    pcr = Rot([(b.ps([128, 512]), Tile()) for _ in range(2)])
    tp = (b.ps([128, NT, 128], BF), Tile())
    pm = (b.ps([128, 512]), Tile())
    gates = b.sb([128, TPC // 128, N_EXP], F32)
    mska = b.sb([128, TPC // 128, N_EXP], F32)
    possb = b.sb([128, NT, N_EXP], F32)
    cntsb = b.sb([128, NHALF * N_EXP], F32)
    t_gate = [Tile() for _ in range(TPC // 128)]
    t_mska = [Tile() for _ in range(TPC // 128)]
    t_pos = [Tile() for _ in range(NT)]
    t_cnt = Tile()
    xfr = Rot([(b.sb([128, 8, 128], F32), Tile(), f"xf{i}") for i in range(2)])
    lg = b.sb([128, N_EXP], F32)
    mx = b.sb([128, 8], F32)
    nm1 = b.sb([128, 1], F32)
    ex = b.sb([128, N_EXP], F32)
    den = b.sb([128, 1], F32)
    rden = b.sb([128, 1], F32)
    t_lg, t_mx, t_nm1, t_ex, t_den, t_rden = (Tile() for _ in range(6))

    xTv = xT.rearrange("(kc p) t -> p kc t", p=128)
    for half in range(NHALF):
        t0 = half * HT
        xtv = x_tok[t0:t0 + HT, :].rearrange("(n p) d -> p n d", p=128)
        for n0 in range(0, NT, 2):
            b.dma("pool", xtb[:, n0:n0 + 2, :], xtv[:, n0:n0 + 2, :], writes=[t_xtb], key="ldx", nowaw=(n0 > 0))
        for ti in range(NT):
            tt = half * NT + ti
            xf, t_xf, k_xf = xfr.next()
            b.dma("sp", xf[:], xTv[:, :, tt * 128:(tt + 1) * 128], writes=[t_xf], key=k_xf)
            b.mm(pm[0][:, 0:N_EXP], [(xf[:, kc, :], rw[:, kc, :]) for kc in range(8)], [t_xf, t_par], [pm[1]])
            b.tt(lg[:], pm[0][:, 0:N_EXP], rb[:], ALU.add, [pm[1], t_par], [t_lg, pm[1]])
            b.P.op("dve", lambda e: e.max(out=mx[:], in_=lg[:]), [t_lg], [t_mx])
            b.ts(mska[:, tt, :], lg[:], mx[:, 1:2], ALU.is_ge, [t_lg, t_mx], [t_mska[tt]])
            b.ts(nm1[:], mx[:, 0:1], -1.0, ALU.mult, [t_mx], [t_nm1])
            b.act(ex[:], lg[:], AF.Exp, [t_lg, t_nm1], [t_ex], bias=nm1[:, 0:1])
            b.tt(ex[:], ex[:], mska[:, tt, :], ALU.mult, [t_ex, t_mska[tt]], [t_ex])
            b.P.op("dve", lambda e: e.reduce_sum(out=den[:], in_=ex[:], axis=mybir.AxisListType.X), [t_ex], [t_den])
            b.P.op("dve", lambda e: e.reciprocal(out=rden[:], in_=den[:]), [t_den], [t_rden])
            b.ts(gates[:, tt, :], ex[:], rden[:, 0:1], ALU.mult, [t_ex, t_rden], [t_gate[tt]])
        mrd = [t_mska[half * NT + ti] for ti in range(NT)]
        for ti in range(NT):
            pairs = [(ONES[:], mska[:, half * NT + tp_, :]) for tp_ in range(ti)] + [(US[:], mska[:, half * NT + ti, :])]
            b.mm(pm[0][:, 0:N_EXP], pairs, mrd[:ti + 1] + [t_par], [pm[1]])
            b.copy(possb[:, ti, :], pm[0][:, 0:N_EXP], [pm[1]], [t_pos[ti], pm[1]])
        b.mm(pm[0][:, 0:N_EXP], [(ONES[:], mska[:, half * NT + ti, :]) for ti in range(NT)], mrd + [t_par], [pm[1]])
        b.copy(cntsb[:, half * N_EXP:(half + 1) * N_EXP], pm[0][:, 0:N_EXP], [pm[1]], [t_cnt, pm[1]])
        for e_ in range(N_EXP):
            for ti in range(NT):
                tt = half * NT + ti
                b.ts(Sel[:, ti, :], iota[:, :], possb[:, ti, e_:e_ + 1], ALU.is_equal,
                     [t_par, t_pos[ti], t_mska[tt]], [t_sel], s2=mska[:, tt, e_:e_ + 1], op1=ALU.mult, nowaw=(ti > 0))
            for kc in range(8):
                pg_, t_pg_ = psr.next()
                b.mm(pg_[:, 0:CAP], [(xtb[:, ti, kc * 128:(kc + 1) * 128], Sel[:, ti, :]) for ti in range(NT)],
                     [t_xtb, t_sel], [t_pg_])
                b.act(xg[:, kc, :], pg_[:, 0:CAP], AF.Copy, [t_pg_], [t_xg, t_pg_])
            for sb_ in range(NSB):
                def ftp(e, sb_=sb_):
                    for ti in range(NT):
                        ins = e.transpose(tp[0][:, ti, :], Sel[:, ti, sb_ * 128:(sb_ + 1) * 128], ident[:])
                    return ins
                b.pe(ftp, [t_sel, t_id], [tp[1]])
                b.copy(SelT[:, sb_, :], tp[0][:].rearrange("p n c -> p (n c)"), [tp[1]], [t_selT[sb_], tp[1]])
            w2q = {}
            for fc in range(NFC):
                wt, t_w, k_w = w13r.next()
                b.dma("sp", wt[:].rearrange("p a j f -> p (a j f)"), w13[e_, fc], writes=[t_w], key=k_w)
                if fc in (2, 4):
                    q = 0 if fc == 2 else 1
                    w2q[q] = w2r.next()
                    b.dma("pool", w2q[q][0][:].rearrange("p c n -> p (c n)"), w2[e_, q], writes=[w2q[q][1]],
                          key=w2q[q][2])
                pa, t_pa = psr.next()
                pg, t_pg = psr.next()
                b.mm(pa[:, 0:CAP], [(wt[:, kc, 0, :], xg[:, kc, :]) for kc in range(8)], [t_w, t_xg], [t_pa])
                b.mm(pg[:, 0:CAP], [(wt[:, kc, 1, :], xg[:, kc, :]) for kc in range(8)], [t_w, t_xg], [t_pg])
                sa, t_sa = sar.next()
                b.act(sa[:], pa[:, 0:CAP], AF.Silu, [t_pa], [t_sa, t_pa])
                b.tt(hT[:, fc, :], pg[:, 0:CAP], sa[:], ALU.mult, [t_pg, t_sa], [t_hT, t_pg], nowaw=(fc > 0))
            for q in range(4):
                if q >= 2:
                    w2q[q] = w2r.next()
                    b.dma("pool", w2q[q][0][:].rearrange("p c n -> p (c n)"), w2[e_, q], writes=[w2q[q][1]],
                          key=w2q[q][2])
                wq, t_wq, _ = w2q[q]
                for sb_ in range(NSB):
                    py, t_py = psr.next()
                    b.mm(py[:, 0:256], [(hT[:, fc, sb_ * 128:(sb_ + 1) * 128], wq[:, fc, :]) for fc in range(NFC)],
                         [t_hT, t_wq], [t_py])
                    b.act(oe[:, sb_, q * 256:(q + 1) * 256], py[:, 0:256], AF.Copy, [t_py], [t_oe[sb_], t_py],
                          )
            for ti in range(NT):
                tt = half * NT + ti
                for nh in range(2):
                    pc, t_pc = pcr.next()
                    b.mm(pc[:, :], [(SelT[:, sb_, ti * 128:(ti + 1) * 128], oe[:, sb_, nh * 512:(nh + 1) * 512])
                                    for sb_ in range(NSB)], t_selT + t_oe, [t_pc])
                    dst = acc[:, ti, nh * 512:(nh + 1) * 512]
                    g = gates[:, tt, e_:e_ + 1]
                    if e_ == 0:
                        b.ts(dst, pc[:, :], g, ALU.mult, [t_pc, t_gate[tt]], [t_acc[ti], t_pc], nowaw=(nh > 0))
                    else:
                        b.stt(dst, pc[:, :], g, dst, ALU.mult, ALU.add, [t_pc, t_gate[tt], t_acc[ti]],
                              [t_acc[ti], t_pc])
        for ti in range(NT):
            tt = half * NT + ti
            rows = slice(tt * 128, (tt + 1) * 128)
            xt, t_xt, k_xt = xtr.next()
            b.dma("sp", xt[:], x_tok[rows, :], writes=[t_xt], key=k_xt)
            b.stt(xt[:], xt[:], ALPHA, acc[:, ti, :], ALU.mult, ALU.add, [t_xt, t_acc[ti]], [t_xt])
            ot, t_ot, k_ot = otr.next()
            ln_tile2(b, xt, t_xt, ot, t_ot, lng, lnb, eps, t_par, lns)
            b.dma("sp", x2[rows, :], ot[:], reads=[t_ot], key=k_ot)
    b.dma("sp", cnt_out[:, :], cntsb[0:1, :], reads=[t_cnt], key="stc")
    return b.finish()


def run_FS(x1, inp, l):
    j = l // 2
    w13b, w2b = run_W(inp, j)
    a = np.arange(128)
    com = {"lng_b": bcast128(inp["ln_g"][l, 1]), "lnb_b": bcast128(inp["ln_b"][l, 1]),
           "eps": np.full((128, 1), LN_EPS, np.float32),
           "rw": np.ascontiguousarray(inp["router_w"][j]), "rb_b": bcast128(inp["router_b"][j]),
           "w13e": w13b.reshape(N_EXP, D_FF_E // 128, 128, 2048),
           "w2e": w2b.reshape(N_EXP, 4, 128, (D_FF_E // 128) * 256),
           "ones": np.ones((128, 128), np.float32),
           "ustrict": (a[:, None] < a[None, :]).astype(np.float32),
           "iota": bcast128(np.arange(CAP, dtype=np.float32)),
           "ident": np.eye(128, dtype=np.float32)}
    nc = build_FS()
    maps = []
    for c in range(NCORES):
        sl = slice(c * TPC, (c + 1) * TPC)
        m = dict(com)
        m["xT"] = np.ascontiguousarray(x1[sl].T)
        m["x_tok"] = np.ascontiguousarray(x1[sl])
        maps.append(m)
    res = run_bass_kernel_spmd(nc, maps, core_ids=list(range(NCORES))).results
    x2 = np.concatenate([np.asarray(res[c]["x2"]) for c in range(NCORES)], 0)
    cnt = max(float(np.asarray(res[c]["cnt"]).max()) for c in range(NCORES))
    return x2, cnt


def _tok(res, name):
    return np.ascontiguousarray(np.concatenate([np.asarray(res[c][name]) for c in range(NCORES)], axis=1).T)


def kernel(**inp):
    inp = {k: np.asarray(v) for k, v in inp.items()}
    x = np.ascontiguousarray(inp["x"][0], dtype=np.float32)
    for l in range(DEPTH):
        rp = run_P(make_xT_ext(x), inp, l)
        parts = run_A(_tok(rp, "qaT"), _tok(rp, "kaT"), _tok(rp, "vaT"), inp["rel_bias"])
        h_raw = run_M(_tok(rp, "qmT"), _tok(rp, "kmT"), _tok(rp, "vmT"), _tok(rp, "ipre"), _tok(rp, "logf"))
        x1 = run_D1(x, parts, h_raw, _tok(rp, "sigzT"), inp, l)
        if l % 2 == 0:
            x = run_F(x1, inp, l)
        else:
            x2, cnt = run_FS(x1, inp, l)
            x = x2 if cnt <= CAP else run_F(x1, inp, l)
    return x[None].astype(np.float32)
```

```python
import contextlib
import numpy as np
import ml_dtypes
import concourse.bass as bass
import concourse.mybir as mybir
from concourse.bass_utils import run_bass_kernel_spmd

F32 = mybir.dt.float32
BF = mybir.dt.bfloat16
AF = mybir.ActivationFunctionType
ALU = mybir.AluOpType
BF_NP = ml_dtypes.bfloat16

NCORES = 8
S = 16384
D = 1024
TPC = S // NCORES
DEPTH = 2
ALPHA = (2.0 * DEPTH) ** 0.25
LN_EPS = 1e-5
D_FF = 2816
D_FF_E = 3584
N_EXP = 8
TRUNC = None


class Tile:
    __slots__ = ("name", "w", "r")

    def __init__(self, name=""):
        self.name = name
        self.w = None
        self.r = []


class Op:
    __slots__ = ("eng", "fn", "deps", "signal", "sem", "val", "dma", "idx")


class Prog:
    ENGS = ("pe", "act", "dve", "pool", "sp")

    def __init__(self, nc):
        self.nc = nc
        self.ops = []

    def op(self, eng, fn, reads=(), writes=(), dma=None, nowaw=False):
        o = Op()
        o.eng, o.fn, o.dma, o.signal = eng, fn, dma, dma is not None
        o.idx = len(self.ops)
        deps = set()
        for t in reads:
            if t.w is not None:
                deps.add(t.w)
        for t in writes:
            if t.w is not None and not nowaw:
                deps.add(t.w)
            deps.update(t.r)
        deps.discard(o.idx)
        o.deps = deps
        self.ops.append(o)
        for t in reads:
            t.r.append(o.idx)
        for t in writes:
            t.w = o.idx
            t.r = []
        return o

    def emit(self, final_wait_eng="sp"):
        nc = self.nc
        if TRUNC is not None:
            self.ops = self.ops[:TRUNC]
        ops = self.ops
        for o in ops:
            for d in o.deps:
                ops[d].signal = True
        with contextlib.ExitStack() as st:
            esem = {e: st.enter_context(nc.semaphore("s_" + e)) for e in self.ENGS}
            dkeys = sorted({o.dma for o in ops if o.dma is not None})
            dsem = {k: st.enter_context(nc.semaphore("d_" + str(k))) for k in dkeys}
            cnt = {}
            for o in ops:
                if o.dma is not None:
                    cnt[("d", o.dma)] = cnt.get(("d", o.dma), 0) + 16
                    o.sem, o.val = dsem[o.dma], cnt[("d", o.dma)]
                elif o.signal:
                    cnt[o.eng] = cnt.get(o.eng, 0) + 1
                    o.sem, o.val = esem[o.eng], cnt[o.eng]
                else:
                    o.sem, o.val = None, None
            finals = [(dsem[k], cnt[("d", k)]) for k in dkeys]
            block = st.enter_context(nc.Block())
            per_eng = {e: [o for o in ops if o.eng == e] for e in self.ENGS}

            def run(e, engobj):
                seen = {}
                for o in per_eng[e]:
                    need = {}
                    for d in o.deps:
                        do = ops[d]
                        key = id(do.sem)
                        if seen.get(key, 0) >= do.val:
                            continue
                        if key not in need or need[key][1] < do.val:
                            need[key] = (do.sem, do.val)
                    for key, (s, v) in need.items():
                        engobj.wait_ge(s, v)
                        seen[key] = v
                    ins = o.fn(engobj)
                    if o.signal:
                        ins.then_inc(o.sem, 16 if o.dma is not None else 1)
                if e == final_wait_eng:
                    for s, v in finals:
                        engobj.wait_ge(s, v)

            @block.tensor
            def _(eng):
                run("pe", eng)

            @block.scalar
            def _(eng):
                run("act", eng)

            @block.vector
            def _(eng):
                run("dve", eng)

            @block.gpsimd
            def _(eng):
                run("pool", eng)

            @block.sync
            def _(eng):
                run("sp", eng)


class Builder:
    def __init__(self):
        self.nc = bass.Bass("TRN2", target_bir_lowering=False)
        self.P = Prog(self.nc)
        self.st = contextlib.ExitStack()
        self.n = 0

    def din(self, name, shape, dt=F32):
        return self.nc.dram_tensor(name, list(shape), dt, kind="ExternalInput").ap()

    def dout(self, name, shape, dt=F32):
        return self.nc.dram_tensor(name, list(shape), dt, kind="ExternalOutput").ap()

    def sb(self, shape, dt, name=None):
        self.n += 1
        return self.st.enter_context(self.nc.sbuf_tensor(name or f"sb{self.n}", list(shape), dt))

    def ps(self, shape, dt=F32, name=None):
        self.n += 1
        return self.st.enter_context(self.nc.psum_tensor(name or f"ps{self.n}", list(shape), dt))

    def dma(self, q, out, in_, reads=(), writes=(), key="ld", nowaw=False):
        kw = {"max_dma_last_dim": 4096} if q == "pool" else {}
        self.P.op(q, lambda e: e.dma_start(out=out, in_=in_, **kw), reads, writes, dma=key, nowaw=nowaw)

    def mm(self, out, pairs, reads, writes):
        def fn(e):
            n = len(pairs)
            for i, (l, r) in enumerate(pairs):
                ins = e.matmul(out, lhsT=l, rhs=r, start=(i == 0), stop=(i == n - 1))
            return ins
        self.P.op("pe", fn, reads, writes)

    def pe(self, fn, reads, writes):
        self.P.op("pe", fn, reads, writes)

    def act(self, out, in_, func, reads, writes, bias=None, scale=None):
        kw = {}
        if bias is not None:
            kw["bias"] = bias
        if scale is not None:
            kw["scale"] = scale
        self.P.op("act", lambda e: e.activation(out=out, in_=in_, func=func, **kw), reads, writes)

    def ts(self, out, in0, s1, op0, reads, writes, s2=None, op1=None, eng="dve", nowaw=False):
        kw = {}
        if op1 is not None:
            kw["op1"] = op1
        self.P.op(eng, lambda e: e.tensor_scalar(out=out, in0=in0, scalar1=s1, scalar2=s2, op0=op0, **kw),
                  reads, writes, nowaw=nowaw)

    def stt(self, out, in0, scalar, in1, op0, op1, reads, writes):
        self.P.op("dve", lambda e: e.scalar_tensor_tensor(out=out, in0=in0, scalar=scalar, in1=in1,
                                                          op0=op0, op1=op1), reads, writes)

    def tt(self, out, in0, in1, op, reads, writes, eng="dve", nowaw=False):
        self.P.op(eng, lambda e: e.tensor_tensor(out=out, in0=in0, in1=in1, op=op), reads, writes, nowaw=nowaw)

    def copy(self, out, in_, reads, writes, eng="dve"):
        self.P.op(eng, lambda e: e.tensor_copy(out=out, in_=in_), reads, writes)

    def finish(self):
        self.P.emit()
        self.st.close()
        return self.nc


class Rot:
    def __init__(self, items):
        self.items = items
        self.i = 0

    def next(self):
        it = self.items[self.i % len(self.items)]
        self.i += 1
        return it


HALO = 128
TG = 512


def build_P():
    b = Builder()
    nc = b.nc
    W = TPC + HALO
    xT = b.din("xT", [D, W])
    w_in = b.din("w_in", [D, 2560])
    convw = b.din("convw", [128, 16])
    convb = b.din("convb", [128, 4])
    wqk = b.din("wqk", [128, 8 * 128])
    wv = b.din("wv", [128, 4 * 128])
    wif = b.din("wif", [128, 12 * 8])
    bif = b.din("bif", [4, 2])
    o_qa = b.dout("qaT", [512, TPC], BF)
    o_ka = b.dout("kaT", [512, TPC], BF)
    o_va = b.dout("vaT", [512, TPC], BF)
    o_sz = b.dout("sigzT", [512, TPC], F32)
    o_qm = b.dout("qmT", [512, TPC], BF)
    o_km = b.dout("kmT", [512, TPC], BF)
    o_vm = b.dout("vmT", [512, TPC], BF)
    o_ip = b.dout("ipre", [4, TPC], F32)
    o_lf = b.dout("logf", [4, TPC], F32)

    xb = b.sb([128, 8, W], BF)
    t_xb = [Tile() for _ in range(8)]
    xTv = xT.rearrange("(kc p) t -> p kc t", p=128)
    for kc in range(8):
        b.dma("pool", xb[:, kc, :], xTv[:, kc, :], writes=[t_xb[kc]], key="ldx")
    cw = b.sb([128, 16], F32)
    cb = b.sb([128, 4], F32)
    wqk_f = b.sb([128, 1024], BF)
    wv_f = b.sb([128, 512], BF)
    wif_b = b.sb([128, 96], BF)
    bif_s = b.sb([4, 2], F32)
    t_par = Tile()
    b.dma("sp", cw[:], convw[:, :], writes=[t_par], key="ldp", nowaw=True)
    b.dma("sp", cb[:], convb[:, :], writes=[t_par], key="ldp", nowaw=True)
    b.dma("sp", bif_s[:], bif[:, :], writes=[t_par], key="ldp", nowaw=True)
    t_wq = Tile()
    b.dma("pool", wqk_f[:], wqk[:, :], writes=[t_wq], key="ldw2", nowaw=True)
    b.dma("pool", wv_f[:], wv[:, :], writes=[t_wq], key="ldw2", nowaw=True)
    b.dma("pool", wif_b[:], wif[:, :], writes=[t_wq], key="ldw2", nowaw=True)

    wring = Rot([(b.sb([128, 8, 512], BF), Tile(), f"w{i}") for i in range(3)])
    w_inv = w_in.rearrange("(kc p) n -> p kc n", p=128)

    psr = Rot([(b.ps([128, 512]), Tile()) for _ in range(4)])
    st_bf = Rot([(b.sb([128, 512], BF), Tile(), f"sb{i}") for i in range(4)])
    st_f = Rot([(b.sb([128, 512], F32), Tile(), f"sf{i}") for i in range(3)])

    xm = b.sb([128, 4, W], F32)
    t_xm = [[Tile() for _ in range(5)] for _ in range(4)]

    outs_bf = {0: (o_qa, 0.125), 1: (o_ka, None), 2: (o_va, None)}
    for blk in range(5):
        wt, t_w, wkey = wring.next()
        b.dma("pool", wt[:], w_inv[:, :, blk * 512:(blk + 1) * 512], writes=[t_w], key=wkey)
        for cc in range(4):
            groups = [(HALO + g * TG, TG, g + 1) for g in range(4)]
            if blk == 3:
                groups = [(0, HALO, 0)] + groups
            for (c0, n, gi) in groups:
                pt, t_p = psr.next()
                b.mm(pt[:, 0:n], [(wt[:, kc, cc * 128:(cc + 1) * 128], xb[:, kc, c0:c0 + n]) for kc in range(8)],
                     reads=[t_w] + t_xb, writes=[t_p])
                if blk in outs_bf:
                    dst, sc = outs_bf[blk]
                    s, t_s, skey = st_bf.next()
                    b.act(s[:, 0:n], pt[:, 0:n], AF.Copy, [t_p], [t_s], scale=sc)
                    b.dma("sp", dst[cc * 128:(cc + 1) * 128, c0 - HALO:c0 - HALO + n], s[:, 0:n],
                          reads=[t_s], key=skey)
                elif blk == 3:
                    b.copy(xm[:, cc, c0:c0 + n], pt[:, 0:n], [t_p], [t_xm[cc][gi]])
                else:
                    s, t_s, skey = st_f.next()
                    b.act(s[:, 0:n], pt[:, 0:n], AF.Sigmoid, [t_p], [t_s])
                    b.dma("sp", o_sz[cc * 128:(cc + 1) * 128, c0 - HALO:c0 - HALO + n], s[:, 0:n],
                          reads=[t_s], key=skey)

    xc = b.sb([128, 4, TPC], BF)
    xmb = b.sb([128, 4, TPC], BF)
    acc = [(b.sb([128, TPC], F32), Tile()) for _ in range(2)]
    t_xc = [Tile() for _ in range(4)]
    t_xmb = [Tile() for _ in range(4)]
    for ch in range(4):
        a, t_a = acc[ch % 2]
        rd = t_xm[ch] + [t_par]
        b.ts(a[:], xm[:, ch, HALO - 3:HALO - 3 + TPC], cw[:, ch * 4:ch * 4 + 1], ALU.mult, rd, [t_a])
        for j in range(1, 4):
            b.stt(a[:], xm[:, ch, HALO - 3 + j:HALO - 3 + j + TPC], cw[:, ch * 4 + j:ch * 4 + j + 1], a[:],
                  ALU.mult, ALU.add, rd + [t_a], [t_a])
        b.act(xc[:, ch, :], a[:], AF.Silu, [t_a, t_par], [t_xc[ch]], bias=cb[:, ch:ch + 1])
        b.copy(xmb[:, ch, :], xm[:, ch, HALO:HALO + TPC], t_xm[ch], [t_xmb[ch]], eng="pool")

    qkv = [(b.sb([128, 12, TG], BF), Tile()) for _ in range(2)]
    psg = [(b.ps([4, TG]), Tile()) for _ in range(2)]
    g_f = b.sb([4, TG], F32)
    g_e = b.sb([4, TG], F32)
    g_l = b.sb([4, TG], F32)
    g_i = b.sb([4, TG], F32)
    t_gf, t_ge, t_gl, t_gi = Tile(), Tile(), Tile(), Tile()
    SC_M = 128.0 ** -0.5
    for g in range(4):
        c0 = g * TG
        qt, t_q = qkv[g % 2]
        for which in range(3):
            for h in range(4):
                pt, t_p = psr.next()
                if which < 2:
                    lhsT = wqk_f[:, (which * 4 + h) * 128:(which * 4 + h + 1) * 128]
                    rhs = xc[:, h, c0:c0 + TG]
                    rd = [t_wq, t_xc[h]]
                else:
                    lhsT = wv_f[:, h * 128:(h + 1) * 128]
                    rhs = xmb[:, h, c0:c0 + TG]
                    rd = [t_wq, t_xmb[h]]
                b.mm(pt[:, :], [(lhsT, rhs)], rd, [t_p])
                b.act(qt[:, which * 4 + h, :], pt[:, :], AF.Copy, [t_p], [t_q])
                s, t_s, skey = st_bf.next()
                if which == 0:
                    b.ts(s[:, :], qt[:, which * 4 + h, :], SC_M, ALU.mult, [t_q], [t_s])
                else:
                    b.copy(s[:, :], qt[:, which * 4 + h, :], [t_q], [t_s])
                dst = (o_qm, o_km, o_vm)[which]
                b.dma("sp", dst[h * 128:(h + 1) * 128, c0:c0 + TG], s[:, :], reads=[t_s], key=skey)
        pi, t_pi = psg[0]
        pf, t_pf = psg[1]
        b.mm(pi[:, :], [(wif_b[:, j * 8:j * 8 + 4], qt[:, j, :]) for j in range(12)], [t_wq, t_q], [t_pi])
        b.mm(pf[:, :], [(wif_b[:, j * 8 + 4:j * 8 + 8], qt[:, j, :]) for j in range(12)], [t_wq, t_q], [t_pf])
        b.act(g_i[:], pi[:, :], AF.Identity, [t_pi, t_par], [t_gi], bias=bif_s[:, 0:1])
        b.dma("sp", o_ip[:, c0:c0 + TG], g_i[:], reads=[t_gi], key="sgi")
        b.act(g_f[:], pf[:, :], AF.Identity, [t_pf, t_par], [t_gf], bias=bif_s[:, 1:2])
        b.act(g_e[:], g_f[:], AF.Exp, [t_gf], [t_ge], scale=-1.0)
        b.act(g_l[:], g_e[:], AF.Ln, [t_ge], [t_gl], bias=1.0)
        b.ts(g_f[:], g_l[:], -1.0, ALU.mult, [t_gl], [t_gf])
        b.dma("sp", o_lf[:, c0:c0 + TG], g_f[:], reads=[t_gf], key="sgf")
    return b.finish()


def run_P(xT_ext_list, inp, l):
    nc = build_P()
    w_in = np.ascontiguousarray(inp["w_in"][l])
    convw = np.ascontiguousarray(inp["conv_w"][l].T.reshape(4, 128, 4).transpose(1, 0, 2).reshape(128, 16))
    convb = np.ascontiguousarray(inp["conv_b"][l].reshape(4, 128).T)
    wqk = np.ascontiguousarray(inp["w_qk_m"][l].reshape(8, 128, 128).transpose(1, 0, 2).reshape(128, 1024))
    wv = np.ascontiguousarray(inp["w_v_m"][l].transpose(1, 0, 2).reshape(128, 512))
    wif = np.ascontiguousarray(inp["w_if"][l].reshape(12, 128, 8).transpose(1, 0, 2).reshape(128, 96))
    bif = np.ascontiguousarray(inp["b_if"][l].reshape(2, 4).T)
    maps = [{"xT": xT_ext_list[c], "w_in": w_in, "convw": convw, "convb": convb, "wqk": wqk, "wv": wv,
             "wif": wif, "bif": bif} for c in range(NCORES)]
    res = run_bass_kernel_spmd(nc, maps, core_ids=list(range(NCORES)))
    return res.results


def make_xT_ext(x_tok):
    out = []
    for c in range(NCORES):
        a = np.zeros((D, HALO + TPC), np.float32)
        lo = c * TPC - HALO
        if c == 0:
            a[:, HALO:] = x_tok[0:TPC].T
        else:
            a[:, :] = x_tok[lo:lo + HALO + TPC].T
        out.append(a)
    return out


PATTERNS = (1, 4, 16)
NEG = -30000.0


def a_layout():
    lay = []
    koff = 0
    boff = 0
    for d in PATTERNS:
        nq = TPC // d
        nb = nq // 128
        lay.append(dict(d=d, nq=nq, nb=nb, kcls=128 + nq, koff=koff, boff=boff))
        koff += d * (128 + nq)
        boff += d * (nb + 1)
    return lay, koff, boff


def build_A():
    b = Builder()
    lay, KTOT, NBLK = a_layout()
    qh = b.din("qh", [4, 3, 64, 2 * TPC], BF)
    kh = b.din("kh", [4, 64, 2, KTOT], BF)
    vh = b.din("vh", [4, 128, NBLK * 130], BF)
    bmn = b.din("bmn", [12, 128, 512])
    bmf = b.din("bmf", [12, 128, 512])
    ident_in = b.din("ident", [128, 128])
    oa = b.dout("oa", [3, 4, TPC, 130], F32)

    ident = b.sb([128, 128], BF)
    t_c = Tile()
    b.dma("pool", ident[:], ident_in[:, :], writes=[t_c], key="ldc", nowaw=True)
    bn_sb = b.sb([128, 12, 512], BF)
    bf_sb = b.sb([128, 12, 512], BF)
    b.dma("pool", bn_sb[:], bmn.rearrange("n p c -> p n c"), writes=[t_c], key="ldc", nowaw=True)
    b.dma("pool", bf_sb[:], bmf.rearrange("n p c -> p n c"), writes=[t_c], key="ldc", nowaw=True)

    pset = []
    for pi, L in enumerate(lay):
        klen = L["d"] * L["kcls"]
        nblk = L["d"] * (L["nb"] + 1)
        pset.append(dict(q=b.sb([64, 2, TPC], BF), k=b.sb([64, 2, klen], BF), v=b.sb([128, nblk, 130], BF),
                         t=Tile(), key=f"in{pi}", klen=klen, nblk=nblk))
    sbank = Rot([(b.ps([128, 512]), Tile()) for _ in range(3)])
    obank = Rot([(b.ps([128, 512]), Tile()) for _ in range(2)])
    ptr = Rot([(b.sb([128, 512], BF), Tile()) for _ in range(3)])
    ostg = Rot([(b.sb([128, 16, 130], F32), Tile(), f"os{i}") for i in range(2)])

    for hp in range(4):
        for pi, L in enumerate(lay):
            d, nq, nb = L["d"], L["nq"], L["nb"]
            s = pset[pi]
            b.dma("sp", s["q"][:].rearrange("p h t -> p (h t)"), qh[hp, pi], writes=[s["t"]], key=s["key"])
            b.dma("sp", s["k"][:], kh[hp][:, :, L["koff"]:L["koff"] + s["klen"]], writes=[s["t"]], key=s["key"],
                  nowaw=True)
            b.dma("sp", s["v"][:].rearrange("p n c -> p (n c)"),
                  vh[hp][:, L["boff"] * 130:(L["boff"] + s["nblk"]) * 130], writes=[s["t"]], key=s["key"], nowaw=True)
            og, t_og, okey = ostg.next()
            for r in range(d):
                for qb in range(1, nb + 1):
                    qpos = r * nq + (qb - 1) * 128
                    sb_, t_sb = sbank.next()
                    tab = bf_sb if qb == 1 else bn_sb
                    n12 = pi * 4 + hp

                    def fn(e, sb_=sb_, tab=tab, n12=n12, s=s, L=L, r=r, qb=qb, qpos=qpos, pi=pi):
                        e.matmul(sb_[:, :], lhsT=ident[:], rhs=tab[:, n12, :], start=True, stop=False,
                                 skip_group_check=True)
                        for hh in range(2):
                            for w in range(2):
                                kpos = r * L["kcls"] + (qb - 1 + w) * 128
                                c0 = (hh * 2 + w) * 128
                                ins = e.matmul(sb_[:, c0:c0 + 128],
                                               lhsT=s["k"][:, hh, kpos:kpos + 128],
                                               rhs=s["q"][:, hh, qpos:qpos + 128],
                                               start=False, stop=True, skip_group_check=True)
                        return ins
                    b.pe(fn, [t_c, s["t"]], [t_sb])
                    pt, t_pt = ptr.next()
                    b.act(pt[:, :], sb_[:, :], AF.Exp, [t_sb], [t_pt, t_sb])
                    ob, t_ob = obank.next()

                    def fn2(e, ob=ob, pt=pt, s=s, L=L, r=r, qb=qb):
                        for hh in range(2):
                            for w in range(2):
                                blk = r * (L["nb"] + 1) + (qb - 1 + w)
                                c0 = (hh * 2 + w) * 128
                                ins = e.matmul(ob[:, hh * 65:hh * 65 + 65], lhsT=pt[:, c0:c0 + 128],
                                               rhs=s["v"][:, blk, hh * 65:hh * 65 + 65],
                                               start=(w == 0), stop=(w == 1))
                        return ins
                    b.pe(fn2, [t_pt, s["t"]], [t_ob])
                    b.copy(og[:, qpos // 128, :], ob[:, 0:130], [t_ob], [t_og, t_ob])
            b.dma("sp", oa[pi, hp].rearrange("(n q) c -> q n c", q=128), og[:], reads=[t_og], key=okey)
    return b.finish()


def _bucket(dist):
    dist = np.asarray(dist, np.int64)
    dd = np.maximum(dist, 16).astype(np.float32)
    lb = 16 + (np.log(dd / np.float32(16)) / np.float32(np.log(2048.0 / 16.0)) * np.float32(16)).astype(np.int32)
    return np.where(dist < 16, dist, np.minimum(lb, 31))


def a_bias_tables(rel_bias):
    k = np.arange(128)[:, None]
    q = np.arange(128)[None, :]
    tn = np.full((3, 4, 128, 2, 2, 128), NEG, np.float32)
    for pi, d in enumerate(PATTERNS):
        for w in range(2):
            rel = q + 128 - (k + 128 * w)
            valid = (rel >= 0) & (rel <= 128)
            bk = _bucket(np.maximum(rel, 0) * d)
            for h in range(8):
                vals = rel_bias[bk, h]
                tn[pi, h // 2, :, h % 2, w, :] = np.where(valid, vals, NEG)
    tf = tn.copy()
    tf[:, :, :, :, 0, :] = NEG
    return tn.reshape(12, 128, 512), tf.reshape(12, 128, 512)


def a_indices(core):
    lay, KTOT, NBLK = a_layout()
    base = core * TPC
    qidx, kidx = [], []
    for L in lay:
        d, nq = L["d"], L["nq"]
        qi = np.concatenate([base + r + d * np.arange(nq) for r in range(d)])
        ki = np.concatenate([base + r + d * (np.arange(128 + nq) - 128) for r in range(d)])
        qidx.append(qi)
        kidx.append(np.where(ki < 0, -1, ki))
    return qidx, np.concatenate(kidx)


def run_A(qa, ka, va, rel_bias):
    nc = build_A()
    tn, tf = a_bias_tables(rel_bias)
    ident = np.eye(128, dtype=np.float32)
    kz = np.concatenate([ka, np.zeros((1, 512), ka.dtype)], 0)
    vz = np.concatenate([va, np.zeros((1, 512), va.dtype)], 0)
    maps = []
    qidx_all = []
    for c in range(NCORES):
        qidx, kidx = a_indices(c)
        qidx_all.append(qidx)
        qh = np.stack([np.stack([qa[qi][:, hp * 128:(hp + 1) * 128].reshape(-1, 2, 64).transpose(2, 1, 0).reshape(64, -1)
                                 for qi in qidx]) for hp in range(4)])
        kg = kz[kidx]
        kh = np.stack([kg[:, hp * 128:(hp + 1) * 128].reshape(-1, 2, 64).transpose(2, 1, 0) for hp in range(4)])
        vg = vz[kidx].reshape(-1, 128, 4, 2, 64)
        ve = np.concatenate([vg, np.ones(vg.shape[:-1] + (1,), vg.dtype)], -1)
        vh = np.ascontiguousarray(ve.transpose(2, 1, 0, 3, 4)).reshape(4, 128, -1)
        maps.append({"qh": np.ascontiguousarray(qh), "kh": np.ascontiguousarray(kh), "vh": vh,
                     "bmn": tn, "bmf": tf if c == 0 else tn, "ident": ident})
    res = run_bass_kernel_spmd(nc, maps, core_ids=list(range(NCORES))).results
    out = np.zeros((3, S, 4, 130), np.float32)
    for c in range(NCORES):
        o = np.asarray(res[c]["oa"])
        for pi in range(3):
            out[pi, qidx_all[c][pi]] = o[pi].transpose(1, 0, 2)
    return out.reshape(3, S, 8, 65)


NCH = S // 128


def build_M():
    b = Builder()
    qT = b.din("qT", [128, S], BF)
    kT = b.din("kT", [128, S], BF)
    ktok = b.din("ktok", [128, NCH * 128], BF)
    vext = b.din("vext", [128, NCH * 65], BF)
    ipre = b.din("ipre", [128, NCH])
    logf = b.din("logf", [128, NCH])
    U_in = b.din("U", [128, 128])
    NG_in = b.din("NEGM", [128, 128])
    on_in = b.din("ones", [128, 128])
    id_in = b.din("identf", [128, 128])
    ho = b.dout("ho", [128, NCH * 64], F32)

    U = b.sb([128, 128], F32)
    NG = b.sb([128, 128], F32)
    ON = b.sb([128, 128], F32)
    IDF = b.sb([128, 128], F32)
    ip_sb = b.sb([128, NCH], F32)
    lf_sb = b.sb([128, NCH], F32)
    t_c = Tile()
    for dst, src in ((U, U_in), (NG, NG_in), (ON, on_in), (IDF, id_in), (ip_sb, ipre), (lf_sb, logf)):
        b.dma("sp", dst[:], src[:, :], writes=[t_c], key="ldc", nowaw=True)
    q_sb = b.sb([128, S], BF)
    k_sb = b.sb([128, S], BF)
    kt_sb = b.sb([128, NCH, 128], BF)
    v_sb = b.sb([128, NCH, 65], BF)
    NPC = 4
    CPP = NCH // NPC
    t_in = [Tile() for _ in range(NPC)]
    for pc in range(NPC):
        key = f"in{pc}"
        c0, c1 = pc * CPP, (pc + 1) * CPP
        b.dma("sp", q_sb[:, c0 * 128:c1 * 128], qT[:, c0 * 128:c1 * 128], writes=[t_in[pc]], key=key, nowaw=True)
        b.dma("sp", k_sb[:, c0 * 128:c1 * 128], kT[:, c0 * 128:c1 * 128], writes=[t_in[pc]], key=key, nowaw=True)
        b.dma("sp", kt_sb[:, c0:c1, :].rearrange("p n c -> p (n c)"), ktok[:, c0 * 128:c1 * 128],
              writes=[t_in[pc]], key=key, nowaw=True)
        b.dma("sp", v_sb[:, c0:c1, :].rearrange("p n c -> p (n c)"), vext[:, c0 * 65:c1 * 65],
              writes=[t_in[pc]], key=key, nowaw=True)

    Bmr = Rot([(b.ps([128, 512]), Tile()) for _ in range(2)])
    ps_b = Bmr.items[0]
    bcol = b.sb([128, NCH], F32)
    imb = b.sb([128, NCH], F32)
    eb = b.sb([128, NCH], F32)
    t_bcol, t_imb, t_eb = Tile(), Tile(), Tile()
    b.mm(ps_b[0][:, 0:NCH], [(U[:], lf_sb[:])], [t_c], [ps_b[1]])
    b.copy(bcol[:], ps_b[0][:, 0:NCH], [ps_b[1]], [t_bcol, ps_b[1]])
    b.tt(imb[:], ip_sb[:], bcol[:], ALU.subtract, [t_c, t_bcol], [t_imb])
    b.act(eb[:], bcol[:], AF.Exp, [t_bcol], [t_eb])

    C = b.sb([128, 65], F32)
    Cb = b.sb([128, 65], BF)
    t_C, t_Cb = Tile(), Tile()
    b.P.op("dve", lambda e: e.memset(C[:], 0.0), (), [t_C])
    b.P.op("dve", lambda e: e.memset(Cb[:], 0.0), (), [t_Cb])

    def rot(n, shape, dt):
        return Rot([(b.sb(shape, dt), Tile()) for _ in range(n)])
    LUr = rot(2, [128, 128], F32)
    DTr = rot(2, [128, 128], F32)
    dcr = rot(2, [128, 1], F32)
    SWr = rot(2, [128, 128], BF)
    hir = rot(2, [128, 65], F32)
    Htr = rot(2, [128, 65], F32)
    denr = rot(2, [128, 1], F32)
    rdr = rot(2, [128, 1], F32)
    kwr = rot(2, [128, 128], BF)
    STr = Rot([(b.ps([128, 512]), Tile()) for _ in range(1)])
    Hi = (b.ps([128, 512]), Tile())
    Her = Rot([(b.ps([128, 512]), Tile()) for _ in range(2)])
    dCr = Rot([(b.ps([128, 512]), Tile()) for _ in range(2)])
    GRP = 16
    hout = Rot([(b.sb([128, GRP, 64], F32), Tile(), f"ho{i}") for i in range(2)])
    hcur_of = {}
    stA = {}

    def stage_a(c):
        tin = t_in[c // CPP]
        cs = slice(c * 128, (c + 1) * 128)
        LU, t_LU = LUr.next()
        b.ts(LU[:], U[:], lf_sb[:, c:c + 1], ALU.mult, [t_c], [t_LU])
        Bm, t_Bm = Bmr.next()

        def fnb(e, Bm=Bm, LU=LU):
            e.matmul(Bm[:, 0:128], lhsT=ON[:], rhs=LU[:], start=True, stop=False)
            return e.matmul(Bm[:, 0:128], lhsT=IDF[:], rhs=NG[:], start=False, stop=True)
        b.pe(fnb, [t_c, t_LU], [t_Bm])
        DT, t_DT = DTr.next()
        b.act(DT[:], Bm[:, 0:128], AF.Exp, [t_Bm, t_imb], [t_DT, t_Bm], bias=imb[:, c:c + 1])
        dcol, t_dc = dcr.next()
        b.act(dcol[:], Bm[:, 127:128], AF.Exp, [t_Bm], [t_dc, t_Bm])
        ST, t_ST = STr.next()
        b.mm(ST[:, 0:128], [(k_sb[:, cs], q_sb[:, cs])], [tin], [t_ST])
        SW, t_SW = SWr.next()
        b.tt(SW[:], ST[:, 0:128], DT[:], ALU.mult, [t_ST, t_DT], [t_SW, t_ST])
        b.mm(Hi[0][:, 0:65], [(SW[:], v_sb[:, c, :])], [t_SW, tin], [Hi[1]])
        hi, t_hi = hir.next()
        b.act(hi[:], Hi[0][:, 0:65], AF.Copy, [Hi[1]], [t_hi, Hi[1]])
        kw, t_kw = kwr.next()
        b.ts(kw[:], kt_sb[:, c, :], DT[:, 127:128], ALU.mult, [tin, t_DT], [t_kw])
        dC, t_dC = dCr.next()
        b.mm(dC[:, 0:65], [(kw[:], v_sb[:, c, :])], [t_kw, tin], [t_dC])
        stA[c] = (hi, t_hi, dcol, t_dc, dC, t_dC)

    def stage_b(c):
        tin = t_in[c // CPP]
        cs = slice(c * 128, (c + 1) * 128)
        hi, t_hi, dcol, t_dc, dC, t_dC = stA.pop(c)
        if c % GRP == 0:
            hcur_of[c // GRP] = hout.next()
        hcur = hcur_of[c // GRP]
        He, t_He = Her.next()
        b.mm(He[:, 0:65], [(q_sb[:, cs], Cb[:])], [tin, t_Cb], [t_He])
        b.stt(C[:], C[:], dcol[:, 0:1], dC[:, 0:65], ALU.mult, ALU.add, [t_C, t_dc, t_dC], [t_C, t_dC])
        b.act(Cb[:], C[:], AF.Copy, [t_C], [t_Cb])
        Ht, t_Ht = Htr.next()
        b.stt(Ht[:], He[:, 0:65], eb[:, c:c + 1], hi[:], ALU.mult, ALU.add, [t_He, t_eb, t_hi], [t_Ht, t_He])
        den, t_den = denr.next()
        b.act(den[:], Ht[:, 64:65], AF.Abs, [t_Ht], [t_den])
        b.ts(den[:], den[:], 1.0, ALU.max, [t_den], [t_den])
        rd, t_rd = rdr.next()
        b.P.op("dve", lambda e, rd=rd, den=den: e.reciprocal(out=rd[:], in_=den[:]), [t_den], [t_rd])
        b.ts(hcur[0][:, c % GRP, :], Ht[:, 0:64], rd[:, 0:1], ALU.mult, [t_Ht, t_rd], [hcur[1]])
        if c % GRP == GRP - 1:
            g = c // GRP
            b.dma("sp", ho[:, g * GRP * 64:(g + 1) * GRP * 64], hcur[0][:].rearrange("p n c -> p (n c)"),
                  reads=[hcur[1]], key=hcur[2])

    stage_a(0)
    for c in range(NCH):
        if c + 1 < NCH:
            stage_a(c + 1)
        stage_b(c)
    return b.finish()


def run_M(qm, km, vm, ipre, logf):
    nc = build_M()
    a = np.arange(128)
    U = (a[:, None] <= a[None, :]).astype(np.float32)
    NEGM = np.where(a[:, None] <= a[None, :], 0.0, NEG).astype(np.float32)
    ones = np.ones((128, 128), np.float32)
    identf = np.eye(128, dtype=np.float32)
    maps = []
    for c in range(NCORES):
        h, vh = c // 2, c % 2
        qh = qm[:, h * 128:(h + 1) * 128]
        kh = km[:, h * 128:(h + 1) * 128]
        v = vm[:, h * 128 + vh * 64:h * 128 + vh * 64 + 64].reshape(NCH, 128, 64)
        ve = np.concatenate([v, np.ones((NCH, 128, 1), v.dtype)], -1)
        maps.append({
            "qT": np.ascontiguousarray(qh.T), "kT": np.ascontiguousarray(kh.T),
            "ktok": np.ascontiguousarray(kh.reshape(NCH, 128, 128).transpose(1, 0, 2)).reshape(128, -1),
            "vext": np.ascontiguousarray(ve.transpose(1, 0, 2)).reshape(128, -1),
            "ipre": np.ascontiguousarray(ipre[:, h].reshape(NCH, 128).T),
            "logf": np.ascontiguousarray(logf[:, h].reshape(NCH, 128).T),
            "U": U, "NEGM": NEGM, "ones": ones, "identf": identf})
    res = run_bass_kernel_spmd(nc, maps, core_ids=list(range(NCORES))).results
    out = np.zeros((S, 4, 128), np.float32)
    for c in range(NCORES):
        h, vh = c // 2, c % 2
        o = np.asarray(res[c]["ho"]).reshape(128, NCH, 64)
        out[:, h, vh * 64:(vh + 1) * 64] = o.transpose(1, 0, 2).reshape(S, 64)
    return out


def ln_tile(b, z, t_z, out, t_out, lng, lnb, t_par, scr):
    st, mv, sq, rs = scr
    t_st, t_mv, t_sq, t_rs = Tile(), Tile(), Tile(), Tile()
    b.P.op("dve", lambda e: e.bn_stats(out=st[:, 0:6], in_=z[:, 0:512]), [t_z], [t_st])
    b.P.op("dve", lambda e: e.bn_stats(out=st[:, 6:12], in_=z[:, 512:1024]), [t_z], [t_st], nowaw=True)
    b.P.op("dve", lambda e: e.bn_aggr(out=mv[:, 0:2], in_=st[:, 0:12]), [t_st], [t_mv])
    b.act(sq[:, 0:1], mv[:, 1:2], AF.Sqrt, [t_mv, t_par], [t_sq], bias=EPS_AP[0][:, 0:1])
    b.P.op("dve", lambda e: e.reciprocal(out=rs[:, 0:1], in_=sq[:, 0:1]), [t_sq], [t_rs])
    b.ts(out[:], z[:], mv[:, 0:1], ALU.subtract, [t_z, t_mv, t_rs], [t_out], s2=rs[:, 0:1], op1=ALU.mult)
    b.tt(out[:], out[:], lng[:], ALU.mult, [t_out, t_par], [t_out])
    b.tt(out[:], out[:], lnb[:], ALU.add, [t_out, t_par], [t_out])


EPS_AP = [None]


def build_D1():
    b = Builder()
    xT = b.din("xT", [D, TPC])
    x_tok = b.din("x_tok", [TPC, D])
    oa_tok = b.din("oa_tok", [3, TPC, 520])
    hm_in = b.din("hm", [TPC, 512])
    sz_in = b.din("sigz", [TPC, 512])
    gm_in = b.din("gm_b", [128, 512])
    lng_in = b.din("lng_b", [128, D])
    lnb_in = b.din("lnb_b", [128, D])
    wg_in = b.din("w_gate", [D, 2048])
    bg_in = b.din("bg", [128, 16])
    wbra_in = b.din("w_br_a", [512, D])
    wbrm_in = b.din("w_br_m", [512, D])
    wo_in = b.din("w_o", [D, D])
    id_in = b.din("ident", [128, 128])
    eps_in = b.din("eps", [128, 1])
    x1 = b.dout("x1", [TPC, D], F32)

    t_par = Tile()
    gm = b.sb([128, 512], F32)
    lng = b.sb([128, D], F32)
    lnb = b.sb([128, D], F32)
    bg = b.sb([128, 16], F32)
    eps = b.sb([128, 1], F32)
    EPS_AP[0] = eps
    for dst, src in ((gm, gm_in), (lng, lng_in), (lnb, lnb_in), (bg, bg_in), (eps, eps_in)):
        b.dma("sp", dst[:], src[:, :], writes=[t_par], key="ldp", nowaw=True)
    ident = b.sb([128, 128], BF)
    wbra = b.sb([128, 4, D], BF)
    wbrm = b.sb([128, 4, D], BF)
    wo = b.sb([128, 8, D], BF)
    xb = b.sb([128, 8, TPC], BF)
    t_w = Tile()
    b.dma("pool", ident[:], id_in[:, :], writes=[t_w], key="ldw", nowaw=True)
    t_xb = Tile()
    xTv = xT.rearrange("(kc p) t -> p kc t", p=128)
    for kc in range(8):
        b.dma("pool", xb[:, kc, :], xTv[:, kc, :], writes=[t_xb], key="ldx", nowaw=True)
    b.dma("pool", wbra[:], wbra_in.rearrange("(kc p) n -> p kc n", p=128), writes=[t_w], key="ldw", nowaw=True)
    b.dma("pool", wbrm[:], wbrm_in.rearrange("(kc p) n -> p kc n", p=128), writes=[t_w], key="ldw", nowaw=True)
    wov = wo_in.rearrange("(kc p) n -> p kc n", p=128)
    for kc in range(0, 8, 2):
        b.dma("pool", wo[:, kc:kc + 2, :], wov[:, kc:kc + 2, :], writes=[t_w], key="ldw", nowaw=True)
    wgv = wg_in.rearrange("(kc p) (j n) -> p kc j n", p=128, j=2)
    wgr = Rot([(b.sb([128, 8, 2, 128], BF), Tile(), f"wg{i}") for i in range(2)])

    def rot(n, shape, dt, key=None):
        return Rot([(b.sb(shape, dt), Tile(), (f"{key}{i}" if key else None)) for i in range(n)])
    o3r = rot(1, [128, 3, 520], F32, "o3")
    hmr = rot(2, [128, 512], F32, "hm")
    szr = rot(2, [128, 512], F32, "sz")
    s1 = (b.sb([128, 520], F32), Tile())
    rd = (b.sb([128, 8], F32), Tile())
    yab = (b.sb([128, 512], BF), Tile())
    ymb = (b.sb([128, 512], BF), Tile())
    hn = (b.sb([128, 512], F32), Tile())
    st6 = b.sb([128, 4, 6], F32)
    mv = b.sb([128, 4, 2], F32)
    sq4 = b.sb([128, 4], F32)
    rs4 = b.sb([128, 4], F32)
    t_st6, t_mv, t_sq4, t_rs4 = Tile(), Tile(), Tile(), Tile()
    yTr = Rot([(b.sb([128, 4, TG], BF), b.sb([128, 4, TG], BF), Tile()) for _ in range(2)])
    mgr = Rot([(b.sb([128, 8, TG], BF), Tile()) for _ in range(2)])
    sgr = rot(2, [128, TG], F32)
    t1r = rot(2, [128, TG], F32)
    xtr = rot(2, [128, D], F32, "xt")
    zr = rot(2, [128, D], F32)
    outr = rot(2, [128, D], F32, "ot")
    lscr = (b.sb([128, 12], F32), b.sb([128, 2], F32), b.sb([128, 1], F32), b.sb([128, 1], F32))
    psr = Rot([(b.ps([128, 512]), Tile()) for _ in range(5)])
    tp = (b.ps([128, 8, 128], BF), Tile())
    ybk = Rot([(b.ps([128, 512]), Tile()) for _ in range(2)])

    for tg in range(4):
        yaT, ymT, t_yT = yTr.next()
        for ti in range(4):
            tt = tg * 4 + ti
            rows = slice(tt * 128, (tt + 1) * 128)
            o3, t_o3, k_o3 = o3r.next()
            b.dma("sp", o3[:], oa_tok[:, rows, :].rearrange("n q c -> q n c"), writes=[t_o3], key=k_o3)
            hmt, t_hm, k_hm = hmr.next()
            b.dma("sp", hmt[:], hm_in[rows, :], writes=[t_hm], key=k_hm)
            szt, t_sz, k_sz = szr.next()
            b.dma("sp", szt[:], sz_in[rows, :], writes=[t_sz], key=k_sz)
            b.tt(s1[0][:], o3[:, 0, :], o3[:, 1, :], ALU.add, [t_o3], [s1[1]])
            b.tt(s1[0][:], s1[0][:], o3[:, 2, :], ALU.add, [t_o3, s1[1]], [s1[1]])
            s1v = s1[0][:].rearrange("p (h c) -> p h c", c=65)
            b.P.op("dve", lambda e, s1v=s1v: e.reciprocal(out=rd[0][:], in_=s1v[:, :, 64]), [s1[1]], [rd[1]])
            for h in range(8):
                b.ts(yab[0][:, h * 64:(h + 1) * 64], s1v[:, h, 0:64], rd[0][:, h:h + 1], ALU.mult,
                     [s1[1], rd[1]], [yab[1]])
            for h in range(4):
                b.P.op("dve", lambda e, h=h, hmt=hmt: e.bn_stats(out=st6[:, h, :], in_=hmt[:, h * 128:(h + 1) * 128]),
                       [t_hm], [t_st6])
                b.P.op("dve", lambda e, h=h: e.bn_aggr(out=mv[:, h, :], in_=st6[:, h, :]), [t_st6], [t_mv])
            b.act(sq4[:], mv[:, :, 1], AF.Sqrt, [t_mv, t_par], [t_sq4], bias=eps[:, 0:1])
            b.P.op("dve", lambda e: e.reciprocal(out=rs4[:], in_=sq4[:]), [t_sq4], [t_rs4])
            for h in range(4):
                b.ts(hn[0][:, h * 128:(h + 1) * 128], hmt[:, h * 128:(h + 1) * 128], mv[:, h, 0:1], ALU.subtract,
                     [t_hm, t_mv, t_rs4], [hn[1]], s2=rs4[:, h:h + 1], op1=ALU.mult)
            b.tt(hn[0][:], hn[0][:], gm[:], ALU.mult, [hn[1], t_par], [hn[1]])
            b.tt(ymb[0][:], hn[0][:], szt[:], ALU.mult, [hn[1], t_sz], [ymb[1]])

            def ftp(e):
                for j in range(4):
                    e.transpose(tp[0][:, j, :], yab[0][:, j * 128:(j + 1) * 128], ident[:])
                for j in range(4):
                    ins = e.transpose(tp[0][:, 4 + j, :], ymb[0][:, j * 128:(j + 1) * 128], ident[:])
                return ins
            b.pe(ftp, [yab[1], ymb[1], t_w], [tp[1]])
            cs = slice(ti * 128, (ti + 1) * 128)
            b.copy(yaT[:, :, cs], tp[0][:, 0:4, :], [tp[1]], [t_yT, tp[1]])
            b.act(ymT[:, :, cs], tp[0][:, 4:8, :], AF.Copy, [tp[1]], [t_yT, tp[1]])
        mg, t_mg = mgr.next()
        tcols = slice(tg * TG, (tg + 1) * TG)
        for n in range(8):
            wg, t_wg, k_wg = wgr.next()
            b.dma("pool", wg[:, :, 0, :], wgv[:, :, 0, n * 128:(n + 1) * 128], writes=[t_wg], key=k_wg)
            b.dma("pool", wg[:, :, 1, :], wgv[:, :, 1, n * 128:(n + 1) * 128], writes=[t_wg], key=k_wg, nowaw=True)
            t1, t_t1, _ = t1r.next()
            for j, (wbr, yT) in enumerate(((wbra, yaT), (wbrm, ymT))):
                pa, t_pa = psr.next()
                b.mm(pa[:, :], [(wbr[:, kc, n * 128:(n + 1) * 128], yT[:, kc, :]) for kc in range(4)],
                     [t_w, t_yT], [t_pa])
                ga, t_ga = psr.next()
                b.mm(ga[:, :], [(wg[:, kc, j, :], xb[:, kc, tcols]) for kc in range(8)], [t_wg, t_xb], [t_ga])
                sg, t_sg, _ = sgr.next()
                b.act(sg[:], ga[:, :], AF.Sigmoid, [t_ga, t_par], [t_sg, t_ga], bias=bg[:, j * 8 + n:j * 8 + n + 1])
                if j == 0:
                    b.tt(t1[:], pa[:, :], sg[:], ALU.mult, [t_pa, t_sg], [t_t1, t_pa])
                else:
                    b.tt(sg[:], pa[:, :], sg[:], ALU.mult, [t_pa, t_sg], [t_sg, t_pa])
                    b.tt(mg[:, n, :], t1[:], sg[:], ALU.add, [t_t1, t_sg], [t_mg])
        for ti in range(4):
            tt = tg * 4 + ti
            rows = slice(tt * 128, (tt + 1) * 128)
            xt, t_xt, k_xt = xtr.next()
            b.dma("sp", xt[:], x_tok[rows, :], writes=[t_xt], key=k_xt)
            z, t_z, _ = zr.next()
            for nh in range(2):
                yb, t_yb = ybk.next()
                b.mm(yb[:, :], [(mg[:, kc, ti * 128:(ti + 1) * 128], wo[:, kc, nh * 512:(nh + 1) * 512])
                                for kc in range(8)], [t_mg, t_w], [t_yb])
                b.stt(z[:, nh * 512:(nh + 1) * 512], xt[:, nh * 512:(nh + 1) * 512], ALPHA, yb[:, :],
                      ALU.mult, ALU.add, [t_xt, t_yb], [t_z, t_yb])
            ot, t_ot, k_ot = outr.next()
            ln_tile(b, z, t_z, ot, t_ot, lng, lnb, t_par, lscr)
            b.dma("sp", x1[rows, :], ot[:], reads=[t_ot], key=k_ot)
    return b.finish()


def bcast128(v):
    return np.ascontiguousarray(np.broadcast_to(np.asarray(v, np.float32)[None, :], (128, v.shape[0])))


def run_D1(x_tok, oa_parts, h_raw, sigz_tok, inp, l):
    nc = build_D1()
    com = {"gm_b": bcast128(inp["m_norm_g"][l]), "lng_b": bcast128(inp["ln_g"][l, 0]),
           "lnb_b": bcast128(inp["ln_b"][l, 0]), "w_gate": np.ascontiguousarray(inp["w_gate"][l]),
           "bg": np.ascontiguousarray(inp["b_gate"][l].reshape(16, 128).T),
           "w_br_a": np.ascontiguousarray(inp["w_br_a"][l]), "w_br_m": np.ascontiguousarray(inp["w_br_m"][l]),
           "w_o": np.ascontiguousarray(inp["w_o"][l]), "ident": np.eye(128, dtype=np.float32),
           "eps": np.full((128, 1), LN_EPS, np.float32)}
    maps = []
    for c in range(NCORES):
        sl = slice(c * TPC, (c + 1) * TPC)
        m = dict(com)
        m["xT"] = np.ascontiguousarray(x_tok[sl].T)
        m["x_tok"] = np.ascontiguousarray(x_tok[sl])
        m["oa_tok"] = np.ascontiguousarray(oa_parts[:, sl].reshape(3, TPC, 520))
        m["hm"] = np.ascontiguousarray(h_raw[sl].reshape(TPC, 512))
        m["sigz"] = np.ascontiguousarray(sigz_tok[sl])
        maps.append(m)
    res = run_bass_kernel_spmd(nc, maps, core_ids=list(range(NCORES))).results
    return np.concatenate([np.asarray(res[c]["x1"]) for c in range(NCORES)], 0)


class LNS:
    def __init__(self, b):
        self.st = b.sb([128, 12], F32)
        self.mv = b.sb([128, 2], F32)
        self.sq = b.sb([128, 1], F32)
        self.rs = b.sb([128, 1], F32)
        self.t_st, self.t_mv, self.t_sq, self.t_rs = Tile(), Tile(), Tile(), Tile()


def ln_tile2(b, z, t_z, out, t_out, lng, lnb, eps, t_par, S_):
    b.P.op("dve", lambda e: e.bn_stats(out=S_.st[:, 0:6], in_=z[:, 0:512]), [t_z], [S_.t_st])
    b.P.op("dve", lambda e: e.bn_stats(out=S_.st[:, 6:12], in_=z[:, 512:1024]), [t_z], [S_.t_st], nowaw=True)
    b.P.op("dve", lambda e: e.bn_aggr(out=S_.mv[:, 0:2], in_=S_.st[:, 0:12]), [S_.t_st], [S_.t_mv])
    b.act(S_.sq[:, 0:1], S_.mv[:, 1:2], AF.Sqrt, [S_.t_mv, t_par], [S_.t_sq], bias=eps[:, 0:1])
    b.P.op("dve", lambda e: e.reciprocal(out=S_.rs[:, 0:1], in_=S_.sq[:, 0:1]), [S_.t_sq], [S_.t_rs])
    b.ts(out[:], z[:], S_.mv[:, 0:1], ALU.subtract, [t_z, S_.t_mv, S_.t_rs], [t_out], s2=S_.rs[:, 0:1], op1=ALU.mult)
    b.tt(out[:], out[:], lng[:], ALU.mult, [t_out, t_par], [t_out])
    b.tt(out[:], out[:], lnb[:], ALU.add, [t_out, t_par], [t_out])


def build_F(n_units, cpu, moe):
    b = Builder()
    FU = cpu * 128
    HT = 1024
    xT = b.din("xT", [D, TPC])
    x_tok = b.din("x_tok", [TPC, D])
    w13 = b.din("w13u", [n_units, D, 2, FU])
    w2 = b.din("w2u", [n_units, FU, D])
    lng_in = b.din("lng_b", [128, D])
    lnb_in = b.din("lnb_b", [128, D])
    eps_in = b.din("eps", [128, 1])
    if moe:
        rw_in = b.din("rw", [D, N_EXP])
        rb_in = b.din("rb_b", [128, N_EXP])
        upe = n_units // N_EXP
    x2 = b.dout("x2", [TPC, D], F32)

    t_par = Tile()
    lng = b.sb([128, D], F32)
    lnb = b.sb([128, D], F32)
    eps = b.sb([128, 1], F32)
    par = [(lng, lng_in), (lnb, lnb_in), (eps, eps_in)]
    if moe:
        rb = b.sb([128, N_EXP], F32)
        par.append((rb, rb_in))
        rw = b.sb([128, 8, N_EXP], F32)
    for dst, src in par:
        b.dma("sp", dst[:], src[:, :], writes=[t_par], key="ldp", nowaw=True)
    if moe:
        b.dma("sp", rw[:], rw_in.rearrange("(kc p) e -> p kc e", p=128), writes=[t_par], key="ldp", nowaw=True)

    xb = b.sb([128, 8, HT], BF)
    t_xb = Tile()
    hT = b.sb([128, cpu, HT], BF)
    t_h = [Tile() for _ in range(HT // 512)]
    w2r = Rot([(b.sb([128, cpu, D], BF), Tile(), f"w2{i}") for i in range(2)])
    w13r = Rot([(b.sb([128, 8, 2, 128], BF), Tile(), f"w13{i}") for i in range(3)])
    acc = b.sb([128, HT // 128, D], F32)
    t_acc = [Tile() for _ in range(HT // 128)]
    sar = Rot([(b.sb([128, 512], F32), Tile()) for _ in range(2)])
    xtr = Rot([(b.sb([128, D], F32), Tile(), f"xt{i}") for i in range(2)])
    otr = Rot([(b.sb([128, D], F32), Tile(), f"ot{i}") for i in range(2)])
    lns = LNS(b)
    pa_r = Rot([(b.ps([128, 512]), Tile()) for _ in range(2)])
    pg_r = Rot([(b.ps([128, 512]), Tile()) for _ in range(2)])
    py_r = Rot([(b.ps([128, 512]), Tile()) for _ in range(3)])
    if moe:
        gates = b.sb([128, TPC // 128, N_EXP], F32)
        t_gate = [Tile() for _ in range(TPC // 128)]
        xfr = Rot([(b.sb([128, 8, 128], F32), Tile(), f"xf{i}") for i in range(2)])
        pl = (b.ps([128, 512]), Tile())
        lg = b.sb([128, N_EXP], F32)
        mx = b.sb([128, 8], F32)
        msk = b.sb([128, N_EXP], F32)
        nm1 = b.sb([128, 1], F32)
        ex = b.sb([128, N_EXP], F32)
        den = b.sb([128, 1], F32)
        rden = b.sb([128, 1], F32)
        t_lg, t_mx, t_msk, t_nm1, t_ex, t_den, t_rden = (Tile() for _ in range(7))

    xTv = xT.rearrange("(kc p) t -> p kc t", p=128)
    w13v = w13.rearrange("u (kc p) j f -> u p kc j f", p=128)
    w2v = w2.rearrange("u (fc p) n -> u p fc n", p=128)
    for half in range(TPC // HT):
        t0 = half * HT
        for kc in range(0, 8, 2):
            b.dma("pool", xb[:, kc:kc + 2, :], xTv[:, kc:kc + 2, t0:t0 + HT], writes=[t_xb], key="ldx",
                  nowaw=(kc > 0))
        if moe:
            for ti in range(HT // 128):
                tt = half * (HT // 128) + ti
                xf, t_xf, k_xf = xfr.next()
                b.dma("sp", xf[:], xTv[:, :, tt * 128:(tt + 1) * 128], writes=[t_xf], key=k_xf)
                b.mm(pl[0][:, 0:N_EXP], [(xf[:, kc, :], rw[:, kc, :]) for kc in range(8)], [t_xf, t_par], [pl[1]])
                b.tt(lg[:], pl[0][:, 0:N_EXP], rb[:], ALU.add, [pl[1], t_par], [t_lg, pl[1]])
                b.P.op("dve", lambda e: e.max(out=mx[:], in_=lg[:]), [t_lg], [t_mx])
                b.ts(msk[:], lg[:], mx[:, 1:2], ALU.is_ge, [t_lg, t_mx], [t_msk])
                b.ts(nm1[:], mx[:, 0:1], -1.0, ALU.mult, [t_mx], [t_nm1])
                b.act(ex[:], lg[:], AF.Exp, [t_lg, t_nm1], [t_ex], bias=nm1[:, 0:1])
                b.tt(ex[:], ex[:], msk[:], ALU.mult, [t_ex, t_msk], [t_ex])
                b.P.op("dve", lambda e: e.reduce_sum(out=den[:], in_=ex[:], axis=mybir.AxisListType.X),
                       [t_ex], [t_den])
                b.P.op("dve", lambda e: e.reciprocal(out=rden[:], in_=den[:]), [t_den], [t_rden])
                b.ts(gates[:, tt, :], ex[:], rden[:, 0:1], ALU.mult, [t_ex, t_rden], [t_gate[tt]])
        for u in range(n_units):
            w2t = None
            for fc in range(cpu):
                wt, t_w, k_w = w13r.next()
                b.dma("pool", wt[:, :, 0, :], w13v[u][:, :, 0, fc * 128:(fc + 1) * 128], writes=[t_w], key=k_w)
                b.dma("pool", wt[:, :, 1, :], w13v[u][:, :, 1, fc * 128:(fc + 1) * 128], writes=[t_w], key=k_w,
                      nowaw=True)
                if fc == min(1, cpu - 1):
                    w2t, t_w2, k_w2 = w2r.next()
                    b.dma("pool", w2t[:], w2v[u], writes=[t_w2], key=k_w2)
                for tg in range(HT // 512):
                    cs = slice(tg * 512, (tg + 1) * 512)
                    pa, t_pa = pa_r.next()
                    pg, t_pg = pg_r.next()
                    b.mm(pa[:, :], [(wt[:, kc, 0, :], xb[:, kc, cs]) for kc in range(8)], [t_w, t_xb], [t_pa])
                    b.mm(pg[:, :], [(wt[:, kc, 1, :], xb[:, kc, cs]) for kc in range(8)], [t_w, t_xb], [t_pg])
                    sa, t_sa = sar.next()
                    b.act(sa[:], pa[:, :], AF.Silu, [t_pa], [t_sa, t_pa])
                    b.tt(hT[:, fc, cs], pg[:, :], sa[:], ALU.mult, [t_pg, t_sa], [t_h[tg], t_pg], nowaw=(fc > 0))
            for ti in range(HT // 128):
                tt = half * (HT // 128) + ti
                for nh in range(2):
                    py, t_py = py_r.next()
                    b.mm(py[:, :], [(hT[:, fc, ti * 128:(ti + 1) * 128], w2t[:, fc, nh * 512:(nh + 1) * 512])
                                    for fc in range(cpu)], [t_h[ti // 4], t_w2], [t_py])
                    dst = acc[:, ti, nh * 512:(nh + 1) * 512]
                    if moe:
                        g = gates[:, tt, u // upe:u // upe + 1]
                        rd = [t_py, t_gate[tt]]
                    else:
                        g = 1.0
                        rd = [t_py]
                    if u == 0:
                        b.ts(dst, py[:, :], g, ALU.mult, rd, [t_acc[ti], t_py], nowaw=(nh > 0))
                    else:
                        b.stt(dst, py[:, :], g, dst, ALU.mult, ALU.add, rd + [t_acc[ti]], [t_acc[ti], t_py])
        for ti in range(HT // 128):
            tt = half * (HT // 128) + ti
            rows = slice(tt * 128, (tt + 1) * 128)
            xt, t_xt, k_xt = xtr.next()
            b.dma("sp", xt[:], x_tok[rows, :], writes=[t_xt], key=k_xt)
            b.stt(xt[:], xt[:], ALPHA, acc[:, ti, :], ALU.mult, ALU.add, [t_xt, t_acc[ti]], [t_xt])
            ot, t_ot, k_ot = otr.next()
            ln_tile2(b, xt, t_xt, ot, t_ot, lng, lnb, eps, t_par, lns)
            b.dma("sp", x2[rows, :], ot[:], reads=[t_ot], key=k_ot)
    return b.finish()


def run_F(x1, inp, l):
    j = l // 2
    com = {"lng_b": bcast128(inp["ln_g"][l, 1]), "lnb_b": bcast128(inp["ln_b"][l, 1]),
           "eps": np.full((128, 1), LN_EPS, np.float32)}
    if l % 2 == 0:
        n_units, cpu, moe = 2, 11, False
        w13 = inp["ffn_w13"][j].reshape(D, 2, n_units, cpu * 128).transpose(2, 0, 1, 3)
        w2 = inp["ffn_w2"][j].reshape(n_units, cpu * 128, D)
    else:
        upe, cpu, moe = 4, 7, True
        n_units = N_EXP * upe
        w13 = inp["exp_w13"][j].reshape(N_EXP, D, 2, upe, cpu * 128).transpose(0, 3, 1, 2, 4).reshape(
            n_units, D, 2, cpu * 128)
        w2 = inp["exp_w2"][j].reshape(n_units, cpu * 128, D)
        com["rw"] = np.ascontiguousarray(inp["router_w"][j])
        com["rb_b"] = bcast128(inp["router_b"][j])
    com["w13u"] = np.ascontiguousarray(w13)
    com["w2u"] = np.ascontiguousarray(w2)
    nc = build_F(n_units, cpu, moe)
    maps = []
    for c in range(NCORES):
        sl = slice(c * TPC, (c + 1) * TPC)
        m = dict(com)
        m["xT"] = np.ascontiguousarray(x1[sl].T)
        m["x_tok"] = np.ascontiguousarray(x1[sl])
        maps.append(m)
    res = run_bass_kernel_spmd(nc, maps, core_ids=list(range(NCORES))).results
    return np.concatenate([np.asarray(res[c]["x2"]) for c in range(NCORES)], 0)


def build_W():
    b = Builder()
    NFC = D_FF_E // 128
    w13 = b.din("w13", [D, 2, D_FF_E])
    w2 = b.din("w2", [D_FF_E, D])
    w13b = b.dout("w13b", [NFC, 128, 8, 2, 128], BF)
    w2b = b.dout("w2b", [4, 128, NFC, 256], BF)
    ring = Rot([(b.sb([128, 2 * D_FF_E], BF), Tile(), f"r{i}") for i in range(3)])
    for kc in range(8):
        t, t_t, k = ring.next()
        tv = t[:].rearrange("p (j n) -> p j n", j=2)
        b.dma("pool", tv, w13[kc * 128:(kc + 1) * 128, :, :], writes=[t_t], key=k)
        for j in range(2):
            b.dma("sp", w13b[:, :, kc, j, :].rearrange("c p f -> p c f"),
                  tv[:, j, :].rearrange("p (c f) -> p c f", f=128), reads=[t_t], key="s" + k)
    w2v = w2.rearrange("(fc p) n -> p fc n", p=128)
    for g in range(4):
        t, t_t, k = ring.next()
        tv = t[:].rearrange("p (c n) -> p c n", n=D)
        b.dma("pool", tv, w2v[:, g * 7:(g + 1) * 7, :], writes=[t_t], key=k)
        for q in range(4):
            b.dma("sp", w2b[q][:, g * 7:(g + 1) * 7, :], tv[:, :, q * 256:(q + 1) * 256], reads=[t_t], key="s" + k)
    return b.finish()


def run_W(inp, j):
    nc = build_W()
    maps = [{"w13": np.ascontiguousarray(inp["exp_w13"][j][e]).reshape(D, 2, D_FF_E),
             "w2": np.ascontiguousarray(inp["exp_w2"][j][e])} for e in range(N_EXP)]
    res = run_bass_kernel_spmd(nc, maps, core_ids=list(range(N_EXP))).results
    w13b = np.stack([np.asarray(res[e]["w13b"]) for e in range(N_EXP)])
    w2b = np.stack([np.asarray(res[e]["w2b"]) for e in range(N_EXP)])
    return w13b, w2b


CAP = 384
NSB = CAP // 128


def build_FS():
    b = Builder()
    HT = 1024
    NT = HT // 128
    NHALF = TPC // HT
    NFC = D_FF_E // 128
    xT = b.din("xT", [D, TPC])
    x_tok = b.din("x_tok", [TPC, D])
    w13 = b.din("w13e", [N_EXP, NFC, 128, 8 * 2 * 128], BF)
    w2 = b.din("w2e", [N_EXP, 4, 128, NFC * 256], BF)
    lng_in = b.din("lng_b", [128, D])
    lnb_in = b.din("lnb_b", [128, D])
    eps_in = b.din("eps", [128, 1])
    rw_in = b.din("rw", [D, N_EXP])
    rb_in = b.din("rb_b", [128, N_EXP])
    ones_in = b.din("ones", [128, 128])
    us_in = b.din("ustrict", [128, 128])
    iota_in = b.din("iota", [128, CAP])
    id_in = b.din("ident", [128, 128])
    x2 = b.dout("x2", [TPC, D], F32)
    cnt_out = b.dout("cnt", [1, NHALF * N_EXP], F32)

    t_par = Tile()
    lng = b.sb([128, D], F32)
    lnb = b.sb([128, D], F32)
    eps = b.sb([128, 1], F32)
    rb = b.sb([128, N_EXP], F32)
    rw = b.sb([128, 8, N_EXP], F32)
    ONES = b.sb([128, 128], F32)
    US = b.sb([128, 128], F32)
    iota = b.sb([128, CAP], F32)
    ident = b.sb([128, 128], BF)
    for dst, src in ((lng, lng_in), (lnb, lnb_in), (eps, eps_in), (rb, rb_in), (ONES, ones_in), (US, us_in),
                     (iota, iota_in)):
        b.dma("sp", dst[:], src[:, :], writes=[t_par], key="ldp", nowaw=True)
    b.dma("sp", rw[:], rw_in.rearrange("(kc p) e -> p kc e", p=128), writes=[t_par], key="ldp", nowaw=True)
    t_id = Tile()
    b.dma("pool", ident[:], id_in[:, :], writes=[t_id], key="ldi")

    xtb = b.sb([128, NT, D], BF)
    t_xtb = Tile()
    Sel = b.sb([128, NT, CAP], BF)
    t_sel = Tile()
    SelT = b.sb([128, NSB, HT], BF)
    t_selT = [Tile() for _ in range(NSB)]
    xg = b.sb([128, 8, CAP], BF)
    t_xg = Tile()
    oe = b.sb([128, NSB, D], BF)
    t_oe = [Tile() for _ in range(NSB)]
    hT = b.sb([128, NFC, CAP], BF)
    t_hT = Tile()
    w13r = Rot([(b.sb([128, 8, 2, 128], BF), Tile(), f"w13{i}") for i in range(3)])
    w2r = Rot([(b.sb([128, NFC, 256], BF), Tile(), f"w2{i}") for i in range(2)])
    acc = b.sb([128, NT, D], F32)
    t_acc = [Tile() for _ in range(NT)]
    sar = Rot([(b.sb([128, CAP], F32), Tile()) for _ in range(2)])
    xtr = Rot([(b.sb([128, D], F32), Tile(), f"xt{i}") for i in range(2)])
    otr = Rot([(b.sb([128, D], F32), Tile(), f"ot{i}") for i in range(2)])
    lns = LNS(b)
    psr = Rot([(b.ps([128, 512]), Tile()) for _ in range(4)])
    pcr = Rot([(b.ps([128, 512]), Tile()) for _ in range(2)])
    tp = (b.ps([128, NT, 128], BF), Tile())
    pm = (b.ps([128, 512]), Tile())
    gates = b.sb([128, TPC // 128, N_EXP], F32)
    mska = b.sb([128, TPC // 128, N_EXP], F32)
    possb = b.sb([128, NT, N_EXP], F32)
    cntsb = b.sb([128, NHALF * N_EXP], F32)
    t_gate = [Tile() for _ in range(TPC // 128)]
    t_mska = [Tile() for _ in range(TPC // 128)]
    t_pos = [Tile() for _ in range(NT)]
    t_cnt = Tile()
    xfr = Rot([(b.sb([128, 8, 128], F32), Tile(), f"xf{i}") for i in range(2)])
    lg = b.sb([128, N_EXP], F32)
    mx = b.sb([128, 8], F32)
    nm1 = b.sb([128, 1], F32)
    ex = b.sb([128, N_EXP], F32)
    den = b.sb([128, 1], F32)
    rden = b.sb([128, 1], F32)
    t_lg, t_mx, t_nm1, t_ex, t_den, t_rden = (Tile() for _ in range(6))

    xTv = xT.rearrange("(kc p) t -> p kc t", p=128)
    for half in range(NHALF):
        t0 = half * HT
        xtv = x_tok[t0:t0 + HT, :].rearrange("(n p) d -> p n d", p=128)
        for n0 in range(0, NT, 2):
            b.dma("pool", xtb[:, n0:n0 + 2, :], xtv[:, n0:n0 + 2, :], writes=[t_xtb], key="ldx", nowaw=(n0 > 0))
        for ti in range(NT):
            tt = half * NT + ti
            xf, t_xf, k_xf = xfr.next()
            b.dma("sp", xf[:], xTv[:, :, tt * 128:(tt + 1) * 128], writes=[t_xf], key=k_xf)
            b.mm(pm[0][:, 0:N_EXP], [(xf[:, kc, :], rw[:, kc, :]) for kc in range(8)], [t_xf, t_par], [pm[1]])
            b.tt(lg[:], pm[0][:, 0:N_EXP], rb[:], ALU.add, [pm[1], t_par], [t_lg, pm[1]])
            b.P.op("dve", lambda e: e.max(out=mx[:], in_=lg[:]), [t_lg], [t_mx])
            b.ts(mska[:, tt, :], lg[:], mx[:, 1:2], ALU.is_ge, [t_lg, t_mx], [t_mska[tt]])
            b.ts(nm1[:], mx[:, 0:1], -1.0, ALU.mult, [t_mx], [t_nm1])
            b.act(ex[:], lg[:], AF.Exp, [t_lg, t_nm1], [t_ex], bias=nm1[:, 0:1])
            b.tt(ex[:], ex[:], mska[:, tt, :], ALU.mult, [t_ex, t_mska[tt]], [t_ex])
            b.P.op("dve", lambda e: e.reduce_sum(out=den[:], in_=ex[:], axis=mybir.AxisListType.X), [t_ex], [t_den])
            b.P.op("dve", lambda e: e.reciprocal(out=rden[:], in_=den[:]), [t_den], [t_rden])
            b.ts(gates[:, tt, :], ex[:], rden[:, 0:1], ALU.mult, [t_ex, t_rden], [t_gate[tt]])
        mrd = [t_mska[half * NT + ti] for ti in range(NT)]
        for ti in range(NT):
            pairs = [(ONES[:], mska[:, half * NT + tp_, :]) for tp_ in range(ti)] + [(US[:], mska[:, half * NT + ti, :])]
            b.mm(pm[0][:, 0:N_EXP], pairs, mrd[:ti + 1] + [t_par], [pm[1]])
            b.copy(possb[:, ti, :], pm[0][:, 0:N_EXP], [pm[1]], [t_pos[ti], pm[1]])
        b.mm(pm[0][:, 0:N_EXP], [(ONES[:], mska[:, half * NT + ti, :]) for ti in range(NT)], mrd + [t_par], [pm[1]])
        b.copy(cntsb[:, half * N_EXP:(half + 1) * N_EXP], pm[0][:, 0:N_EXP], [pm[1]], [t_cnt, pm[1]])
        for e_ in range(N_EXP):
            for ti in range(NT):
                tt = half * NT + ti
                b.ts(Sel[:, ti, :], iota[:, :], possb[:, ti, e_:e_ + 1], ALU.is_equal,
                     [t_par, t_pos[ti], t_mska[tt]], [t_sel], s2=mska[:, tt, e_:e_ + 1], op1=ALU.mult, nowaw=(ti > 0))
            for kc in range(8):
                pg_, t_pg_ = psr.next()
                b.mm(pg_[:, 0:CAP], [(xtb[:, ti, kc * 128:(kc + 1) * 128], Sel[:, ti, :]) for ti in range(NT)],
                     [t_xtb, t_sel], [t_pg_])
                b.act(xg[:, kc, :], pg_[:, 0:CAP], AF.Copy, [t_pg_], [t_xg, t_pg_])
            for sb_ in range(NSB):
                def ftp(e, sb_=sb_):
                    for ti in range(NT):
                        ins = e.transpose(tp[0][:, ti, :], Sel[:, ti, sb_ * 128:(sb_ + 1) * 128], ident[:])
                    return ins
                b.pe(ftp, [t_sel, t_id], [tp[1]])
                b.copy(SelT[:, sb_, :], tp[0][:].rearrange("p n c -> p (n c)"), [tp[1]], [t_selT[sb_], tp[1]])
            w2q = {}
            for fc in range(NFC):
                wt, t_w, k_w = w13r.next()
                b.dma("sp", wt[:].rearrange("p a j f -> p (a j f)"), w13[e_, fc], writes=[t_w], key=k_w)
                if fc in (2, 4):
                    q = 0 if fc == 2 else 1
                    w2q[q] = w2r.next()
                    b.dma("pool", w2q[q][0][:].rearrange("p c n -> p (c n)"), w2[e_, q], writes=[w2q[q][1]],
                          key=w2q[q][2])
                pa, t_pa = psr.next()
                pg, t_pg = psr.next()
                b.mm(pa[:, 0:CAP], [(wt[:, kc, 0, :], xg[:, kc, :]) for kc in range(8)], [t_w, t_xg], [t_pa])
                b.mm(pg[:, 0:CAP], [(wt[:, kc, 1, :], xg[:, kc, :]) for kc in range(8)], [t_w, t_xg], [t_pg])
                sa, t_sa = sar.next()
                b.act(sa[:], pa[:, 0:CAP], AF.Silu, [t_pa], [t_sa, t_pa])
                b.tt(hT[:, fc, :], pg[:, 0:CAP], sa[:], ALU.mult, [t_pg, t_sa], [t_hT, t_pg], nowaw=(fc > 0))
            for q in range(4):
                if q >= 2:
                    w2q[q] = w2r.next()
                    b.dma("pool", w2q[q][0][:].rearrange("p c n -> p (c n)"), w2[e_, q], writes=[w2q[q][1]],
                          key=w2q[q][2])
                wq, t_wq, _ = w2q[q]
                for sb_ in range(NSB):
                    py, t_py = psr.next()
                    b.mm(py[:, 0:256], [(hT[:, fc, sb_ * 128:(sb_ + 1) * 128], wq[:, fc, :]) for fc in range(NFC)],
                         [t_hT, t_wq], [t_py])
                    b.act(oe[:, sb_, q * 256:(q + 1) * 256], py[:, 0:256], AF.Copy, [t_py], [t_oe[sb_], t_py],
                          )
            for ti in range(NT):
                tt = half * NT + ti
                for nh in range(2):
                    pc, t_pc = pcr.next()
                    b.mm(pc[:, :], [(SelT[:, sb_, ti * 128:(ti + 1) * 128], oe[:, sb_, nh * 512:(nh + 1) * 512])
                                    for sb_ in range(NSB)], t_selT + t_oe, [t_pc])
                    dst = acc[:, ti, nh * 512:(nh + 1) * 512]
                    g = gates[:, tt, e_:e_ + 1]
                    if e_ == 0:
                        b.ts(dst, pc[:, :], g, ALU.mult, [t_pc, t_gate[tt]], [t_acc[ti], t_pc], nowaw=(nh > 0))
                    else:
                        b.stt(dst, pc[:, :], g, dst, ALU.mult, ALU.add, [t_pc, t_gate[tt], t_acc[ti]],
                              [t_acc[ti], t_pc])
        for ti in range(NT):
            tt = half * NT + ti
            rows = slice(tt * 128, (tt + 1) * 128)
            xt, t_xt, k_xt = xtr.next()
            b.dma("sp", xt[:], x_tok[rows, :], writes=[t_xt], key=k_xt)
            b.stt(xt[:], xt[:], ALPHA, acc[:, ti, :], ALU.mult, ALU.add, [t_xt, t_acc[ti]], [t_xt])
            ot, t_ot, k_ot = otr.next()
            ln_tile2(b, xt, t_xt, ot, t_ot, lng, lnb, eps, t_par, lns)
            b.dma("sp", x2[rows, :], ot[:], reads=[t_ot], key=k_ot)
    b.dma("sp", cnt_out[:, :], cntsb[0:1, :], reads=[t_cnt], key="stc")
    return b.finish()


def run_FS(x1, inp, l):
    j = l // 2
    w13b, w2b = run_W(inp, j)
    a = np.arange(128)
    com = {"lng_b": bcast128(inp["ln_g"][l, 1]), "lnb_b": bcast128(inp["ln_b"][l, 1]),
           "eps": np.full((128, 1), LN_EPS, np.float32),
           "rw": np.ascontiguousarray(inp["router_w"][j]), "rb_b": bcast128(inp["router_b"][j]),
           "w13e": w13b.reshape(N_EXP, D_FF_E // 128, 128, 2048),
           "w2e": w2b.reshape(N_EXP, 4, 128, (D_FF_E // 128) * 256),
           "ones": np.ones((128, 128), np.float32),
           "ustrict": (a[:, None] < a[None, :]).astype(np.float32),
           "iota": bcast128(np.arange(CAP, dtype=np.float32)),
           "ident": np.eye(128, dtype=np.float32)}
    nc = build_FS()
    maps = []
    for c in range(NCORES):
        sl = slice(c * TPC, (c + 1) * TPC)
        m = dict(com)
        m["xT"] = np.ascontiguousarray(x1[sl].T)
        m["x_tok"] = np.ascontiguousarray(x1[sl])
        maps.append(m)
    res = run_bass_kernel_spmd(nc, maps, core_ids=list(range(NCORES))).results
    x2 = np.concatenate([np.asarray(res[c]["x2"]) for c in range(NCORES)], 0)
    cnt = max(float(np.asarray(res[c]["cnt"]).max()) for c in range(NCORES))
    return x2, cnt


def _tok(res, name):
    return np.ascontiguousarray(np.concatenate([np.asarray(res[c][name]) for c in range(NCORES)], axis=1).T)


def kernel(**inp):
    inp = {k: np.asarray(v) for k, v in inp.items()}
    x = np.ascontiguousarray(inp["x"][0], dtype=np.float32)
    for l in range(DEPTH):
        rp = run_P(make_xT_ext(x), inp, l)
        parts = run_A(_tok(rp, "qaT"), _tok(rp, "kaT"), _tok(rp, "vaT"), inp["rel_bias"])
        h_raw = run_M(_tok(rp, "qmT"), _tok(rp, "kmT"), _tok(rp, "vmT"), _tok(rp, "ipre"), _tok(rp, "logf"))
        x1 = run_D1(x, parts, h_raw, _tok(rp, "sigzT"), inp, l)
        if l % 2 == 0:
            x = run_F(x1, inp, l)
        else:
            x2, cnt = run_FS(x1, inp, l)
            x = x2 if cnt <= CAP else run_F(x1, inp, l)
    return x[None].astype(np.float32)
```
